# Optimizing a Trainium2 kernel written in Bass

```python
import math
import jax
import jax.numpy as jnp
from jax import lax
import numpy as np

D_MODEL = 1024
BATCH = 4
SEQ = 4096
DEPTH = 1

CTX_LEN = 256
GRID_W = 64
SSM_CH_PER_GROUP = 16
SSM_GROUPS = 32
SSM_WIDTH = SSM_GROUPS * SSM_CH_PER_GROUP
SSM_STATE = 64
SSM_DT_MIN = 1e-3
SSM_DT_MAX = 1e-1
HEAD_DIM = 64
N_Q_HEADS = 8
N_KV_HEADS = 2
Q_PER_KV = N_Q_HEADS // N_KV_HEADS
ATTN_WIDTH = N_Q_HEADS * HEAD_DIM
KV_WIDTH = N_KV_HEADS * HEAD_DIM
WINDOW = 128
ATTN_BLOCK = WINDOW
ROPE_BASE = 10000.0
ROPE_PAIRS_PER_AXIS = HEAD_DIM // 4
IN_COLS = SSM_WIDTH + ATTN_WIDTH + 2 * KV_WIDTH + 2 * D_MODEL
N_EXPERTS = 32
TOP_K = 4
D_EXPERT = D_MODEL
SWIGLU_LIMIT = 7.0
SWIGLU_ALPHA = 1.702
MOE_BLOCK = 128
LN_EPS = 1e-5
DEEPNORM_ALPHA = (2 * DEPTH) ** 0.25
DEEPNORM_BETA = (8 * DEPTH) ** -0.25
NEG_INF = -1e30

kernel_name = 'hybrid_s5_swa_moe_prefix_layer'


def layer_norm(x, g, b):
    xf = x.astype(jnp.float32)
    mu = xf.mean(-1, keepdims=True)
    var = jnp.square(xf - mu).mean(-1, keepdims=True)
    y = (xf - mu) * lax.rsqrt(var + LN_EPS) * g.astype(jnp.float32) + b.astype(jnp.float32)
    return y.astype(x.dtype)


def modulate(x, shift, scale):
    return x * (1 + scale) + shift


def split_proj(p):
    i0 = SSM_WIDTH
    i1 = i0 + ATTN_WIDTH
    i2 = i1 + KV_WIDTH
    i3 = i2 + KV_WIDTH
    i4 = i3 + D_MODEL
    return jnp.split(p, [i0, i1, i2, i3, i4], axis=-1)


def to_heads(t, n_heads):
    return t.reshape(t.shape[:-1] + (n_heads, HEAD_DIM))


def to_groups(t):
    return t.reshape(t.shape[:-1] + (SSM_GROUPS, SSM_CH_PER_GROUP))


def rope_2d(x, row_ids, col_ids):
    inv = ROPE_BASE ** (-jnp.arange(ROPE_PAIRS_PER_AXIS, dtype=jnp.float32) / ROPE_PAIRS_PER_AXIS)
    ang = jnp.concatenate([row_ids.astype(jnp.float32)[:, None] * inv,
                           col_ids.astype(jnp.float32)[:, None] * inv], axis=-1)
    cos = jnp.cos(ang)[None, :, None, :].astype(x.dtype)
    sin = jnp.sin(ang)[None, :, None, :].astype(x.dtype)
    x1, x2 = jnp.split(x, 2, axis=-1)
    return jnp.concatenate([x1 * cos - x2 * sin, x2 * cos + x1 * sin], axis=-1)


def cplx_combine(e1, e2):
    a1r, a1i, b1r, b1i = e1
    a2r, a2i, b2r, b2i = e2
    return (a2r * a1r - a2i * a1i,
            a2r * a1i + a2i * a1r,
            a2r * b1r - a2i * b1i + b2r,
            a2r * b1i + a2i * b1r + b2i)


def s5_discretize(lam_re, lam_im, log_step, b_re, b_im):
    f32 = jnp.float32
    lr, li = lam_re.astype(f32), lam_im.astype(f32)
    dt = jnp.exp(log_step.astype(f32))[:, None]
    mag = jnp.exp(lr * dt)
    ar = mag * jnp.cos(li * dt)
    ai = mag * jnp.sin(li * dt)
    den = lr * lr + li * li
    cr = ((ar - 1) * lr + ai * li) / den
    ci = (ai * lr - (ar - 1) * li) / den
    br, bi = b_re.astype(f32), b_im.astype(f32)
    bbr = cr[..., None] * br - ci[..., None] * bi
    bbi = cr[..., None] * bi + ci[..., None] * br
    return ar, ai, bbr, bbi


def s5_scan(u, disc, s0r, s0i, reverse):
    ar, ai, bbr, bbi = disc
    uf = u.astype(jnp.float32)
    xr = jnp.einsum('blgh,gph->blgp', uf, bbr)
    xi = jnp.einsum('blgh,gph->blgp', uf, bbi)
    edge = u.shape[1] - 1 if reverse else 0
    xr = xr.at[:, edge].add(ar * s0r - ai * s0i)
    xi = xi.at[:, edge].add(ar * s0i + ai * s0r)
    shape = (1, u.shape[1]) + ar.shape
    _, _, sr, si = lax.associative_scan(
        cplx_combine, (jnp.broadcast_to(ar, shape), jnp.broadcast_to(ai, shape), xr, xi),
        reverse=reverse, axis=1)
    return sr, si


def s5_readout(u, sf, sb, c_re, c_im, d_skip):
    cr = c_re.astype(jnp.float32)
    ci = c_im.astype(jnp.float32)
    y = (jnp.einsum('blgp,ghp->blgh', sf[0], cr[0]) - jnp.einsum('blgp,ghp->blgh', sf[1], ci[0])
         + jnp.einsum('blgp,ghp->blgh', sb[0], cr[1]) - jnp.einsum('blgp,ghp->blgh', sb[1], ci[1])
         + d_skip.astype(jnp.float32) * u.astype(jnp.float32))
    return y.reshape(u.shape[:2] + (SSM_WIDTH,)).astype(u.dtype)


def window_attention(q, k, v, kc, vc, sink):
    bsz, seq_len = q.shape[:2]
    n_ctx = kc.shape[1]
    nb = seq_len // ATTN_BLOCK
    qb = q.reshape(bsz, nb, ATTN_BLOCK, N_KV_HEADS, Q_PER_KV, HEAD_DIM) * HEAD_DIM ** -0.5
    pad = ((0, 0), (ATTN_BLOCK, ATTN_BLOCK), (0, 0), (0, 0))

    def band(t):
        tp = jnp.pad(t, pad).reshape(bsz, nb + 2, ATTN_BLOCK, N_KV_HEADS, HEAD_DIM)
        return jnp.concatenate([tp[:, :-2], tp[:, 1:-1], tp[:, 2:]], axis=2)

    kb, vb = band(k), band(v)
    s_loc = jnp.einsum('bnqhgd,bnkhd->bhgnqk', qb, kb).astype(jnp.float32)
    s_ctx = jnp.einsum('bnqhgd,bchd->bhgnqc', qb, kc).astype(jnp.float32)
    blk = jnp.arange(nb)[:, None, None]
    qpos = blk * ATTN_BLOCK + jnp.arange(ATTN_BLOCK)[None, :, None]
    kpos = (blk - 1) * ATTN_BLOCK + jnp.arange(3 * ATTN_BLOCK)[None, None, :]
    valid = (jnp.abs(kpos - qpos) <= WINDOW) & (kpos >= 0) & (kpos < seq_len)
    s_loc = jnp.where(valid, s_loc, NEG_INF)
    snk = sink.astype(jnp.float32).reshape(N_KV_HEADS, Q_PER_KV)[None, :, :, None, None, None]
    snk = jnp.broadcast_to(snk, s_loc.shape[:-1] + (1,))
    p = jax.nn.softmax(jnp.concatenate([s_loc, s_ctx, snk], axis=-1), axis=-1).astype(v.dtype)
    p_loc = p[..., :3 * ATTN_BLOCK]
    p_ctx = p[..., 3 * ATTN_BLOCK:3 * ATTN_BLOCK + n_ctx]
    o = (jnp.einsum('bhgnqk,bnkhd->bnqhgd', p_loc, vb)
         + jnp.einsum('bhgnqc,bchd->bnqhgd', p_ctx, vc))
    return o.reshape(bsz, seq_len, ATTN_WIDTH)


def context_attention(q, k, v, sink):
    bsz, n_ctx = q.shape[:2]
    qg = q.reshape(bsz, n_ctx, N_KV_HEADS, Q_PER_KV, HEAD_DIM) * HEAD_DIM ** -0.5
    s = jnp.einsum('bqhgd,bkhd->bhgqk', qg, k).astype(jnp.float32)
    snk = sink.astype(jnp.float32).reshape(N_KV_HEADS, Q_PER_KV)[None, :, :, None, None]
    snk = jnp.broadcast_to(snk, s.shape[:-1] + (1,))
    p = jax.nn.softmax(jnp.concatenate([s, snk], axis=-1), axis=-1)[..., :n_ctx].astype(v.dtype)
    o = jnp.einsum('bhgqk,bkhd->bqhgd', p, v)
    return o.reshape(bsz, n_ctx, ATTN_WIDTH)


def merge_branches(y_ssm, o_att, g_ssm, g_att, w_glu, b_glu, w_ssm_out, w_att_out, w_o):
    z = jax.nn.gelu(y_ssm)
    z = z * jax.nn.sigmoid(z @ w_glu + b_glu)
    m = jax.nn.sigmoid(g_ssm) * (z @ w_ssm_out) + jax.nn.sigmoid(g_att) * (o_att @ w_att_out)
    return m @ w_o


def moe_ffn(h, w_router, b_router, w_gate_up, b_gate_up, w_down, b_down):
    bsz, n_tok, d = h.shape
    t = bsz * n_tok
    xf = h.reshape(t, d)
    logits = (xf @ w_router + b_router).astype(jnp.float32)
    top_v, top_i = lax.top_k(logits, TOP_K)
    gates = jax.nn.softmax(top_v, axis=-1).astype(h.dtype)
    n_rows = t * TOP_K
    e_flat = top_i.reshape(n_rows)
    tok_flat = jnp.arange(n_rows, dtype=jnp.int32) // TOP_K
    order = jnp.argsort(e_flat)
    e_sorted = e_flat[order]
    counts = jnp.bincount(e_flat, length=N_EXPERTS)
    padded = (counts + MOE_BLOCK - 1) // MOE_BLOCK * MOE_BLOCK
    start = jnp.cumsum(counts) - counts
    pad_end = jnp.cumsum(padded)
    pad_start = pad_end - padded
    dest = pad_start[e_sorted] + jnp.arange(n_rows) - start[e_sorted]
    cap = n_rows + N_EXPERTS * MOE_BLOCK
    n_blocks = cap // MOE_BLOCK
    row_tok = jnp.full((cap,), t, jnp.int32).at[dest].set(tok_flat[order])
    row_gate = jnp.zeros((cap,), h.dtype).at[dest].set(gates.reshape(n_rows)[order])
    block_exp = jnp.minimum(
        jnp.searchsorted(pad_end, jnp.arange(n_blocks) * MOE_BLOCK, side='right'), N_EXPERTS - 1)
    x_rows = jnp.concatenate([xf, jnp.zeros((1, d), h.dtype)], axis=0)[row_tok]
    x_rows = x_rows.reshape(n_blocks, MOE_BLOCK, d)

    def expert_block(args):
        xb, e = args
        gu = xb @ w_gate_up[e] + b_gate_up[e]
        glu, lin = jnp.split(gu, 2, axis=-1)
        glu = jnp.minimum(glu, SWIGLU_LIMIT)
        lin = jnp.clip(lin, -SWIGLU_LIMIT, SWIGLU_LIMIT)
        act = glu * jax.nn.sigmoid(SWIGLU_ALPHA * glu) * (lin + 1)
        return act @ w_down[e] + b_down[e]

    y_rows = lax.map(expert_block, (x_rows, block_exp)).reshape(cap, d)
    y = jnp.zeros((t + 1, d), h.dtype).at[row_tok].add(y_rows * row_gate[:, None])
    return y[:t].reshape(bsz, n_tok, d)


def setup_inputs(seed: int = 0) -> dict:
    key = jax.random.key(seed)
    ks = jax.random.split(key, 36)
    f32 = jnp.float32

    def nrm(i, shape, scale):
        return scale * jax.random.normal(ks[i], shape, f32)

    G, P, H, E, F = SSM_GROUPS, SSM_STATE, SSM_CH_PER_GROUP, N_EXPERTS, D_EXPERT
    n_idx = jnp.arange(P, dtype=f32)
    return {
        'x': nrm(0, (BATCH, SEQ, D_MODEL), 1.0),
        'c': nrm(1, (BATCH, D_MODEL), 1.0),
        'ctx': nrm(2, (BATCH, CTX_LEN, D_MODEL), 1.0),
        'c_ctx': nrm(3, (D_MODEL,), 1.0),
        'ln_in_g': 1.0 + nrm(4, (D_MODEL,), 0.02),
        'ln_in_b': nrm(5, (D_MODEL,), 0.02),
        'w_mod': nrm(6, (DEPTH, D_MODEL, 6 * D_MODEL), 0.5 * D_MODEL ** -0.5),
        'b_mod': nrm(7, (DEPTH, 6 * D_MODEL), 0.02),
        'w_in': nrm(8, (DEPTH, D_MODEL, IN_COLS), D_MODEL ** -0.5),
        'ssm_lam_re': -0.5 + nrm(9, (DEPTH, 2, G, P), 0.01),
        'ssm_lam_im': math.pi * n_idx + nrm(10, (DEPTH, 2, G, P), 0.01),
        'ssm_log_step': jax.random.uniform(ks[11], (DEPTH, 2, G), f32,
                                           minval=math.log(SSM_DT_MIN), maxval=math.log(SSM_DT_MAX)),
        'ssm_b_re': nrm(12, (DEPTH, 2, G, P, H), (2 * H) ** -0.5),
        'ssm_b_im': nrm(13, (DEPTH, 2, G, P, H), (2 * H) ** -0.5),
        'ssm_c_re': nrm(14, (DEPTH, 2, G, H, P), (2 * P) ** -0.5),
        'ssm_c_im': nrm(15, (DEPTH, 2, G, H, P), (2 * P) ** -0.5),
        'ssm_d': nrm(16, (DEPTH, G, H), 1.0),
        'w_glu': nrm(17, (DEPTH, SSM_WIDTH, SSM_WIDTH), SSM_WIDTH ** -0.5),
        'b_glu': nrm(18, (DEPTH, SSM_WIDTH), 0.02),
        'attn_sink': nrm(19, (DEPTH, N_Q_HEADS), 0.5),
        'w_ssm_out': nrm(20, (DEPTH, SSM_WIDTH, D_MODEL), SSM_WIDTH ** -0.5),
        'w_att_out': nrm(21, (DEPTH, ATTN_WIDTH, D_MODEL), ATTN_WIDTH ** -0.5),
        'w_o': nrm(22, (DEPTH, D_MODEL, D_MODEL), DEEPNORM_BETA * D_MODEL ** -0.5),
        'ln1_g': 1.0 + nrm(23, (DEPTH, D_MODEL), 0.02),
        'ln1_b': nrm(24, (DEPTH, D_MODEL), 0.02),
        'w_router': nrm(25, (DEPTH, D_MODEL, E), D_MODEL ** -0.5),
        'b_router': nrm(26, (DEPTH, E), 0.01),
        'w_gate_up': nrm(27, (DEPTH, E, D_MODEL, 2 * F), D_MODEL ** -0.5),
        'b_gate_up': nrm(28, (DEPTH, E, 2 * F), 0.01),
        'w_down': nrm(29, (DEPTH, E, F, D_MODEL), DEEPNORM_BETA * F ** -0.5),
        'b_down': nrm(30, (DEPTH, E, D_MODEL), 0.01),
        'ln2_g': 1.0 + nrm(31, (DEPTH, D_MODEL), 0.02),
        'ln2_b': nrm(32, (DEPTH, D_MODEL), 0.02),
    }


def reference(x, c, ctx, c_ctx, ln_in_g, ln_in_b, w_mod, b_mod, w_in,
              ssm_lam_re, ssm_lam_im, ssm_log_step, ssm_b_re, ssm_b_im, ssm_c_re, ssm_c_im, ssm_d,
              w_glu, b_glu, attn_sink, w_ssm_out, w_att_out, w_o, ln1_g, ln1_b,
              w_router, b_router, w_gate_up, b_gate_up, w_down, b_down, ln2_g, ln2_b):
    bsz, seq_len, _ = x.shape
    rows = seq_len // GRID_W
    row_ids = jnp.repeat(jnp.arange(rows), GRID_W)
    col_ids = jnp.tile(jnp.arange(GRID_W), rows)
    zero_state = jnp.zeros((bsz, SSM_GROUPS, SSM_STATE), jnp.float32)
    h = layer_norm(x, ln_in_g, ln_in_b)
    hc = layer_norm(ctx, ln_in_g, ln_in_b)
    for l in range(DEPTH):
        mod = jax.nn.silu(c) @ w_mod[l] + b_mod[l]
        mod_c = jax.nn.silu(c_ctx) @ w_mod[l] + b_mod[l]
        sh1, sc1, g1, sh2, sc2, g2 = [m[:, None, :] for m in jnp.split(mod, 6, axis=-1)]
        sh1c, sc1c, g1c, sh2c, sc2c, g2c = jnp.split(mod_c, 6, axis=-1)
        disc_f = s5_discretize(ssm_lam_re[l, 0], ssm_lam_im[l, 0], ssm_log_step[l, 0],
                               ssm_b_re[l, 0], ssm_b_im[l, 0])
        disc_b = s5_discretize(ssm_lam_re[l, 1], ssm_lam_im[l, 1], ssm_log_step[l, 1],
                               ssm_b_re[l, 1], ssm_b_im[l, 1])

        uc = modulate(hc, sh1c, sc1c)
        s_in_c, q_c, k_c, v_c, gs_c, ga_c = split_proj(uc @ w_in[l])
        k_c = to_heads(k_c, N_KV_HEADS)
        v_c = to_heads(v_c, N_KV_HEADS)
        ug_c = to_groups(s_in_c)
        cf = s5_scan(ug_c, disc_f, zero_state, zero_state, reverse=False)
        cb = s5_scan(ug_c, disc_b, zero_state, zero_state, reverse=True)

        u = modulate(h, sh1, sc1)
        s_in, q, k, v, gs, ga = split_proj(u @ w_in[l])
        q = rope_2d(to_heads(q, N_Q_HEADS), row_ids, col_ids)
        k = rope_2d(to_heads(k, N_KV_HEADS), row_ids, col_ids)
        o_att = window_attention(q, k, to_heads(v, N_KV_HEADS), k_c, v_c, attn_sink[l])
        ug = to_groups(s_in)
        lf = s5_scan(ug, disc_f, cf[0][:, -1], cf[1][:, -1], reverse=False)
        lb = s5_scan(ug, disc_b, cb[0][:, 0], cb[1][:, 0], reverse=True)
        y_ssm = s5_readout(ug, lf, lb, ssm_c_re[l], ssm_c_im[l], ssm_d[l])
        mix = merge_branches(y_ssm, o_att, gs, ga, w_glu[l], b_glu[l], w_ssm_out[l], w_att_out[l], w_o[l])
        h_new = layer_norm(DEEPNORM_ALPHA * h + g1 * mix, ln1_g[l], ln1_b[l])
        ffn = moe_ffn(modulate(h_new, sh2, sc2), w_router[l], b_router[l],
                      w_gate_up[l], b_gate_up[l], w_down[l], b_down[l])
        h_new = layer_norm(DEEPNORM_ALPHA * h_new + g2 * ffn, ln2_g[l], ln2_b[l])

        if l + 1 < DEPTH:
            o_att_c = context_attention(to_heads(q_c, N_Q_HEADS), k_c, v_c, attn_sink[l])
            y_ssm_c = s5_readout(ug_c, cf, cb, ssm_c_re[l], ssm_c_im[l], ssm_d[l])
            mix_c = merge_branches(y_ssm_c, o_att_c, gs_c, ga_c, w_glu[l], b_glu[l],
                                   w_ssm_out[l], w_att_out[l], w_o[l])
            hc = layer_norm(DEEPNORM_ALPHA * hc + g1c * mix_c, ln1_g[l], ln1_b[l])
            ffn_c = moe_ffn(modulate(hc, sh2c, sc2c), w_router[l], b_router[l],
                            w_gate_up[l], b_gate_up[l], w_down[l], b_down[l])
            hc = layer_norm(DEEPNORM_ALPHA * hc + g2c * ffn_c, ln2_g[l], ln2_b[l])
        h = h_new
    return h
```

```python
import math
import contextlib
import numpy as np
import concourse.bass as bass
import concourse.mybir as mybir
from concourse.bass_utils import run_bass_kernel_spmd

F32 = mybir.dt.float32
BF16 = mybir.dt.bfloat16
ALU = mybir.AluOpType
AF = mybir.ActivationFunctionType

NTOK = 2048
DM = 1024
NE = 32
TWO_PI = 2.0 * math.pi
MAGIC = 12582912.0
ALPHA = 2.0 ** 0.25
LN_EPS = 1e-5
_CFG = {}


def _cfg(name, default=None):
    return _CFG.get(name, default)


class Sched:
    ENG = ('pe', 'dve', 'act', 'pool', 'sp')

    def __init__(self, nc):
        self.nc = nc
        self.ops = {e: [] for e in self.ENG}
        self.state = {}
        self.dma_cnt = {}
        self.nsig = {}
        self.barrier_deps = set()

    def barrier(self):
        b = set()
        for e in self.ENG:
            for idx in range(len(self.ops[e]) - 1, -1, -1):
                if self.ops[e][idx]['dma'] is None:
                    b.add(('c', e, idx))
                    break
        for sname, c in self.dma_cnt.items():
            b.add(('d', sname, c))
        self.barrier_deps = b

    def _deps(self, reads, writes):
        deps = set()
        for k in writes:
            if k not in self.state:
                deps.update(self.barrier_deps)
        for k in reads:
            st = self.state.get(k)
            if st and st[0] is not None:
                deps.add(st[0])
        for k in writes:
            st = self.state.get(k)
            if st:
                if st[0] is not None:
                    deps.add(st[0])
                deps.update(st[1])
        return deps

    def _update(self, ev, reads, writes):
        for k in reads:
            st = self.state.setdefault(k, [None, []])
            st[1].append(ev)
        for k in writes:
            self.state[k] = [ev, []]

    def op(self, eng, fn, reads=(), writes=()):
        deps = self._deps(reads, writes)
        idx = len(self.ops[eng])
        ev = ('c', eng, idx)
        if eng == 'pe':
            deps = {d for d in deps if not (d[0] == 'c' and d[1] == 'pe')}
        self.ops[eng].append(dict(fn=fn, deps=deps, ev=ev, sig=False, dma=None))
        self._update(ev, reads, writes)
        return ev

    def dma(self, eng, fn, sem, reads=(), writes=()):
        deps = self._deps(reads, writes)
        c = self.dma_cnt.get(sem, 0) + 1
        self.dma_cnt[sem] = c
        if c > 1:
            deps.add(('d', sem, c - 1))
        ev = ('d', sem, c)
        self.ops[eng].append(dict(fn=fn, deps=deps, ev=ev, sig=False, dma=sem))
        self._update(ev, reads, writes)
        return ev

    def emit(self, final_waits=()):
        nc = self.nc
        for e in self.ENG:
            for o in self.ops[e]:
                for d in o['deps']:
                    if d[0] == 'c':
                        self.ops[d[1]][d[2]]['sig'] = True
        for e in self.ENG:
            n = 0
            for o in self.ops[e]:
                if o['dma'] is None and o['sig']:
                    n += 1
                    o['n'] = n
            self.nsig[e] = n
        with contextlib.ExitStack() as es:
            csem = {e: es.enter_context(nc.semaphore('c_' + e)) for e in self.ENG if self.nsig[e] > 0}
            dsem = {s: es.enter_context(nc.semaphore('d_' + str(s))) for s in self.dma_cnt}
            block = es.enter_context(nc.Block())
            ops = self.ops

            def run(e, engobj):
                waited = {}
                for o in ops[e]:
                    need = {}
                    for d in o['deps']:
                        if d[0] == 'c':
                            tgt = ops[d[1]][d[2]]['n']
                            key = ('c', d[1])
                        else:
                            tgt = 16 * d[2]
                            key = ('d', d[1])
                        if tgt > need.get(key, 0):
                            need[key] = tgt
                    for key in sorted(need):
                        tgt = need[key]
                        if waited.get(key, 0) >= tgt:
                            continue
                        waited[key] = tgt
                        engobj.wait_ge(csem[key[1]] if key[0] == 'c' else dsem[key[1]], tgt)
                    ins = o['fn'](engobj)
                    if o['dma'] is not None:
                        ins.then_inc(dsem[o['dma']], 16)
                    elif o['sig']:
                        ins.then_inc(csem[e], 1)
                if e == 'sp':
                    for ev in final_waits:
                        engobj.wait_ge(dsem[ev[1]], 16 * ev[2])

            @block.tensor
            def _(pe):
                run('pe', pe)

            @block.vector
            def _(v):
                run('dve', v)

            @block.scalar
            def _(a):
                run('act', a)

            @block.gpsimd
            def _(g):
                run('pool', g)

            @block.sync
            def _(s):
                run('sp', s)


class _Stop(Exception):
    pass


def build_nc(dbg=(), stop=None):
    nc = bass.Bass("TRN2", target_bir_lowering=False)
    S = Sched(nc)
    D = {}

    def din(name, shape, dt=F32):
        D[name] = nc.dram_tensor(name, list(shape), dt, kind="ExternalInput").ap()

    def dscr(name, shape, dt):
        D[name] = nc.dram_tensor(name, list(shape), dt, kind="Internal").ap()

    def dout(name, shape, dt=F32):
        D[name] = nc.dram_tensor(name, list(shape), dt, kind="ExternalOutput").ap()

    din('x_own', [NTOK, DM]); din('x_oth', [NTOK, DM]); din('ctxb', [256, DM])
    din('cvec', [128, 16]); din('ln_in_g', [1, DM]); din('ln_in_b', [1, DM])
    din('w_mod', [DM, 6144]); din('b_modT', [128, 48]); din('b_mod_row', [1, 6144])
    din('w_in', [DM, 3328])
    din('lamre', [128, 32]); din('lamim', [128, 32]); din('lstep', [128, 32])
    din('bre', [128, 512]); din('bim', [128, 512]); din('cre', [128, 512]); din('cim', [128, 512])
    din('dcol', [128, 4]); din('par', [128, 4])
    din('w_glu', [512, 512]); din('b_gluT', [128, 4]); din('sinkrow', [1, 1024])
    din('w_ssm_out', [512, DM]); din('w_att_out', [512, DM]); din('w_o', [DM, DM])
    din('ln1_g', [1, DM]); din('ln1_b', [1, DM]); din('w_router', [DM, NE]); din('b_router', [1, NE])
    din('w_gate_up', [NE, DM, 2048]); din('b_guT', [128, NE * 16]); din('w_down', [NE, DM, DM]); din('b_down', [NE, DM])
    din('ln2_g', [1, DM]); din('ln2_b', [1, DM])
    din('ident', [128, 128]); din('ropeC', [128, 2176]); din('ropeS', [128, 2176])
    din('masks', [128, 384]); din('iota1', [128, 512])
    dscr('h_scr', [NTOK, DM], F32); dscr('h1_scr', [NTOK, DM], F32)
    dscr('sgs_scr', [8, 128, NTOK], BF16); dscr('sga_scr', [8, 128, NTOK], BF16)
    dscr('soth_scr', [4, 128, NTOK], BF16); dscr('hmT_scr', [8, 128, NTOK], BF16); dscr('o_scr', [4, 128, NTOK], BF16)
    dout('out', [NTOK, DM])
    for item in dbg:
        dout(item[0], item[1], item[2] if len(item) > 2 else F32)

    es = contextlib.ExitStack()
    with es:
        def sb(name, shape, dt=F32):
            return es.enter_context(nc.sbuf_tensor('s_' + name, list(shape), dt))

        psb = [es.enter_context(nc.psum_tensor('psb%d' % i, [128, 512], F32)) for i in range(8)]
        psctr = [0]

        def nps():
            i = psctr[0] % 8
            psctr[0] += 1
            return psb[i], 'ps%d' % i

        def dma(eng, out, in_, sem, r=(), w=()):
            return S.dma(eng, lambda e: e.dma_start(out=out, in_=in_), sem, r, w)

        def mm(out, lhsT, rhs, start, stop, r=(), w=()):
            S.op('pe', lambda e: e.matmul(out, lhsT=lhsT, rhs=rhs, start=start, stop=stop), r, w)

        def tr(out, in_, ident, r=(), w=()):
            S.op('pe', lambda e: e.transpose(out, in_, ident), r, w)

        def act(out, in_, func, r=(), w=(), bias=None, scale=None, eng='act'):
            kw = {}
            if bias is not None:
                kw['bias'] = bias
            if scale is not None:
                kw['scale'] = scale
            S.op(eng, lambda e: e.activation(out=out, in_=in_, func=func, **kw), r, w)

        def tt(eng, out, in0, in1, op, r=(), w=()):
            S.op(eng, lambda e: e.tensor_tensor(out=out, in0=in0, in1=in1, op=op), r, w)

        def ts(eng, out, in0, s1, op0, r=(), w=(), s2=None, op1=None):
            if op1 is None:
                S.op(eng, lambda e: e.tensor_scalar(out=out, in0=in0, scalar1=s1, scalar2=None, op0=op0), r, w)
            else:
                S.op(eng, lambda e: e.tensor_scalar(out=out, in0=in0, scalar1=s1, scalar2=s2, op0=op0, op1=op1), r, w)

        def stt(eng, out, in0, scalar, in1, op0, op1, r=(), w=()):
            S.op(eng, lambda e: e.scalar_tensor_tensor(out=out, in0=in0, scalar=scalar, in1=in1, op0=op0, op1=op1), r, w)

        def cp(eng, out, in_, r=(), w=()):
            if eng == 'act':
                S.op(eng, lambda e: e.copy(out=out, in_=in_), r, w)
            else:
                S.op(eng, lambda e: e.tensor_copy(out=out, in_=in_), r, w)

        def mset(eng, ap, val, w=()):
            S.op(eng, lambda e: e.memset(ap, val), (), w)

        def scan(eng, out, d0, d1, init, r=(), w=()):
            S.op(eng, lambda e: e.tensor_tensor_scan(out=out, data0=d0, data1=d1, initial=init,
                                                     op0=ALU.mult, op1=ALU.add), r, w)

        def pbc(ap):
            return ap.partition_broadcast(128)

        CONST = {}
        OPEN = []

        def rstd(st, sk):
            act(st[:, 14:15], st[:, 13:14], AF.Sqrt, r=[sk, 'epsT'], w=[sk], bias=CONST['epsT'][:, 0:1])
            S.op('dve', lambda e: e.reciprocal(out=st[:, 14:15], in_=st[:, 14:15]), [sk], [sk])

        def debug_out(name, src_ap, keys, eng='pool'):
            if any(it[0] == name for it in dbg):
                dma(eng, D[name], src_ap, 'dbg_' + name, r=list(keys))

        def finish(finals=()):
            fw = {}
            for ev in finals:
                fw[ev[1]] = max(fw.get(ev[1], 0), ev[2])
            for it in dbg:
                name = it[0]
                s_ = 'dbg_' + name
                if s_ in S.dma_cnt:
                    fw[s_] = S.dma_cnt[s_]
            S.emit(final_waits=[('d', k, v) for k, v in fw.items()])

        def checkpoint(tag):
            if stop == tag:
                raise _Stop()

        def body():
            identf = sb('identf', [128, 128])
            dma('sp', identf[:], D['ident'], 'c_ident', w=['identf'])
            halfpi = sb('halfpi', [128, 1])
            mset('dve', halfpi[:], 0.5 * math.pi, w=['halfpi'])
            epsT = sb('epsT', [128, 1])
            mset('dve', epsT[:], LN_EPS, w=['epsT'])
            CONST['epsT'] = epsT
            ones_f = sb('ones_f', [128, 128])
            mset('dve', ones_f[:], 1.0, w=['ones_f'])
            ones_b = sb('ones_b', [128, 128], BF16)
            mset('dve', ones_b[:], 1.0, w=['ones_b'])
            iota1 = sb('iota1', [128, 512])
            dma('sp', iota1[:], D['iota1'], 'c_iota', w=['iota1'])
            masks = sb('masks', [128, 384], BF16)
            dma('pool', masks[:], D['masks'], 'c_masks', w=['masks'])
            dcol = sb('dcol', [128, 4])
            dma('sp', dcol[:], D['dcol'], 'c_dcol', w=['dcol'])
            par = sb('par', [128, 4])
            dma('sp', par[:], D['par'], 'c_par', w=['par'])
            bgluT = sb('bgluT', [128, 4])
            dma('sp', bgluT[:], D['b_gluT'], 'c_bglu', w=['bgluT'])
            esink = sb('esink', [1, 1024], BF16)
            with nc.sbuf_tensor('s_sinkf', [1, 1024], F32) as sinkf:
                dma('sp', sinkf[:], D['sinkrow'], 'c_sink', w=['sinkf'])
                act(esink[:], sinkf[:], AF.Exp, r=['sinkf'], w=['esink'])
            S.barrier()

            cv = sb('cv', [128, 16])
            dma('sp', cv[:], D['cvec'], 'c_cv', w=['cv'])
            sc_ = sb('sc_', [128, 16])
            act(sc_[:], cv[:], AF.Silu, r=['cv'], w=['sc_'])
            sc3 = sc_[:].rearrange("p (k w) -> p k w", w=2)
            bmodT = sb('bmodT', [128, 48])
            dma('sp', bmodT[:], D['b_modT'], 'c_bmodT', w=['bmodT'])
            modT = sb('modT', [128, 32])
            modbc = sb('modbc', [128, 4096])
            dma('sp', modbc[:], pbc(D['b_mod_row'][:, 2048:6144]), 'c_modbc', w=['modbc'])
            wm_v = D['w_mod'].rearrange("(k p) c -> p k c", p=128)
            with contextlib.ExitStack() as es0:
                sbc = es0.enter_context(nc.sbuf_tensor('s_sbc', [128, 8, 128], F32))
                sbcx = es0.enter_context(nc.sbuf_tensor('s_sbcx', [128, 8, 128], F32))
                tmpd = es0.enter_context(nc.sbuf_tensor('s_tmpd', [128, 128], F32))
                mT3 = modT[:].rearrange("p (c w) -> p c w", w=2)
                for k in range(8):
                    cp('dve', sbc[:, k, :], sc3[:, k, 0:1].to_broadcast([128, 128]), r=['sc_'], w=['sbc'])
                    cp('dve', sbcx[:, k, :], sc3[:, k, 1:2].to_broadcast([128, 128]), r=['sc_'], w=['sbcx'])
                wmb = [es0.enter_context(nc.sbuf_tensor('s_wmb%d' % i, [128, 8, 512], F32)) for i in range(2)]
                for i in range(12):
                    wb = wmb[i % 2]
                    wk = 'wmb%d' % (i % 2)
                    dma('sp', wb[:], wm_v[:, :, i * 512:(i + 1) * 512], wk, w=[wk])
                    if i < 4:
                        for which, sbw, sbk in ((0, sbc, 'sbc'), (1, sbcx, 'sbcx')):
                            ps, pk = nps()
                            for k in range(8):
                                mm(ps[:], sbw[:, k, :], wb[:, k, :], k == 0, k == 7, r=[wk, sbk], w=[pk])
                            for ctl in range(4):
                                ct = i * 4 + ctl
                                tt('dve', tmpd[:], ps[:, ctl * 128:(ctl + 1) * 128], identf[:], ALU.mult, r=[pk, 'identf'], w=['tmpd'])
                                S.op('dve', lambda e, ct=ct, which=which: e.reduce_sum(out=mT3[:, ct, which:which + 1], in_=tmpd[:],
                                                                                     axis=mybir.AxisListType.X), ['tmpd'], ['modT'])
                    else:
                        ps, pk = nps()
                        for k in range(8):
                            mm(ps[:], sbc[:, k, :], wb[:, k, :], k == 0, k == 7, r=[wk, 'sbc'], w=[pk])
                        c0 = (i - 4) * 512
                        tt('dve', modbc[:, c0:c0 + 512], ps[:], modbc[:, c0:c0 + 512], ALU.add, r=[pk, 'modbc'], w=['modbc'])
                tt('dve', mT3, mT3, bmodT[:, 0:16].unsqueeze(2).to_broadcast([128, 16, 2]), ALU.add, r=['modT', 'bmodT'], w=['modT'])
            S.barrier()
            onep = sb('onep', [128, 16])
            ts('dve', onep[:], modT[:, 16:32], 1.0, ALU.add, r=['modT'], w=['onep'])
            onep3 = onep[:].rearrange("p (k w) -> p k w", w=2)
            sh13 = modT[:, 0:16].rearrange("p (k w) -> p k w", w=2)
            ts('pool', modbc[:, 2048:3072], modbc[:, 2048:3072], 1.0, ALU.add, r=['modbc'], w=['modbc'])
            G1 = modbc[:, 0:1024]; SH2 = modbc[:, 1024:2048]; ONESC2 = modbc[:, 2048:3072]; G2 = modbc[:, 3072:4096]
            debug_out('dbg_modT', modT[:], ['modT'], 'sp')
            debug_out('dbg_modbc', modbc[:], ['modbc'], 'sp')
            checkpoint('p0')

            def s5_setup(Bm, Cm, magt, thr):
                with contextlib.ExitStack() as es0:
                    def t0(name, shape, dt=F32):
                        return es0.enter_context(nc.sbuf_tensor('s_' + name, list(shape), dt))
                    lr = t0('lr', [128, 32]); li = t0('li', [128, 32]); ls = t0('ls', [128, 32])
                    dma('sp', lr[:], D['lamre'], 'c_lr', w=['lr']); dma('sp', li[:], D['lamim'], 'c_li', w=['li'])
                    dma('sp', ls[:], D['lstep'], 'c_ls', w=['ls'])
                    bre = t0('bre', [128, 512]); bim = t0('bim', [128, 512]); cre = t0('cre', [128, 512]); cim = t0('cim', [128, 512])
                    dma('sp', bre[:], D['bre'], 'c_bre', w=['bre']); dma('sp', bim[:], D['bim'], 'c_bim', w=['bim'])
                    dma('sp', cre[:], D['cre'], 'c_cre', w=['cre']); dma('sp', cim[:], D['cim'], 'c_cim', w=['cim'])
                    dt_ = t0('dt_', [128, 32]); lrdt = t0('lrdt', [128, 32]); lidt = t0('lidt', [128, 32])
                    a1 = t0('a1', [128, 32]); a2 = t0('a2', [128, 32]); sinv = t0('sinv', [128, 32]); cosv = t0('cosv', [128, 32])
                    ar = t0('ar', [128, 32]); ai = t0('ai', [128, 32]); den = t0('den', [128, 32]); tq = t0('tq', [128, 32])
                    crr = t0('crr', [128, 32]); cii = t0('cii', [128, 32])
                    act(dt_[:], ls[:], AF.Exp, r=['ls'], w=['dt_'])
                    tt('dve', lrdt[:], lr[:], dt_[:], ALU.mult, r=['lr', 'dt_'], w=['lrdt'])
                    tt('dve', lidt[:], li[:], dt_[:], ALU.mult, r=['li', 'dt_'], w=['lidt'])
                    act(magt[:], lrdt[:], AF.Exp, r=['lrdt'], w=['magt'])
                    ts('dve', a1[:], lidt[:], 1.0 / TWO_PI, ALU.mult, r=['lidt'], w=['a1'])
                    ts('dve', a2[:], a1[:], MAGIC, ALU.add, r=['a1'], w=['a2'])
                    stt('dve', thr[:], a2[:], MAGIC, a1[:], ALU.subtract, ALU.subtract, r=['a2', 'a1'], w=['thr'])
                    act(a2[:], thr[:], AF.Abs, r=['thr'], w=['a2'])
                    act(sinv[:], thr[:], AF.Sin, r=['thr'], w=['sinv'], scale=-TWO_PI)
                    act(cosv[:], a2[:], AF.Sin, r=['a2', 'halfpi'], w=['cosv'], bias=halfpi[:, 0:1], scale=-TWO_PI)
                    ts('dve', thr[:], thr[:], -1.0, ALU.mult, r=['thr', 'sinv'], w=['thr'])
                    tt('dve', ar[:], magt[:], cosv[:], ALU.mult, r=['magt', 'cosv'], w=['ar'])
                    tt('dve', ai[:], magt[:], sinv[:], ALU.mult, r=['magt', 'sinv'], w=['ai'])
                    tt('dve', den[:], lr[:], lr[:], ALU.mult, r=['lr'], w=['den'])
                    tt('dve', tq[:], li[:], li[:], ALU.mult, r=['li'], w=['tq'])
                    tt('dve', den[:], den[:], tq[:], ALU.add, r=['den', 'tq'], w=['den'])
                    S.op('dve', lambda e: e.reciprocal(out=den[:], in_=den[:]), ['den'], ['den'])
                    ts('dve', ar[:], ar[:], -1.0, ALU.add, r=['ar'], w=['ar'])
                    tt('dve', crr[:], ar[:], lr[:], ALU.mult, r=['ar', 'lr'], w=['crr'])
                    tt('dve', tq[:], ai[:], li[:], ALU.mult, r=['ai', 'li'], w=['tq'])
                    tt('dve', crr[:], crr[:], tq[:], ALU.add, r=['crr', 'tq'], w=['crr'])
                    tt('dve', crr[:], crr[:], den[:], ALU.mult, r=['crr', 'den'], w=['crr'])
                    tt('dve', cii[:], ai[:], lr[:], ALU.mult, r=['ai', 'lr'], w=['cii'])
                    tt('dve', tq[:], ar[:], li[:], ALU.mult, r=['ar', 'li'], w=['tq'])
                    tt('dve', cii[:], cii[:], tq[:], ALU.subtract, r=['cii', 'tq'], w=['cii'])
                    tt('dve', cii[:], cii[:], den[:], ALU.mult, r=['cii', 'den'], w=['cii'])
                    bbr = t0('bbr', [128, 512]); bbi = t0('bbi', [128, 512]); tb = t0('tb', [128, 512])
                    crb = crr[:].unsqueeze(2).to_broadcast([128, 32, 16]); cib = cii[:].unsqueeze(2).to_broadcast([128, 32, 16])
                    v3 = lambda t: t[:].rearrange("p (a h) -> p a h", h=16)
                    tt('dve', v3(bbr), v3(bre), crb, ALU.mult, r=['bre', 'crr'], w=['bbr'])
                    tt('dve', v3(tb), v3(bim), cib, ALU.mult, r=['bim', 'cii'], w=['tb'])
                    tt('dve', bbr[:], bbr[:], tb[:], ALU.subtract, r=['bbr', 'tb'], w=['bbr'])
                    tt('dve', v3(bbi), v3(bim), crb, ALU.mult, r=['bim', 'crr'], w=['bbi'])
                    tt('dve', v3(tb), v3(bre), cib, ALU.mult, r=['bre', 'cii', 'bbr'], w=['tb'])
                    tt('dve', bbi[:], bbi[:], tb[:], ALU.add, r=['bbi', 'tb'], w=['bbi'])
                    Bst = t0('Bst', [128, 64 * 128])
                    mset('pool', Bst[:], 0.0, w=['Bst'])
                    for d in range(2):
                        for s in range(2):
                            for ri, src in ((0, bbr), (1, bbi)):
                                base = Bst[s * 64:(s + 1) * 64, ((d * 16) * 2 + ri) * 128 + s * 16:((d * 16) * 2 + ri) * 128 + s * 16 + 1]
                                dst = bass.AP(base.tensor, base.offset, [list(base.ap[0]), [1024, 4], [288, 4], [1, 16]])
                                sv = src[s * 64:(s + 1) * 64, d * 256:(d + 1) * 256].rearrange("p (c j h) -> p c j h", c=4, j=4)
                                cp('dve', dst, sv, r=['bbr', 'bbi', 'Bst'], w=['Bst'])
                    for q4 in range(16):
                        ps, pk = nps()
                        for i4 in range(4):
                            idx = q4 * 4 + i4
                            tr(ps[:, i4 * 128:(i4 + 1) * 128], Bst[:, idx * 128:(idx + 1) * 128], identf[:], r=['Bst', 'identf'], w=[pk])
                        cp('act', Bm[:, q4 * 4:(q4 + 1) * 4, :], ps[:].rearrange("p (a c) -> p a c", c=128), r=[pk], w=['Bm'])
                    Xc = t0('Xc', [128, 16 * 128])
                    Xv = Xc[:].rearrange("p (a r c) -> p a r c", r=2, c=128)
                    c3 = lambda t: t[:].rearrange("p (a q) -> p a q", q=64)
                    for s in range(2):
                        ts('dve', Xv[:, :, 0, s * 64:(s + 1) * 64], c3(cre), par[:, s:s + 1], ALU.mult, r=['cre', 'par', 'Xc'], w=['Xc'])
                        ts('dve', Xv[:, :, 1, s * 64:(s + 1) * 64], c3(cim), par[:, 2 + s:3 + s], ALU.mult, r=['cim', 'par', 'Xc'], w=['Xc'])
                    mset('pool', Cm[:], 0.0, w=['Cm'])
                    for q4 in range(4):
                        ps, pk = nps()
                        for i4 in range(4):
                            idx = q4 * 4 + i4
                            tr(ps[:, i4 * 128:(i4 + 1) * 128], Xc[:, idx * 128:(idx + 1) * 128], identf[:], r=['Xc', 'identf'], w=[pk])
                        for i4 in range(4):
                            idx = q4 * 4 + i4
                            dct, ri = idx // 2, idx % 2
                            d, ct = dct // 4, dct % 4
                            base = Cm[:, (d * 16 + ct * 4) * 2 + ri, 0:1]
                            dst = bass.AP(base.tensor, base.offset, [list(base.ap[0]), [2 * 128 + 32, 4], [1, 32]])
                            cp('dve', dst, ps[:, i4 * 128:(i4 + 1) * 128].rearrange("p (j c) -> p j c", c=32), r=[pk, 'Cm'], w=['Cm'])

                S.barrier()

            gates = sb('gates', [128, 16, NE])
            yacc = sb('yacc', [128, 4, NTOK])
            es13 = contextlib.ExitStack()
            es13.__enter__()
            OPEN.append(es13)
            sT_own = es13.enter_context(nc.sbuf_tensor('s_sT_own', [128, 4, NTOK], BF16))
            sT_ctx = es13.enter_context(nc.sbuf_tensor('s_sT_ctx', [128, 4, 256], BF16))
            es1 = contextlib.ExitStack()
            es1.__enter__()
            OPEN.append(es1)

            def t1(name, shape, dt=F32):
                return es1.enter_context(nc.sbuf_tensor('s_' + name, list(shape), dt))
            qT = t1('qT', [128, 4, NTOK], BF16)
            kT = t1('kT', [128, 2, 2432], BF16)
            vv = t1('vv', [128, 19, 128], BF16)
            es1b = contextlib.ExitStack()
            es1b.__enter__()
            OPEN.append(es1b)

            def t1b(name, shape, dt=F32):
                return es1b.enter_context(nc.sbuf_tensor('s_' + name, list(shape), dt))
            gbc = t1b('gbc', [128, DM]); bbc = t1b('bbc', [128, DM])
            dma('sp', gbc[:], pbc(D['ln_in_g']), 'c_gbc', w=['gbc'])
            dma('sp', bbc[:], pbc(D['ln_in_b']), 'c_bbc', w=['bbc'])
            ropeCb = [t1b('ropeC%d' % i, [128, 512]) for i in range(2)]
            ropeSb = [t1b('ropeS%d' % i, [128, 512]) for i in range(2)]
            ropecur = [None, None, None, None]
            win_v = D['w_in'].rearrange("(k p) c -> p k c", p=128)
            w_s = t1b('w_s', [128, 8, 512], BF16); w_q = t1b('w_q', [128, 8, 512], BF16)
            w_k = t1b('w_k', [128, 8, 256], BF16); w_v = t1b('w_v', [128, 8, 128], BF16)
            w_gs = yacc[:, 0:2, :].rearrange('p a b -> p (a b)').bitcast(BF16).rearrange('p (k c) -> p k c', c=1024)
            w_ga = yacc[:, 2:4, :].rearrange('p a b -> p (a b)').bitcast(BF16).rearrange('p (k c) -> p k c', c=1024)
            dma('pool', w_s[:], win_v[:, :, 0:512], 'c_ws', w=['w_s'])
            dma('pool', w_q[:], win_v[:, :, 512:1024], 'c_wq', w=['w_q'])
            for kvh in range(2):
                for dup in range(2):
                    dma('pool', w_k[:, :, kvh * 128 + dup * 64:kvh * 128 + dup * 64 + 64],
                        win_v[:, :, 1024 + kvh * 64:1024 + kvh * 64 + 64], 'c_wk%d%d' % (kvh, dup), w=['w_k%d%d' % (kvh, dup)])
            wk_keys = ['w_k00', 'w_k01', 'w_k10', 'w_k11']
            dma('pool', w_v[:], win_v[:, :, 1152:1280], 'c_wv', w=['w_v'])
            dma('pool', w_gs, win_v[:, :, 1280:2304], 'c_wgs', w=['w_gs'])
            dma('pool', w_ga, win_v[:, :, 2304:3328], 'c_wga', w=['w_ga'])
            xt = [t1b('xt%d' % i, [128, DM]) for i in range(3)]
            ht = [t1b('ht%d' % i, [128, DM]) for i in range(2)]
            stt_ = [t1b('st%d' % i, [128, 16]) for i in range(2)]
            uT = [t1b('uT%d' % i, [128, 8, 512], BF16) for i in range(2)]
            rA = [t1b('rA%d' % i, [128, 512]) for i in range(2)]
            rB = [t1b('rB%d' % i, [128, 512]) for i in range(2)]
            stg = [t1b('stg%d' % i, [128, 512], BF16) for i in range(4)]

            LNENG = _cfg('LNENG', 'dve')
            ROPENG = _cfg('ROPENG', 'pool')
            tilectr = [0]
            stgctr = [0]
            ropectr = [0]

            def ln_tile(src_ap, which, ug, ugk, col0, spill_row=None):
                i = tilectr[0]
                tilectr[0] += 1
                x = xt[i % 3]; xk = 'xt%d' % (i % 3)
                h = ht[i % 2]; hk = 'ht%d' % (i % 2)
                st = stt_[i % 2]; sk = 'st%d' % (i % 2)
                dma('sp', x[:], src_ap, xk, w=[xk])
                S.op('dve', lambda e: e.bn_stats(out=st[:, 0:6], in_=x[:, 0:512]), [xk], [sk])
                S.op('dve', lambda e: e.bn_stats(out=st[:, 6:12], in_=x[:, 512:1024]), [xk, sk], [sk])
                S.op('dve', lambda e: e.bn_aggr(out=st[:, 12:14], in_=st[:, 0:12]), [sk], [sk])
                rstd(st, sk)
                ts('dve', x[:], x[:], st[:, 12:13], ALU.subtract, r=[xk, sk], w=[xk], s2=st[:, 14:15], op1=ALU.mult)
                tt(LNENG, h[:], x[:], gbc[:], ALU.mult, r=[xk, 'gbc'], w=[hk])
                tt(LNENG, h[:], h[:], bbc[:], ALU.add, r=[hk, 'bbc'], w=[hk])
                if spill_row is not None:
                    t = spill_row // 128
                    dma('sp', D['h_scr'][spill_row:spill_row + 128, :], h[:], 'hs%d' % (t % 4), r=[hk], w=[('hscr', t)])
                for half in range(2):
                    ps, pk = nps()
                    for kk in range(4):
                        k = half * 4 + kk
                        tr(ps[:, kk * 128:(kk + 1) * 128], h[:, k * 128:(k + 1) * 128], identf[:], r=[hk, 'identf'], w=[pk])
                    for kk in range(4):
                        k = half * 4 + kk
                        act(uT[ug][:, k, col0:col0 + 128], ps[:, kk * 128:(kk + 1) * 128], AF.Identity,
                            r=[pk, 'onep', 'modT'], w=[ugk], bias=sh13[:, k, which:which + 1], scale=onep3[:, k, which:which + 1])

            def proj(ug, ugk, wt, wkeys, c0, n):
                ps, pk = nps()
                for k in range(8):
                    mm(ps[:, 0:n], wt[:, k, c0:c0 + 128], uT[ug][:, k, 0:n], k == 0, k == 7, r=[ugk] + list(wkeys), w=[pk])
                return ps, pk

            def rope_load(slot, rc0, n):
                dma('sp', ropeCb[slot][:, 0:n], D['ropeC'][:, rc0:rc0 + n], 'ropeC%d' % slot, w=['ropeC%d' % slot])
                dma('sp', ropeSb[slot][:, 0:n], D['ropeS'][:, rc0:rc0 + n], 'ropeS%d' % slot, w=['ropeS%d' % slot])
                ropecur[0] = ropeCb[slot]; ropecur[1] = ropeSb[slot]; ropecur[2] = 'ropeC%d' % slot; ropecur[3] = 'ropeS%d' % slot

            def rope(ps, pk, pc0, n, dst, dkey):
                i = ropectr[0] % 2
                ropectr[0] += 1
                ropeC, ropeS, rck, rsk = ropecur
                A = rA[i]; Ak = 'rA%d' % i; B = rB[i]; Bk = 'rB%d' % i
                tt('dve', A[:, 0:n], ps[:, pc0:pc0 + n], ropeC[:, 0:n], ALU.mult, r=[pk, rck], w=[Ak])
                for (o, s_) in ((0, 32), (32, 0), (64, 96), (96, 64)):
                    tt('dve', B[o:o + 32, 0:n], ps[s_:s_ + 32, pc0:pc0 + n], ropeS[s_:s_ + 32, 0:n], ALU.mult,
                       r=[pk, rsk], w=[Bk])
                tt(ROPENG, dst, A[:, 0:n], B[:, 0:n], ALU.add, r=[Ak, Bk], w=[dkey])

            def group(kind, gi):
                ug = (0 if kind == 'ctx' else 1 + gi + (4 if kind == 'oth' else 0)) % 2
                ugk = 'uT%d' % ug
                n = 256 if kind == 'ctx' else 512
                for t in range(n // 128):
                    if kind == 'ctx':
                        ln_tile(D['ctxb'][t * 128:(t + 1) * 128, :], 1, ug, ugk, t * 128)
                    elif kind == 'own':
                        row = gi * 512 + t * 128
                        ln_tile(D['x_own'][row:row + 128, :], 0, ug, ugk, t * 128, spill_row=row)
                    else:
                        row = gi * 512 + t * 128
                        ln_tile(D['x_oth'][row:row + 128, :], 0, ug, ugk, t * 128)
                for ct in range(4):
                    ps, pk = proj(ug, ugk, w_s, ['w_s'], ct * 128, n)
                    if kind == 'own':
                        cp('act', sT_own[:, ct, gi * 512:(gi + 1) * 512], ps[:, 0:n], r=[pk], w=[('sT_own', gi)])
                    elif kind == 'ctx':
                        cp('act', sT_ctx[:, ct, :], ps[:, 0:n], r=[pk], w=['sT_ctx'])
                    else:
                        si = stgctr[0] % 4
                        stgctr[0] += 1
                        cp('act', stg[si][:], ps[:, 0:n], r=[pk], w=['stg%d' % si])
                        dma('sp', D['soth_scr'][ct, :, gi * 512:(gi + 1) * 512], stg[si][:], 'stg%d' % si,
                            r=['stg%d' % si], w=[('soth', ct, gi)])
                halo = (kind == 'oth' and gi == 3)
                if kind == 'own':
                    rope_load(gi % 2, gi * 512, 512)
                elif halo:
                    rope_load(0, 2048, 128)
                if kind != 'oth' or halo:
                    for kvh in range(2):
                        ps, pk = proj(ug, ugk, w_k, wk_keys, kvh * 128, n)
                        if kind == 'ctx':
                            cp('act', kT[:, kvh, 2176:2432], ps[:, 0:256], r=[pk], w=[('kT', kvh, 'ctx')])
                        elif kind == 'own':
                            rope(ps, pk, 0, 512, kT[:, kvh, gi * 512:(gi + 1) * 512], ('kT', kvh, gi))
                        else:
                            rope(ps, pk, 384, 128, kT[:, kvh, 2048:2176], ('kT', kvh, 'halo'))
                    tl = range(n // 128) if not halo else [3]
                    for t in tl:
                        vt = {'ctx': 17 + t, 'own': gi * 4 + t, 'oth': 16}[kind]
                        ps, pk = nps()
                        for k in range(8):
                            mm(ps[:, 0:128], uT[ug][:, k, t * 128:(t + 1) * 128], w_v[:, k, :], k == 0, k == 7,
                               r=[ugk, 'w_v'], w=[pk])
                        cp('act', vv[:, vt, :], ps[:, 0:128], r=[pk], w=[('vv', vt)])
                if kind == 'own':
                    for qt in range(4):
                        ps, pk = proj(ug, ugk, w_q, ['w_q'], qt * 128, n)
                        rope(ps, pk, 0, 512, qT[:, qt, gi * 512:(gi + 1) * 512], ('qT', gi))
                    for (wt, wkey, scr, nm) in ((w_gs, 'w_gs', 'sgs_scr', 'sgs'), (w_ga, 'w_ga', 'sga_scr', 'sga')):
                        for ot in range(8):
                            ps, pk = proj(ug, ugk, wt, [wkey], ot * 128, n)
                            si = stgctr[0] % 4
                            stgctr[0] += 1
                            act(stg[si][:], ps[:, 0:n], AF.Sigmoid, r=[pk], w=['stg%d' % si])
                            dma('sp', D[scr][ot, :, gi * 512:(gi + 1) * 512], stg[si][:], 'stg%d' % si,
                                r=['stg%d' % si], w=[(nm, ot, gi)])

            group('ctx', 0)
            for gi in range(4):
                group('own', gi)
            for gi in range(4):
                group('oth', gi)
            es1b.__exit__(None, None, None)
            OPEN.remove(es1b)
            S.barrier()
            debug_out('dbg_sT', sT_own[:, :, :], [('sT_own', g) for g in range(4)])
            debug_out('dbg_qT', qT[:, :, :], [('qT', g) for g in range(4)])
            debug_out('dbg_kT', kT[:, :, :], [('kT', a, b) for a in range(2) for b in (0, 1, 2, 3, 'halo', 'ctx')])
            debug_out('dbg_vv', vv[:, :, :], [('vv', t) for t in range(19)])
            checkpoint('p1')

            with contextlib.ExitStack() as es2:
                pT = [es2.enter_context(nc.sbuf_tensor('s_pT%d' % i, [128, 512], BF16)) for i in range(3)]
                rec = [es2.enter_context(nc.sbuf_tensor('s_rec%d' % i, [64, 512], F32)) for i in range(2)]
                ost = [es2.enter_context(nc.sbuf_tensor('s_ost%d' % i, [128, 2, 128], BF16)) for i in range(2)]
                GORD = [0, 2, 1, 3]
                items = []
                for kvh in range(2):
                    for qt in range(16):
                        gi = qt // 4
                        tiles = []
                        if qt > 0:
                            tiles.append(((qt - 1) * 128, qt - 1, 0, ('kT', kvh, (qt - 1) // 4), ('vv', qt - 1)))
                        tiles.append((qt * 128, qt, None, ('kT', kvh, gi), ('vv', qt)))
                        if qt < 15:
                            tiles.append(((qt + 1) * 128, qt + 1, 1, ('kT', kvh, (qt + 1) // 4), ('vv', qt + 1)))
                        else:
                            tiles.append((2048, 16, 2, ('kT', kvh, 'halo'), ('vv', 16)))
                        tiles.append((2176, 17, None, ('kT', kvh, 'ctx'), ('vv', 17)))
                        tiles.append((2304, 18, None, ('kT', kvh, 'ctx'), ('vv', 18)))
                        for ti, tl in enumerate(tiles):
                            items.append((kvh, qt, ti, len(tiles), tl))

                def emit_scores(idx):
                    kvh, qt, ti, nt, (kc0, vt, mk, kkey, vkey) = items[idx]
                    gi = qt // 4
                    sb_ = 4 + (idx % 2) * 2
                    pssA, pskA = psb[sb_], 'ps%d' % sb_
                    pssB, pskB = psb[sb_ + 1], 'ps%d' % (sb_ + 1)
                    for s_ in range(4):
                        g = GORD[s_]
                        h = kvh * 4 + g
                        hh = h % 2
                        pss, psk = (pssA, pskA) if hh == 0 else (pssB, pskB)
                        c0_ = (s_ % 2) * 128
                        mm(pss[:, c0_:c0_ + 128], kT[hh * 64:(hh + 1) * 64, kvh, kc0:kc0 + 128],
                           qT[hh * 64:(hh + 1) * 64, h // 2, qt * 128:(qt + 1) * 128], True, True,
                           r=[kkey, ('qT', gi)], w=[psk])
                    p = pT[idx % 3]; pkey = 'pT%d' % (idx % 3)
                    act(p[:, 0:256], pssA[:, 0:256], AF.Exp, r=[pskA], w=[pkey], scale=0.125)
                    act(p[:, 256:512], pssB[:, 0:256], AF.Exp, r=[pskB, pkey], w=[pkey], scale=0.125)
                    if mk is not None:
                        mv = masks[:, mk * 128:(mk + 1) * 128].unsqueeze(1).to_broadcast([128, 4, 128])
                        tt('dve', p[:].rearrange("p (g q) -> p g q", g=4), p[:].rearrange("p (g q) -> p g q", g=4), mv,
                           ALU.mult, r=[pkey, 'masks'], w=[pkey])

                def emit_pv(idx):
                    kvh, qt, ti, nt, (kc0, vt, mk, kkey, vkey) = items[idx]
                    it = kvh * 16 + qt
                    ab_ = (it % 2) * 2
                    pso, pok = psb[ab_], 'ps%d' % ab_
                    psd, pdk = psb[ab_ + 1], 'ps%d' % (ab_ + 1)
                    p = pT[idx % 3]; pkey = 'pT%d' % (idx % 3)
                    mm(pso[0:64, :], vv[:, vt, kvh * 64:(kvh + 1) * 64], p[:], ti == 0, ti == nt - 1, r=[vkey, pkey], w=[pok])
                    mm(psd[0:64, :], ones_b[:, 0:64], p[:], ti == 0, False, r=['ones_b', pkey], w=[pdk])
                    if ti < nt - 1:
                        return
                    mm(psd[0:64, :], ones_b[0:1, 0:64], esink[0:1, kvh * 512:(kvh + 1) * 512], False, True,
                       r=['ones_b', 'esink'], w=[pdk])
                    rc = rec[it % 2]; rk = 'rec%d' % (it % 2)
                    S.op('dve', lambda e: e.reciprocal(out=rc[:], in_=psd[0:64, :]), [pdk], [rk])
                    osl = it % 2
                    for s_ in range(4):
                        g = GORD[s_]
                        hh = g % 2
                        tt('dve', ost[osl][hh * 64:(hh + 1) * 64, g // 2, :],
                           pso[0:64, s_ * 128:(s_ + 1) * 128], rc[:, s_ * 128:(s_ + 1) * 128], ALU.mult,
                           r=[pok, rk], w=['ost%d' % osl])
                    dma('sp', D['o_scr'][kvh * 2:kvh * 2 + 2, :, qt * 128:(qt + 1) * 128].rearrange("k p t -> p k t"),
                        ost[osl][:], 'ost%d' % osl, r=['ost%d' % osl], w=[('oscr', kvh, qt)])

                for idx in range(len(items)):
                    emit_scores(idx)
                    if idx > 0:
                        emit_pv(idx - 1)
                emit_pv(len(items) - 1)
            es1.__exit__(None, None, None)
            OPEN.remove(es1)
            S.barrier()
            debug_out('dbg_o', D['o_scr'], [('oscr', a, b) for a in range(2) for b in range(16)], 'sp')
            checkpoint('p2')

            with contextlib.ExitStack() as es3:
                def t3(name, shape, dt=F32):
                    return es3.enter_context(nc.sbuf_tensor('s_' + name, list(shape), dt))
                Bm = t3('Bm', [128, 64, 128], BF16)
                Cm = t3('Cm', [128, 64, 128], BF16)
                magt = t3('magt', [128, 32]); thr = t3('thr', [128, 32])
                s5_setup(Bm, Cm, magt, thr)
                nmU = ['XR', 'XI', 'M1', 'M2', 'TR', 'TI']
                ub = [{nm: t3('%s%d' % (nm, i), [128, 512]) for nm in nmU} for i in range(4)]
                tabC = [t3('tabC%d' % i, [128, 512]) for i in range(4)]
                tabS = [t3('tabS%d' % i, [128, 512]) for i in range(4)]
                sre = [[t3('sre%d_%d' % (i, jj), [128, 512], BF16) for jj in range(4)] for i in range(2)]
                sim = [[t3('sim%d_%d' % (i, jj), [128, 512], BF16) for jj in range(4)] for i in range(2)]
                soth = [t3('soth%d' % i, [128, 512], BF16) for i in range(2)]
                carry = t3('carry', [128, 64])
                ctmp = t3('ctmp', [128, 8])
                sctr = 0
                octr = 0

                def rev(ap2):
                    n = ap2.ap[-1][1]
                    return bass.AP(ap2.tensor, ap2.offset + (n - 1) * ap2.ap[-1][0], [list(ap2.ap[0]), [-ap2.ap[-1][0], n]])

                ENGJ = ['dve', 'dve', 'dve', 'dve']
                for d in range(2):
                    if d == 0:
                        segs = [('ctx', 0, False)] + [('own', c, False) for c in range(4)]
                    else:
                        segs = [('ctx', 0, True)] + [('oth', c, False) for c in range(4)] + [('own', c, True) for c in (3, 2, 1, 0)]
                    for ct in range(4):
                        for jj in range(4):
                            dj = d * 16 + ct * 4 + jj
                            E = ENGJ[jj]
                            u = ub[jj]
                            kM1 = 'M1_%d' % jj; kM2 = 'M2_%d' % jj; kC = 'tabC%d' % jj; kS = 'tabS%d' % jj
                            ts(E, u['M1'][:], iota1[:], thr[:, dj:dj + 1], ALU.mult, r=['iota1', 'thr'], w=[kM1])
                            ts(E, u['M2'][:], u['M1'][:], MAGIC, ALU.add, r=[kM1], w=[kM2])
                            ts(E, u['M2'][:], u['M2'][:], -MAGIC, ALU.add, r=[kM2], w=[kM2])
                            tt(E, tabS[jj][:], u['M2'][:], u['M1'][:], ALU.subtract, r=[kM2, kM1], w=[kS])
                            act(tabC[jj][:], tabS[jj][:], AF.Abs, r=[kS], w=[kC])
                            act(tabS[jj][:], tabS[jj][:], AF.Sin, r=[kS, kC], w=[kS], scale=-TWO_PI)
                            act(tabC[jj][:], tabC[jj][:], AF.Sin, r=[kC, 'halfpi'], w=[kC], bias=halfpi[:, 0:1], scale=-TWO_PI)
                        for si_, (kind, c, rv) in enumerate(segs):
                            n = 256 if kind == 'ctx' else 512
                            if kind == 'ctx':
                                src = sT_ctx[:, ct, :]; skey = 'sT_ctx'
                            elif kind == 'own':
                                src = sT_own[:, ct, c * 512:(c + 1) * 512]; skey = ('sT_own', c)
                            else:
                                so = soth[octr % 2]; skey = 'soth%d' % (octr % 2)
                                octr += 1
                                dma('sp', so[:], D['soth_scr'][ct, :, c * 512:(c + 1) * 512], skey, r=[('soth', ct, c)], w=[skey])
                                src = so[:]
                            if rv:
                                src = rev(src)
                            sslot = sctr % 2
                            if kind == 'own':
                                sctr += 1
                            K = lambda nm, jj: '%s_%d' % (nm, jj)
                            for jj in range(4):
                                dj = d * 16 + ct * 4 + jj
                                u = ub[jj]
                                psr, prk = nps()
                                psi, pik = nps()
                                mm(psr[:, 0:n], Bm[:, dj * 2, :], src, True, True, r=[skey, 'Bm'], w=[prk])
                                mm(psi[:, 0:n], Bm[:, dj * 2 + 1, :], src, True, True, r=[skey, 'Bm'], w=[pik])
                                cp('act', u['XR'][:, 0:n], psr[:, 0:n], r=[prk], w=[K('XR', jj)])
                                cp('act', u['XI'][:, 0:n], psi[:, 0:n], r=[pik], w=[K('XI', jj)])
                            for jj in range(4):
                                E = ENGJ[jj]; u = ub[jj]
                                tt(E, u['M1'][:, 0:n], u['XR'][:, 0:n], tabC[jj][:, 0:n], ALU.mult, r=[K('XR', jj), 'tabC%d' % jj], w=[K('M1', jj)])
                                tt(E, u['M2'][:, 0:n], u['XI'][:, 0:n], tabS[jj][:, 0:n], ALU.mult, r=[K('XI', jj), 'tabS%d' % jj], w=[K('M2', jj)])
                            for jj in range(4):
                                E = ENGJ[jj]; u = ub[jj]
                                tt(E, u['TR'][:, 0:n], u['M1'][:, 0:n], u['M2'][:, 0:n], ALU.add, r=[K('M1', jj), K('M2', jj)], w=[K('TR', jj)])
                            for jj in range(4):
                                E = ENGJ[jj]; u = ub[jj]
                                tt(E, u['M1'][:, 0:n], u['XI'][:, 0:n], tabC[jj][:, 0:n], ALU.mult, r=[K('XI', jj), 'tabC%d' % jj], w=[K('M1', jj)])
                                tt(E, u['M2'][:, 0:n], u['XR'][:, 0:n], tabS[jj][:, 0:n], ALU.mult, r=[K('XR', jj), 'tabS%d' % jj], w=[K('M2', jj)])
                            for jj in range(4):
                                E = ENGJ[jj]; u = ub[jj]
                                tt(E, u['TI'][:, 0:n], u['M1'][:, 0:n], u['M2'][:, 0:n], ALU.subtract, r=[K('M1', jj), K('M2', jj)], w=[K('TI', jj)])
                            for jj in range(4):
                                dj = d * 16 + ct * 4 + jj
                                u = ub[jj]
                                ck = ('carry', dj)
                                mg = magt[:, dj:dj + 1].to_broadcast([128, n])
                                if si_ == 0:
                                    scan('dve', u['XR'][:, 0:n], mg, u['TR'][:, 0:n], 0.0, r=['magt', K('TR', jj)], w=[K('XR', jj)])
                                    scan('dve', u['XI'][:, 0:n], mg, u['TI'][:, 0:n], 0.0, r=['magt', K('TI', jj)], w=[K('XI', jj)])
                                else:
                                    scan('dve', u['XR'][:, 0:n], mg, u['TR'][:, 0:n], carry[:, 2 * dj:2 * dj + 1], r=['magt', K('TR', jj), ck], w=[K('XR', jj)])
                                    scan('dve', u['XI'][:, 0:n], mg, u['TI'][:, 0:n], carry[:, 2 * dj + 1:2 * dj + 2], r=['magt', K('TI', jj), ck], w=[K('XI', jj)])
                            if si_ < len(segs) - 1:
                                for jj in range(4):
                                    dj = d * 16 + ct * 4 + jj
                                    u = ub[jj]
                                    ck = ('carry', dj)
                                    cn = tabC[jj][:, n - 1:n]; sn_ = tabS[jj][:, n - 1:n]
                                    rl = u['XR'][:, n - 1:n]; il = u['XI'][:, n - 1:n]
                                    tk_ = 'ctmp%d' % jj
                                    tt('dve', ctmp[:, 2 * jj:2 * jj + 1], il, sn_, ALU.mult, r=[K('XI', jj), 'tabS%d' % jj], w=[tk_])
                                    tt('dve', ctmp[:, 2 * jj + 1:2 * jj + 2], il, cn, ALU.mult, r=[K('XI', jj), 'tabC%d' % jj, tk_], w=[tk_])
                                    stt('dve', carry[:, 2 * dj:2 * dj + 1], rl, cn, ctmp[:, 2 * jj:2 * jj + 1], ALU.mult, ALU.subtract,
                                        r=[K('XR', jj), 'tabC%d' % jj, tk_, ck], w=[ck])
                                    stt('dve', carry[:, 2 * dj + 1:2 * dj + 2], rl, sn_, ctmp[:, 2 * jj + 1:2 * jj + 2], ALU.mult, ALU.add,
                                        r=[K('XR', jj), 'tabS%d' % jj, tk_, ck], w=[ck])
                            if kind == 'own':
                                for jj in range(4):
                                    E = ENGJ[jj]; u = ub[jj]
                                    tt(E, u['M1'][:, 0:n], u['XR'][:, 0:n], tabC[jj][:, 0:n], ALU.mult, r=[K('XR', jj), 'tabC%d' % jj], w=[K('M1', jj)])
                                    tt(E, u['M2'][:, 0:n], u['XI'][:, 0:n], tabS[jj][:, 0:n], ALU.mult, r=[K('XI', jj), 'tabS%d' % jj], w=[K('M2', jj)])
                                for jj in range(4):
                                    E = ENGJ[jj]; u = ub[jj]
                                    tt(E, sre[sslot][jj][:], u['M1'][:, 0:n], u['M2'][:, 0:n], ALU.subtract, r=[K('M1', jj), K('M2', jj)], w=[('sre', sslot, jj)])
                                for jj in range(4):
                                    E = ENGJ[jj]; u = ub[jj]
                                    tt(E, u['M1'][:, 0:n], u['XR'][:, 0:n], tabS[jj][:, 0:n], ALU.mult, r=[K('XR', jj), 'tabS%d' % jj], w=[K('M1', jj)])
                                    tt(E, u['M2'][:, 0:n], u['XI'][:, 0:n], tabC[jj][:, 0:n], ALU.mult, r=[K('XI', jj), 'tabC%d' % jj], w=[K('M2', jj)])
                                for jj in range(4):
                                    E = ENGJ[jj]; u = ub[jj]
                                    tt(E, sim[sslot][jj][:], u['M1'][:, 0:n], u['M2'][:, 0:n], ALU.add, r=[K('M1', jj), K('M2', jj)], w=[('sim', sslot, jj)])
                                psy, pyk = nps()
                                for jj in range(4):
                                    dj = d * 16 + ct * 4 + jj
                                    a_re = sre[sslot][jj][:]; a_im = sim[sslot][jj][:]
                                    if rv:
                                        a_re = rev(a_re); a_im = rev(a_im)
                                    mm(psy[:], Cm[:, dj * 2, :], a_re, jj == 0, False, r=['Cm', ('sre', sslot, jj)], w=[pyk])
                                    mm(psy[:], Cm[:, dj * 2 + 1, :], a_im, False, jj == 3, r=['Cm', ('sim', sslot, jj)], w=[pyk])
                                ysl = yacc[:, ct, c * 512:(c + 1) * 512]
                                if d == 0:
                                    stt('dve', ysl, sT_own[:, ct, c * 512:(c + 1) * 512], dcol[:, ct:ct + 1], psy[:], ALU.mult, ALU.add,
                                        r=[pyk, ('sT_own', c), 'dcol'], w=[('yacc', ct, c)])
                                else:
                                    tt('dve', ysl, ysl, psy[:], ALU.add, r=[pyk, ('yacc', ct, c)], w=[('yacc', ct, c)])
            es13.__exit__(None, None, None)
            OPEN.remove(es13)
            S.barrier()
            debug_out('dbg_y', yacc[:, :, :], [('yacc', a, b) for a in range(4) for b in range(4)], 'sp')
            checkpoint('p3')

            GELUENG = _cfg('GELUENG', 'dve'); MTENG = _cfg('MTENG', 'dve'); LN1ENG = _cfg('LN1ENG', 'dve')
            HMENG = _cfg('HMENG', 'dve'); HBENG = _cfg('HBENG', 'dve')
            with contextlib.ExitStack() as es4:
                def t4(name, shape, dt=F32):
                    return es4.enter_context(nc.sbuf_tensor('s_' + name, list(shape), dt))
                wglu = t4('wglu', [128, 4, 512], BF16); wso = t4('wso', [128, 4, DM], BF16); wao = t4('wao', [128, 4, DM], BF16)
                wo = t4('wo', [128, 8, DM], BF16); wr = t4('wr', [128, 8, NE]); brt = t4('brt', [1, NE])
                dma('pool', wglu[:], D['w_glu'].rearrange("(k p) c -> p k c", p=128), 'c_wglu', w=['wglu'])
                dma('pool', wso[:], D['w_ssm_out'].rearrange("(k p) c -> p k c", p=128), 'c_wso', w=['wso'])
                dma('pool', wao[:], D['w_att_out'].rearrange("(k p) c -> p k c", p=128), 'c_wao', w=['wao'])
                dma('pool', wo[:], D['w_o'].rearrange("(k p) c -> p k c", p=128), 'c_wo', w=['wo'])
                dma('sp', wr[:], D['w_router'].rearrange("(k p) c -> p k c", p=128), 'c_wr', w=['wr'])
                dma('sp', brt[:], D['b_router'], 'c_br', w=['brt'])
                l1g = t4('l1g', [128, DM]); l1b = t4('l1b', [128, DM])
                dma('sp', l1g[:], pbc(D['ln1_g']), 'c_l1g', w=['l1g'])
                dma('sp', l1b[:], pbc(D['ln1_b']), 'c_l1b', w=['l1b'])
                sgs = [t4('sgs%d' % i, [128, 8, 512], BF16) for i in range(1)]
                sga = [t4('sga%d' % i, [128, 8, 512], BF16) for i in range(1)]
                oTc = [t4('oTc%d' % i, [128, 4, 512], BF16) for i in range(2)]
                g_t = [t4('g_t%d' % i, [128, 512]) for i in range(2)]
                g_w = [t4('g_w%d' % i, [128, 512]) for i in range(2)]
                zT = t4('zT', [128, 4, 512], BF16); z2T = t4('z2T', [128, 4, 512], BF16)
                sgl = [t4('sgl%d' % i, [128, 512], BF16) for i in range(2)]
                m1 = [t4('m1_%d' % i, [128, 512]) for i in range(2)]
                m2 = [t4('m2_%d' % i, [128, 512]) for i in range(2)]
                mT = t4('mT', [128, 8, 512], BF16)
                hr = [t4('hr%d' % i, [128, DM]) for i in range(2)]
                tk = [t4('tk%d' % i, [128, DM]) for i in range(2)]
                hmf = [t4('hmf%d' % i, [128, 8, 128]) for i in range(2)]
                hmb = [t4('hmb%d' % i, [128, 8, 128], BF16) for i in range(2)]
                st4 = [t4('st4_%d' % i, [128, 16]) for i in range(2)]
                lg = [t4('lg%d' % i, [128, NE]) for i in range(2)]
                mx = [t4('mx%d' % i, [128, 16]) for i in range(2)]
                msk = [t4('msk%d' % i, [128, NE]) for i in range(2)]
                P4STOP = _cfg('P4STOP', '')
                for c in range(4):
                    if P4STOP and c > 0:
                        break
                    cs_ = slice(c * 512, (c + 1) * 512)
                    b2 = 0
                    oc = oTc[c % 2]; ock = 'oTc%d' % (c % 2)
                    for kvh in range(2):
                        dma('sp', oc[:, kvh * 2:kvh * 2 + 2, :], D['o_scr'][kvh * 2:kvh * 2 + 2, :, cs_].rearrange("k p t -> p k t"),
                            ock, r=[('oscr', kvh, qt) for qt in range(c * 4, c * 4 + 4)], w=[ock])
                    for ot in range(8):
                        dma('sp', sgs[b2][:, ot, :], D['sgs_scr'][ot, :, cs_], 'sgsl%d' % b2, r=[('sgs', ot, c)], w=['sgs%d' % b2])
                        dma('sp', sga[b2][:, ot, :], D['sga_scr'][ot, :, cs_], 'sgal%d' % b2, r=[('sga', ot, c)], w=['sga%d' % b2])
                    if P4STOP == 'loads':
                        break
                    for ct in range(4):
                        i = ct % 2
                        y = yacc[:, ct, cs_]; yk = ('yacc', ct, c)
                        tt(GELUENG, g_t[i][:], y, y, ALU.mult, r=[yk], w=['g_t%d' % i])
                        ts(GELUENG, g_t[i][:], g_t[i][:], 0.044715, ALU.mult, r=['g_t%d' % i], w=['g_t%d' % i], s2=1.0, op1=ALU.add)
                        tt(GELUENG, g_w[i][:], g_t[i][:], y, ALU.mult, r=['g_t%d' % i, yk], w=['g_w%d' % i])
                        act(g_w[i][:], g_w[i][:], AF.Sigmoid, r=['g_w%d' % i], w=['g_w%d' % i], scale=1.5957691216057308)
                        tt(GELUENG, zT[:, ct, :], g_w[i][:], y, ALU.mult, r=['g_w%d' % i, yk], w=[('zT', ct)])
                    if P4STOP == 'gelu':
                        break
                    for ct in range(4):
                        ps, pk = nps()
                        for k in range(4):
                            mm(ps[:], wglu[:, k, ct * 128:(ct + 1) * 128], zT[:, k, :], k == 0, k == 3, r=['wglu', ('zT', k)], w=[pk])
                        i = ct % 2
                        act(sgl[i][:], ps[:], AF.Sigmoid, r=[pk, 'bgluT'], w=['sgl%d' % i], bias=bgluT[:, ct:ct + 1])
                        tt('dve', z2T[:, ct, :], zT[:, ct, :], sgl[i][:], ALU.mult, r=[('zT', ct), 'sgl%d' % i], w=[('z2T', ct)])
                    if P4STOP == 'glu':
                        break
                    for ot in range(8):
                        i = ot % 2
                        psa, pak = nps()
                        for k in range(4):
                            mm(psa[:], wso[:, k, ot * 128:(ot + 1) * 128], z2T[:, k, :], k == 0, k == 3, r=['wso', ('z2T', k)], w=[pak])
                        psb_, pbk = nps()
                        for k in range(4):
                            mm(psb_[:], wao[:, k, ot * 128:(ot + 1) * 128], oc[:, k, :], k == 0, k == 3, r=['wao', ock], w=[pbk])
                        tt('dve', m1[i][:], psa[:], sgs[b2][:, ot, :], ALU.mult, r=[pak, 'sgs%d' % b2], w=['m1_%d' % i])
                        tt('dve', m2[i][:], psb_[:], sga[b2][:, ot, :], ALU.mult, r=[pbk, 'sga%d' % b2], w=['m2_%d' % i])
                        tt(MTENG, mT[:, ot, :], m1[i][:], m2[i][:], ALU.add, r=['m1_%d' % i, 'm2_%d' % i], w=[('mT', ot)])
                    if P4STOP == 'branch':
                        break
                    for t in range(4):
                        tg = c * 4 + t
                        i = tg % 2
                        row = tg * 128
                        h = hr[i]; hk = 'hr%d' % i
                        x = tk[i]; xk = 'tk%d' % i
                        st = st4[i]; sk = 'st4_%d' % i
                        dma('sp', h[:], D['h_scr'][row:row + 128, :], hk, r=[('hscr', tg)], w=[hk])
                        for half in range(2):
                            ps, pk = nps()
                            for k in range(8):
                                mm(ps[:], mT[:, k, t * 128:(t + 1) * 128], wo[:, k, half * 512:(half + 1) * 512], k == 0, k == 7,
                                   r=[('mT', k), 'wo'], w=[pk])
                            tt('dve', x[:, half * 512:(half + 1) * 512], ps[:], G1[:, half * 512:(half + 1) * 512], ALU.mult,
                               r=[pk, 'modbc'], w=[xk])
                        stt('dve', x[:], h[:], ALPHA, x[:], ALU.mult, ALU.add, r=[hk, xk], w=[xk])
                        if P4STOP == 'mix':
                            continue
                        S.op('dve', lambda e, st=st, x=x: e.bn_stats(out=st[:, 0:6], in_=x[:, 0:512]), [xk], [sk])
                        S.op('dve', lambda e, st=st, x=x: e.bn_stats(out=st[:, 6:12], in_=x[:, 512:1024]), [xk, sk], [sk])
                        S.op('dve', lambda e, st=st: e.bn_aggr(out=st[:, 12:14], in_=st[:, 0:12]), [sk], [sk])
                        rstd(st, sk)
                        ts('dve', x[:], x[:], st[:, 12:13], ALU.subtract, r=[xk, sk], w=[xk], s2=st[:, 14:15], op1=ALU.mult)
                        tt(LN1ENG, x[:], x[:], l1g[:], ALU.mult, r=[xk, 'l1g'], w=[xk])
                        tt(LN1ENG, h[:], x[:], l1b[:], ALU.add, r=[xk, 'l1b', hk], w=[hk])
                        if P4STOP == 'ln':
                            continue
                        dma('sp', D['h1_scr'][row:row + 128, :], h[:], 'h1s%d' % (tg % 4), r=[hk], w=[('h1scr', tg)])
                        tt(HMENG, x[:], h[:], ONESC2, ALU.mult, r=[hk, 'modbc'], w=[xk])
                        tt(HMENG, x[:], x[:], SH2, ALU.add, r=[xk, 'modbc'], w=[xk])
                        if P4STOP == 'spill':
                            continue
                        hf_ = hmf[i]; hfk = 'hmf%d' % i
                        hb_ = hmb[i]; hbk = 'hmb%d' % i
                        for half in range(2):
                            ps, pk = nps()
                            for kk in range(4):
                                k = half * 4 + kk
                                tr(ps[:, kk * 128:(kk + 1) * 128], x[:, k * 128:(k + 1) * 128], identf[:], r=[xk, 'identf'], w=[pk])
                            cp('act', hf_[:, half * 4:(half + 1) * 4, :], ps[:].rearrange("p (a c) -> p a c", c=128), r=[pk], w=[hfk])
                        cp(HBENG, hb_[:], hf_[:], r=[hfk], w=[hbk])
                        for k in range(8):
                            dma('sp', D['hmT_scr'][k, :, row:row + 128], hb_[:, k, :], 'hmts%d' % i, r=[hbk], w=[('hmT', tg, k)])
                        if 'router' in _cfg('P4SKIP', ''):
                            mset('dve', gates[:, tg, :], 0.25, w=[('gates', tg)])
                            continue
                        ps, pk = nps()
                        for k in range(8):
                            mm(ps[:, 0:NE], hf_[:, k, :], wr[:, k, :], k == 0, False, r=[hfk, 'wr'], w=[pk])
                        mm(ps[:, 0:NE], ones_f[0:1, :], brt[0:1, :], False, True, r=['ones_f', 'brt'], w=[pk])
                        L = lg[i]; lk = 'lg%d' % i
                        M = mx[i]; mk_ = 'mx%d' % i
                        K_ = msk[i]; kk_ = 'msk%d' % i
                        cp('act', L[:], ps[:, 0:NE], r=[pk], w=[lk])
                        S.op('dve', lambda e, M=M, L=L: e.max(out=M[:, 0:8], in_=L[:]), [lk], [mk_])
                        ts('dve', K_[:], L[:], M[:, 3:4], ALU.is_ge, r=[lk, mk_], w=[kk_])
                        ts('dve', M[:, 8:9], M[:, 0:1], -1.0, ALU.mult, r=[mk_], w=[mk_])
                        act(L[:], L[:], AF.Exp, r=[lk, mk_], w=[lk], bias=M[:, 8:9])
                        tt('dve', L[:], L[:], K_[:], ALU.mult, r=[lk, kk_], w=[lk])
                        S.op('dve', lambda e, M=M, L=L: e.reduce_sum(out=M[:, 9:10], in_=L[:], axis=mybir.AxisListType.X), [lk, mk_], [mk_])
                        S.op('dve', lambda e, M=M: e.reciprocal(out=M[:, 10:11], in_=M[:, 9:10]), [mk_], [mk_])
                        ts('dve', gates[:, tg, :], L[:], M[:, 10:11], ALU.mult, r=[lk, mk_], w=[('gates', tg)])

            S.barrier()
            debug_out('dbg_gates', gates[:, :, :], [('gates', t) for t in range(16)], 'sp')
            debug_out('dbg_h1', D['h1_scr'], [('h1scr', t) for t in range(16)], 'sp')
            checkpoint('p4')
            finals = []
            with contextlib.ExitStack() as es5:
                def t5(name, shape, dt=F32):
                    return es5.enter_context(nc.sbuf_tensor('s_' + name, list(shape), dt))
                bguT = t5('bguT', [128, NE * 16]); bgu1 = t5('bgu1', [128, NE * 16])
                dma('sp', bguT[:], D['b_guT'], 'c_bgu', w=['bguT'])
                ts('pool', bgu1[:], bguT[:], 1.0, ALU.add, r=['bguT'], w=['bgu1'])
                l2g = t5('l2g', [128, DM]); l2b = t5('l2b', [128, DM])
                dma('sp', l2g[:], pbc(D['ln2_g']), 'c_l2g', w=['l2g'])
                dma('sp', l2b[:], pbc(D['ln2_b']), 'c_l2b', w=['l2b'])
                acc = yacc[:, :, :].rearrange('p a (b c) -> p (a b) c', c=1024)
                hmT = t5('hmT', [128, 8, 1024], BF16)
                NR = 8
                ring = [t5('ring%d' % i, [128, 8, 512], BF16) for i in range(NR)]
                bdall = t5('bdall', [NE, DM])
                dma('sp', bdall[:], D['b_down'], 'c_bdall', w=['bdall'])
                gTt = [t5('gTt%d' % i, [NE, 128]) for i in range(2)]
                actT = t5('actT', [128, 8, 1024], BF16)
                NGS = 4
                Gt = [t5('Gt%d' % i, [128, 512]) for i in range(NGS)]
                Lt = [t5('Lt%d' % i, [128, 512]) for i in range(NGS)]
                Sg = [t5('Sg%d' % i, [128, 512]) for i in range(NGS)]
                pend = []
                h1t = [t5('h1t%d' % i, [128, DM]) for i in range(2)]
                st6 = [t5('st6_%d' % i, [128, 16]) for i in range(2)]
                rctr = 0
                ectr = 0
                bctr = 0
                for half in range(2):
                    for k in range(8):
                        dma('sp', hmT[:, k, :], D['hmT_scr'][k, :, half * 1024:(half + 1) * 1024], 'hmTl',
                            r=[('hmT', tg, k) for tg in range(half * 8, half * 8 + 8)], w=['hmT'])
                    for t in range(8):
                        tg = half * 8 + t
                        gT = gTt[t % 2]; gTk = 'gTt%d' % (t % 2)
                        ps, pk = nps()
                        tr(ps[0:NE, 0:128], gates[:, tg, :], identf[:], r=[('gates', tg), 'identf'], w=[pk])
                        cp('act', gT[:], ps[0:NE, 0:128], r=[pk], w=[gTk])
                        for dh in range(2):
                            ps2, pk2 = nps()
                            mm(ps2[:], gT[:], bdall[:, dh * 512:(dh + 1) * 512], True, True, r=[gTk, 'bdall'], w=[pk2])
                            cp('act', acc[:, t, dh * 512:(dh + 1) * 512], ps2[:], r=[pk2], w=[('acc', t, dh)])
                    NODMA = bool(_cfg('MOE_NODMA'))
                    for e in range(NE):
                        if NODMA and (e > 0 or half > 0):
                            units = list(range(6))
                        wgu_v = D['w_gate_up'][e].rearrange("(k p) c -> p k c", p=128)
                        wd_v = D['w_down'][e].rearrange("(k p) c -> p k c", p=128)
                        units = [] if not (NODMA and (e > 0 or half > 0)) else units
                        for c in range(4):
                            if NODMA and (e > 0 or half > 0):
                                break
                            ri_ = rctr % NR
                            rctr += 1
                            dma('pool', ring[ri_][:, :, 0:256], wgu_v[:, :, c * 256:(c + 1) * 256], 'ringa%d' % ri_, w=[('ring', ri_, 0)])
                            dma('pool', ring[ri_][:, :, 256:512], wgu_v[:, :, 1024 + c * 256:1024 + (c + 1) * 256], 'ringb%d' % ri_, w=[('ring', ri_, 1)])
                            units.append(ri_)
                        for dh in range(2):
                            if NODMA and (e > 0 or half > 0):
                                break
                            ri_ = rctr % NR
                            rctr += 1
                            dma('pool', ring[ri_][:, :, 0:256], wd_v[:, :, dh * 512:dh * 512 + 256], 'ringa%d' % ri_, w=[('ring', ri_, 0)])
                            dma('pool', ring[ri_][:, :, 256:512], wd_v[:, :, dh * 512 + 256:(dh + 1) * 512], 'ringb%d' % ri_, w=[('ring', ri_, 1)])
                            units.append(ri_)
                        for c in range(4):
                            ru = units[c]
                            for sub in range(2):
                                fi = 2 * c + sub
                                for tch in range(2):
                                    tok = slice(tch * 512, (tch + 1) * 512)
                                    psg, pgk = nps()
                                    psl, plk = nps()
                                    for k in range(8):
                                        mm(psg[:], ring[ru][:, k, sub * 128:(sub + 1) * 128], hmT[:, k, tok], k == 0, k == 7,
                                           r=[('ring', ru, 0), 'hmT'], w=[pgk])
                                    for k in range(8):
                                        mm(psl[:], ring[ru][:, k, 256 + sub * 128:256 + (sub + 1) * 128], hmT[:, k, tok], k == 0, k == 7,
                                           r=[('ring', ru, 1), 'hmT'], w=[plk])
                                    i = ectr % NGS
                                    ectr += 1
                                    G = Gt[i]; gk = 'Gt%d' % i
                                    L = Lt[i]; lk = 'Lt%d' % i
                                    Sg_ = Sg[i]; sgk = 'Sg%d' % i
                                    ts('dve', G[:], psg[:], bguT[:, e * 16 + fi:e * 16 + fi + 1], ALU.add, r=[pgk, 'bguT'], w=[gk], s2=7.0, op1=ALU.min)
                                    if not _cfg('MOE_SKIPL'):
                                        ts('dve', L[:], psl[:], bgu1[:, e * 16 + 8 + fi:e * 16 + 8 + fi + 1], ALU.add, r=[plk, 'bgu1'], w=[lk], s2=8.0, op1=ALU.min)
                                    act(Sg_[:], G[:], AF.Sigmoid, r=[gk], w=[sgk], scale=1.702)
                                    tt(_cfg('MOEGENG', 'pool'), G[:], G[:], Sg_[:], ALU.mult, r=[gk, sgk], w=[gk])
                                    pend.append((actT[:, fi, tok], L[:], G[:], lk, gk, ('actT', fi, tch)))
                                    if len(pend) > 2:
                                        o_, l_, g_, lk_, gk_, ak_ = pend.pop(0)
                                        stt('dve', o_, l_, -6.0, g_, ALU.max, ALU.mult, r=[lk_, gk_], w=[ak_])
                        while pend:
                            o_, l_, g_, lk_, gk_, ak_ = pend.pop(0)
                            stt('dve', o_, l_, -6.0, g_, ALU.max, ALU.mult, r=[lk_, gk_], w=[ak_])
                        for t in range(8):
                            tg = half * 8 + t
                            tch = t // 4
                            for dh in range(2):
                                ru = units[4 + dh]
                                ps, pk = nps()
                                for k in range(8):
                                    mm(ps[:], actT[:, k, t * 128:(t + 1) * 128], ring[ru][:, k, :], k == 0, k == 7,
                                       r=[('actT', k, tch), ('ring', ru, 0), ('ring', ru, 1)], w=[pk])
                                asl = acc[:, t, dh * 512:(dh + 1) * 512]
                                ak = ('acc', t, dh)
                                stt('dve', asl, ps[:], gates[:, tg, e:e + 1], asl, ALU.mult, ALU.add, r=[pk, ('gates', tg), ak], w=[ak])
                    for t in range(8):
                        tg = half * 8 + t
                        i = tg % 2
                        row = tg * 128
                        h = h1t[i]; hk = 'h1t%d' % i
                        st = st6[i]; sk = 'st6_%d' % i
                        x = acc[:, t, :]
                        xk0 = ('acc', t, 0); xk1 = ('acc', t, 1)
                        dma('sp', h[:], D['h1_scr'][row:row + 128, :], hk, r=[('h1scr', tg)], w=[hk])
                        tt(_cfg('LN2ENG', 'dve'), x, x, G2, ALU.mult, r=[xk0, xk1, 'modbc'], w=[xk0, xk1])
                        stt('dve', x, h[:], ALPHA, x, ALU.mult, ALU.add, r=[hk, xk0, xk1], w=[xk0, xk1])
                        S.op('dve', lambda e, st=st, x=x: e.bn_stats(out=st[:, 0:6], in_=x[:, 0:512]), [xk0, xk1], [sk])
                        S.op('dve', lambda e, st=st, x=x: e.bn_stats(out=st[:, 6:12], in_=x[:, 512:1024]), [xk0, xk1, sk], [sk])
                        S.op('dve', lambda e, st=st: e.bn_aggr(out=st[:, 12:14], in_=st[:, 0:12]), [sk], [sk])
                        rstd(st, sk)
                        ts('dve', x, x, st[:, 12:13], ALU.subtract, r=[xk0, xk1, sk], w=[xk0, xk1], s2=st[:, 14:15], op1=ALU.mult)
                        tt('dve', x, x, l2g[:], ALU.mult, r=[xk0, xk1, 'l2g'], w=[xk0, xk1])
                        tt(_cfg('LN2ENG', 'dve'), x, x, l2b[:], ALU.add, r=[xk0, xk1, 'l2b'], w=[xk0, xk1])
                        ev = dma('sp', D['out'][row:row + 128, :], x, 'outs%d' % i, r=[xk0, xk1])
                        finals.append(ev)
            return finals

        try:
            finals_ = body()
        except _Stop:
            finals_ = []
            for st_ in reversed(OPEN):
                st_.__exit__(None, None, None)
        finish(finals_)
    return nc


def _rope_tables(local_real):
    pos = np.asarray(local_real, dtype=np.int64)
    row = (pos // 64).astype(np.float64)
    col = (pos % 64).astype(np.float64)
    inv = 10000.0 ** (-np.arange(16, dtype=np.float64) / 16.0)
    ang = np.concatenate([row[None, :] * inv[:, None], col[None, :] * inv[:, None]], axis=0)
    c = np.cos(ang); s = np.sin(ang)
    C = np.concatenate([c, c, c, c], axis=0)
    Sg = np.concatenate([s, -s, s, -s], axis=0)
    return C.astype(np.float32), Sg.astype(np.float32)


def _make_inputs(inp, r):
    b, hf = r // 2, r % 2
    f = np.float32
    x = inp['x'][b]
    L = np.arange(4096) if hf == 0 else np.arange(4095, -1, -1)
    own = L[:2048]
    oth = L[2048:][::-1]
    m = {}
    m['x_own'] = np.ascontiguousarray(x[own])
    m['x_oth'] = np.ascontiguousarray(x[oth])
    cx = inp['ctx'][b]
    m['ctxb'] = np.ascontiguousarray(cx if hf == 0 else cx[::-1])
    cv = np.zeros((128, 8, 2), f)
    cv[:, :, 0] = inp['c'][b].reshape(8, 128).T
    cv[:, :, 1] = inp['c_ctx'].reshape(8, 128).T
    m['cvec'] = cv.reshape(128, 16)
    m['ln_in_g'] = inp['ln_in_g'].reshape(1, -1); m['ln_in_b'] = inp['ln_in_b'].reshape(1, -1)
    m['w_mod'] = inp['w_mod'][0]
    m['b_modT'] = np.ascontiguousarray(inp['b_mod'][0].reshape(48, 128).T)
    m['b_mod_row'] = inp['b_mod'][0].reshape(1, -1)
    m['w_in'] = inp['w_in'][0]
    dsel = [0, 1] if hf == 0 else [1, 0]

    def sp_layout(a):
        a = a[dsel].reshape(2, 16, 2, 64)
        return np.ascontiguousarray(a.transpose(2, 3, 0, 1).reshape(128, 32))
    m['lamre'] = sp_layout(inp['ssm_lam_re'][0]); m['lamim'] = sp_layout(inp['ssm_lam_im'][0])
    m['lstep'] = sp_layout(np.broadcast_to(inp['ssm_log_step'][0][:, :, None], (2, 32, 64)))

    def b_layout(a):
        a = a[dsel].reshape(2, 16, 2, 64, 16)
        return np.ascontiguousarray(a.transpose(2, 3, 0, 1, 4).reshape(128, 512))
    m['bre'] = b_layout(inp['ssm_b_re'][0]); m['bim'] = b_layout(inp['ssm_b_im'][0])

    def c_layout(a):
        a = a[dsel].reshape(2, 4, 8, 16, 64)
        return np.ascontiguousarray(a.transpose(2, 3, 0, 1, 4).reshape(128, 512))
    m['cre'] = c_layout(inp['ssm_c_re'][0]); m['cim'] = c_layout(inp['ssm_c_im'][0])
    m['dcol'] = np.ascontiguousarray(inp['ssm_d'][0].reshape(4, 128).T)
    gl = np.arange(128) // 16
    par = np.zeros((128, 4), f)
    par[:, 0] = (gl % 2 == 0); par[:, 1] = (gl % 2 == 1); par[:, 2] = -par[:, 0]; par[:, 3] = -par[:, 1]
    m['par'] = par
    m['w_glu'] = inp['w_glu'][0]
    m['b_gluT'] = np.ascontiguousarray(inp['b_glu'][0].reshape(4, 128).T)
    m['sinkrow'] = np.ascontiguousarray(np.repeat(inp['attn_sink'][0][[0, 2, 1, 3, 4, 6, 5, 7]], 128).reshape(1, 1024))
    m['w_ssm_out'] = inp['w_ssm_out'][0]; m['w_att_out'] = inp['w_att_out'][0]; m['w_o'] = inp['w_o'][0]
    m['ln1_g'] = inp['ln1_g'][0].reshape(1, -1); m['ln1_b'] = inp['ln1_b'][0].reshape(1, -1)
    m['w_router'] = inp['w_router'][0]; m['b_router'] = inp['b_router'][0].reshape(1, -1)
    m['w_gate_up'] = inp['w_gate_up'][0]
    m['b_guT'] = np.ascontiguousarray(inp['b_gate_up'][0].reshape(32, 16, 128).transpose(2, 0, 1).reshape(128, 512))
    m['w_down'] = inp['w_down'][0]; m['b_down'] = inp['b_down'][0]
    m['ln2_g'] = inp['ln2_g'][0].reshape(1, -1); m['ln2_b'] = inp['ln2_b'][0].reshape(1, -1)
    m['ident'] = np.eye(128, dtype=f)
    halo = oth[1920:2048]
    C, Sg = _rope_tables(np.concatenate([own, halo]))
    m['ropeC'] = C; m['ropeS'] = Sg
    ki = np.arange(128)[:, None]; qi = np.arange(128)[None, :]
    mk = np.zeros((128, 3, 128), f)
    mk[:, 0] = (qi <= ki); mk[:, 1] = (ki <= qi); mk[:, 2] = (ki + qi >= 127)
    m['masks'] = mk.reshape(128, 384)
    m['iota1'] = np.ascontiguousarray(np.broadcast_to(np.arange(1, 513, dtype=f)[None, :], (128, 512)))
    return {k: np.ascontiguousarray(v, dtype=f) for k, v in m.items()}


_NC_CACHE = {}


def kernel(**inputs):
    inp = {k: np.asarray(v) for k, v in inputs.items()}
    if 'nc' not in _NC_CACHE:
        _NC_CACHE['nc'] = build_nc()
    nc = _NC_CACHE['nc']
    in_maps = [_make_inputs(inp, r) for r in range(8)]
    res = run_bass_kernel_spmd(nc, in_maps, core_ids=list(range(8)))
    out = np.zeros((4, 4096, 1024), np.float32)
    for r in range(8):
        b, hf = r // 2, r % 2
        o = np.asarray(res.results[r]['out'])
        if hf == 0:
            out[b, :2048] = o
        else:
            out[b, 2048:] = o[::-1]
    return out
```

```python
import math
import contextlib
import numpy as np
import concourse.bass as bass
import concourse.mybir as mybir
from concourse.bass_utils import run_bass_kernel_spmd

F32 = mybir.dt.float32
BF16 = mybir.dt.bfloat16
ALU = mybir.AluOpType
AF = mybir.ActivationFunctionType

NTOK = 2048
DM = 1024
NE = 32
TWO_PI = 2.0 * math.pi
MAGIC = 12582912.0
ALPHA = 2.0 ** 0.25
LN_EPS = 1e-5
_CFG = {}


def _cfg(name, default=None):
    return _CFG.get(name, default)


class Sched:
    ENG = ('pe', 'dve', 'act', 'pool', 'sp')

    def __init__(self, nc):
        self.nc = nc
        self.ops = {e: [] for e in self.ENG}
        self.state = {}
        self.dma_cnt = {}
        self.nsig = {}
        self.barrier_deps = set()

    def barrier(self):
        b = set()
        for e in self.ENG:
            for idx in range(len(self.ops[e]) - 1, -1, -1):
                if self.ops[e][idx]['dma'] is None:
                    b.add(('c', e, idx))
                    break
        for sname, c in self.dma_cnt.items():
            b.add(('d', sname, c))
        self.barrier_deps = b

    def _deps(self, reads, writes):
        deps = set()
        for k in writes:
            if k not in self.state:
                deps.update(self.barrier_deps)
        for k in reads:
            st = self.state.get(k)
            if st and st[0] is not None:
                deps.add(st[0])
        for k in writes:
            st = self.state.get(k)
            if st:
                if st[0] is not None:
                    deps.add(st[0])
                deps.update(st[1])
        return deps

    def _update(self, ev, reads, writes):
        for k in reads:
            st = self.state.setdefault(k, [None, []])
            st[1].append(ev)
        for k in writes:
            self.state[k] = [ev, []]

    def op(self, eng, fn, reads=(), writes=()):
        deps = self._deps(reads, writes)
        idx = len(self.ops[eng])
        ev = ('c', eng, idx)
        if eng == 'pe':
            deps = {d for d in deps if not (d[0] == 'c' and d[1] == 'pe')}
        self.ops[eng].append(dict(fn=fn, deps=deps, ev=ev, sig=False, dma=None))
        self._update(ev, reads, writes)
        return ev

    def dma(self, eng, fn, sem, reads=(), writes=()):
        deps = self._deps(reads, writes)
        c = self.dma_cnt.get(sem, 0) + 1
        self.dma_cnt[sem] = c
        if c > 1:
            deps.add(('d', sem, c - 1))
        ev = ('d', sem, c)
        self.ops[eng].append(dict(fn=fn, deps=deps, ev=ev, sig=False, dma=sem))
        self._update(ev, reads, writes)
        return ev

    def emit(self, final_waits=()):
        nc = self.nc
        for e in self.ENG:
            for o in self.ops[e]:
                for d in o['deps']:
                    if d[0] == 'c':
                        self.ops[d[1]][d[2]]['sig'] = True
        for e in self.ENG:
            n = 0
            for o in self.ops[e]:
                if o['dma'] is None and o['sig']:
                    n += 1
                    o['n'] = n
            self.nsig[e] = n
        with contextlib.ExitStack() as es:
            csem = {e: es.enter_context(nc.semaphore('c_' + e)) for e in self.ENG if self.nsig[e] > 0}
            dsem = {s: es.enter_context(nc.semaphore('d_' + str(s))) for s in self.dma_cnt}
            block = es.enter_context(nc.Block())
            ops = self.ops

            def run(e, engobj):
                waited = {}
                for o in ops[e]:
                    need = {}
                    for d in o['deps']:
                        if d[0] == 'c':
                            tgt = ops[d[1]][d[2]]['n']
                            key = ('c', d[1])
                        else:
                            tgt = 16 * d[2]
                            key = ('d', d[1])
                        if tgt > need.get(key, 0):
                            need[key] = tgt
                    todo = []
                    for key in sorted(need):
                        tgt = need[key]
                        if waited.get(key, 0) >= tgt:
                            continue
                        waited[key] = tgt
                        todo.append((csem[key[1]] if key[0] == 'c' else dsem[key[1]], tgt))
                    attach = None
                    if todo and _cfg('ATTACH', True) and o['dma'] is None:
                        attach = todo.pop()
                    for sem_, tgt in todo:
                        engobj.wait_ge(sem_, tgt)
                    ins = o['fn'](engobj)
                    if attach is not None:
                        ins._wait_ge(attach[0], attach[1])
                    if o['dma'] is not None:
                        ins.then_inc(dsem[o['dma']], 16)
                    elif o['sig']:
                        ins.then_inc(csem[e], 1)
                if e == 'sp':
                    for ev in final_waits:
                        engobj.wait_ge(dsem[ev[1]], 16 * ev[2])

            @block.tensor
            def _(pe):
                run('pe', pe)

            @block.vector
            def _(v):
                run('dve', v)

            @block.scalar
            def _(a):
                run('act', a)

            @block.gpsimd
            def _(g):
                run('pool', g)

            @block.sync
            def _(s):
                run('sp', s)


class _Stop(Exception):
    pass


def build_nc(dbg=(), stop=None):
    nc = bass.Bass("TRN2", target_bir_lowering=False)
    S = Sched(nc)
    D = {}

    def din(name, shape, dt=F32):
        D[name] = nc.dram_tensor(name, list(shape), dt, kind="ExternalInput").ap()

    def dscr(name, shape, dt):
        D[name] = nc.dram_tensor(name, list(shape), dt, kind="Internal").ap()

    def dout(name, shape, dt=F32):
        D[name] = nc.dram_tensor(name, list(shape), dt, kind="ExternalOutput").ap()

    din('x_own', [NTOK, DM]); din('x_oth', [NTOK, DM]); din('ctxb', [256, DM])
    din('cvec', [128, 16]); din('ln_in_g', [1, DM]); din('ln_in_b', [1, DM])
    din('w_mod', [DM, 6144]); din('b_modT', [128, 48]); din('b_mod_row', [1, 6144])
    din('w_in', [DM, 3328])
    din('lamre', [128, 32]); din('lamim', [128, 32]); din('lstep', [128, 32])
    din('bre', [128, 512]); din('bim', [128, 512]); din('cre', [128, 512]); din('cim', [128, 512])
    din('dcol', [128, 4]); din('par', [128, 4])
    din('w_glu', [512, 512]); din('b_gluT', [128, 4]); din('sinkrow', [1, 1024])
    din('w_ssm_out', [512, DM]); din('w_att_out', [512, DM]); din('w_o', [DM, DM])
    din('ln1_g', [1, DM]); din('ln1_b', [1, DM]); din('w_router', [DM, NE]); din('b_router', [1, NE])
    din('w_gate_up', [NE, DM, 2048]); din('b_guT', [128, NE * 16]); din('w_down', [NE, DM, DM]); din('b_down', [NE, DM])
    din('ln2_g', [1, DM]); din('ln2_b', [1, DM])
    din('ident', [128, 128]); din('ropeC', [128, 2176]); din('ropeS', [128, 2176])
    din('masks', [128, 384]); din('iota1', [128, 512])
    dscr('h_scr', [NTOK, DM], F32); dscr('h1_scr', [NTOK, DM], F32)
    dscr('sgs_scr', [8, 128, NTOK], BF16); dscr('sga_scr', [8, 128, NTOK], BF16)
    dscr('soth_scr', [4, 128, NTOK], BF16); dscr('hmT_scr', [8, 128, NTOK], BF16); dscr('o_scr', [4, 128, NTOK], BF16)
    dout('out', [NTOK, DM])
    for item in dbg:
        dout(item[0], item[1], item[2] if len(item) > 2 else F32)

    es = contextlib.ExitStack()
    with es:
        def sb(name, shape, dt=F32):
            return es.enter_context(nc.sbuf_tensor('s_' + name, list(shape), dt))

        psb = [es.enter_context(nc.psum_tensor('psb%d' % i, [128, 512], F32)) for i in range(8)]
        psctr = [0]

        def nps():
            i = psctr[0] % 8
            psctr[0] += 1
            return psb[i], 'ps%d' % i

        def dma(eng, out, in_, sem, r=(), w=()):
            return S.dma(eng, lambda e: e.dma_start(out=out, in_=in_), sem, r, w)

        def mm(out, lhsT, rhs, start, stop, r=(), w=()):
            S.op('pe', lambda e: e.matmul(out, lhsT=lhsT, rhs=rhs, start=start, stop=stop), r, w)

        def tr(out, in_, ident, r=(), w=()):
            S.op('pe', lambda e: e.transpose(out, in_, ident), r, w)

        def act(out, in_, func, r=(), w=(), bias=None, scale=None, eng='act'):
            kw = {}
            if bias is not None:
                kw['bias'] = bias
            if scale is not None:
                kw['scale'] = scale
            S.op(eng, lambda e: e.activation(out=out, in_=in_, func=func, **kw), r, w)

        def tt(eng, out, in0, in1, op, r=(), w=()):
            S.op(eng, lambda e: e.tensor_tensor(out=out, in0=in0, in1=in1, op=op), r, w)

        def ts(eng, out, in0, s1, op0, r=(), w=(), s2=None, op1=None):
            if op1 is None:
                S.op(eng, lambda e: e.tensor_scalar(out=out, in0=in0, scalar1=s1, scalar2=None, op0=op0), r, w)
            else:
                S.op(eng, lambda e: e.tensor_scalar(out=out, in0=in0, scalar1=s1, scalar2=s2, op0=op0, op1=op1), r, w)

        def stt(eng, out, in0, scalar, in1, op0, op1, r=(), w=()):
            S.op(eng, lambda e: e.scalar_tensor_tensor(out=out, in0=in0, scalar=scalar, in1=in1, op0=op0, op1=op1), r, w)

        def cp(eng, out, in_, r=(), w=()):
            if eng == 'act':
                S.op(eng, lambda e: e.copy(out=out, in_=in_), r, w)
            else:
                S.op(eng, lambda e: e.tensor_copy(out=out, in_=in_), r, w)

        def mset(eng, ap, val, w=()):
            S.op(eng, lambda e: e.memset(ap, val), (), w)

        def scan(eng, out, d0, d1, init, r=(), w=()):
            S.op(eng, lambda e: e.tensor_tensor_scan(out=out, data0=d0, data1=d1, initial=init,
                                                     op0=ALU.mult, op1=ALU.add), r, w)

        def pbc(ap):
            return ap.partition_broadcast(128)

        CONST = {}
        OPEN = []

        def rstd(st, sk):
            act(st[:, 14:15], st[:, 13:14], AF.Sqrt, r=[sk, 'epsT'], w=[sk], bias=CONST['epsT'][:, 0:1])
            S.op('dve', lambda e: e.reciprocal(out=st[:, 14:15], in_=st[:, 14:15]), [sk], [sk])

        def debug_out(name, src_ap, keys, eng='pool'):
            if any(it[0] == name for it in dbg):
                dma(eng, D[name], src_ap, 'dbg_' + name, r=list(keys))

        def finish(finals=()):
            fw = {}
            for ev in finals:
                fw[ev[1]] = max(fw.get(ev[1], 0), ev[2])
            for it in dbg:
                name = it[0]
                s_ = 'dbg_' + name
                if s_ in S.dma_cnt:
                    fw[s_] = S.dma_cnt[s_]
            S.emit(final_waits=[('d', k, v) for k, v in fw.items()])

        def checkpoint(tag):
            if stop == tag:
                raise _Stop()

        def body():
            identf = sb('identf', [128, 128])
            dma('sp', identf[:], D['ident'], 'c_ident', w=['identf'])
            halfpi = sb('halfpi', [128, 1])
            mset('dve', halfpi[:], 0.5 * math.pi, w=['halfpi'])
            epsT = sb('epsT', [128, 1])
            mset('dve', epsT[:], LN_EPS, w=['epsT'])
            CONST['epsT'] = epsT
            ones_f = sb('ones_f', [128, 128])
            mset('dve', ones_f[:], 1.0, w=['ones_f'])
            ones_b = sb('ones_b', [128, 128], BF16)
            mset('dve', ones_b[:], 1.0, w=['ones_b'])
            iota1 = sb('iota1', [128, 512])
            dma('sp', iota1[:], D['iota1'], 'c_iota', w=['iota1'])
            masks = sb('masks', [128, 384], BF16)
            dma('pool', masks[:], D['masks'], 'c_masks', w=['masks'])
            dcol = sb('dcol', [128, 4])
            dma('sp', dcol[:], D['dcol'], 'c_dcol', w=['dcol'])
            par = sb('par', [128, 4])
            dma('sp', par[:], D['par'], 'c_par', w=['par'])
            bgluT = sb('bgluT', [128, 4])
            dma('sp', bgluT[:], D['b_gluT'], 'c_bglu', w=['bgluT'])
            esink = sb('esink', [1, 1024], BF16)
            with nc.sbuf_tensor('s_sinkf', [1, 1024], F32) as sinkf:
                dma('sp', sinkf[:], D['sinkrow'], 'c_sink', w=['sinkf'])
                act(esink[:], sinkf[:], AF.Exp, r=['sinkf'], w=['esink'])
            S.barrier()

            cv = sb('cv', [128, 16])
            dma('sp', cv[:], D['cvec'], 'c_cv', w=['cv'])
            sc_ = sb('sc_', [128, 16])
            act(sc_[:], cv[:], AF.Silu, r=['cv'], w=['sc_'])
            sc3 = sc_[:].rearrange("p (k w) -> p k w", w=2)
            bmodT = sb('bmodT', [128, 48])
            dma('sp', bmodT[:], D['b_modT'], 'c_bmodT', w=['bmodT'])
            modT = sb('modT', [128, 32])
            modbc = sb('modbc', [128, 4096])
            dma('sp', modbc[:], pbc(D['b_mod_row'][:, 2048:6144]), 'c_modbc', w=['modbc'])
            wm_v = D['w_mod'].rearrange("(k p) c -> p k c", p=128)
            with contextlib.ExitStack() as es0:
                sbc = es0.enter_context(nc.sbuf_tensor('s_sbc', [128, 8, 128], F32))
                sbcx = es0.enter_context(nc.sbuf_tensor('s_sbcx', [128, 8, 128], F32))
                tmpd = es0.enter_context(nc.sbuf_tensor('s_tmpd', [128, 128], F32))
                mT3 = modT[:].rearrange("p (c w) -> p c w", w=2)
                for k in range(8):
                    cp('dve', sbc[:, k, :], sc3[:, k, 0:1].to_broadcast([128, 128]), r=['sc_'], w=['sbc'])
                    cp('dve', sbcx[:, k, :], sc3[:, k, 1:2].to_broadcast([128, 128]), r=['sc_'], w=['sbcx'])
                wmb = [es0.enter_context(nc.sbuf_tensor('s_wmb%d' % i, [128, 8, 512], F32)) for i in range(2)]
                for i in range(12):
                    wb = wmb[i % 2]
                    wk = 'wmb%d' % (i % 2)
                    dma('sp', wb[:], wm_v[:, :, i * 512:(i + 1) * 512], wk, w=[wk])
                    if i < 4:
                        for which, sbw, sbk in ((0, sbc, 'sbc'), (1, sbcx, 'sbcx')):
                            ps, pk = nps()
                            for k in range(8):
                                mm(ps[:], sbw[:, k, :], wb[:, k, :], k == 0, k == 7, r=[wk, sbk], w=[pk])
                            for ctl in range(4):
                                ct = i * 4 + ctl
                                tt('dve', tmpd[:], ps[:, ctl * 128:(ctl + 1) * 128], identf[:], ALU.mult, r=[pk, 'identf'], w=['tmpd'])
                                S.op('dve', lambda e, ct=ct, which=which: e.reduce_sum(out=mT3[:, ct, which:which + 1], in_=tmpd[:],
                                                                                     axis=mybir.AxisListType.X), ['tmpd'], ['modT'])
                    else:
                        ps, pk = nps()
                        for k in range(8):
                            mm(ps[:], sbc[:, k, :], wb[:, k, :], k == 0, k == 7, r=[wk, 'sbc'], w=[pk])
                        c0 = (i - 4) * 512
                        tt('dve', modbc[:, c0:c0 + 512], ps[:], modbc[:, c0:c0 + 512], ALU.add, r=[pk, 'modbc'], w=['modbc'])
                tt('dve', mT3, mT3, bmodT[:, 0:16].unsqueeze(2).to_broadcast([128, 16, 2]), ALU.add, r=['modT', 'bmodT'], w=['modT'])
            S.barrier()
            onep = sb('onep', [128, 16])
            ts('dve', onep[:], modT[:, 16:32], 1.0, ALU.add, r=['modT'], w=['onep'])
            onep3 = onep[:].rearrange("p (k w) -> p k w", w=2)
            sh13 = modT[:, 0:16].rearrange("p (k w) -> p k w", w=2)
            ts('pool', modbc[:, 2048:3072], modbc[:, 2048:3072], 1.0, ALU.add, r=['modbc'], w=['modbc'])
            G1 = modbc[:, 0:1024]; SH2 = modbc[:, 1024:2048]; ONESC2 = modbc[:, 2048:3072]; G2 = modbc[:, 3072:4096]
            debug_out('dbg_modT', modT[:], ['modT'], 'sp')
            debug_out('dbg_modbc', modbc[:], ['modbc'], 'sp')
            checkpoint('p0')

            def s5_setup(Bm, Cm, magt, thr):
                with contextlib.ExitStack() as es0:
                    def t0(name, shape, dt=F32):
                        return es0.enter_context(nc.sbuf_tensor('s_' + name, list(shape), dt))
                    lr = t0('lr', [128, 32]); li = t0('li', [128, 32]); ls = t0('ls', [128, 32])
                    dma('sp', lr[:], D['lamre'], 'c_lr', w=['lr']); dma('sp', li[:], D['lamim'], 'c_li', w=['li'])
                    dma('sp', ls[:], D['lstep'], 'c_ls', w=['ls'])
                    bre = t0('bre', [128, 512]); bim = t0('bim', [128, 512]); cre = t0('cre', [128, 512]); cim = t0('cim', [128, 512])
                    dma('sp', bre[:], D['bre'], 'c_bre', w=['bre']); dma('sp', bim[:], D['bim'], 'c_bim', w=['bim'])
                    dma('sp', cre[:], D['cre'], 'c_cre', w=['cre']); dma('sp', cim[:], D['cim'], 'c_cim', w=['cim'])
                    dt_ = t0('dt_', [128, 32]); lrdt = t0('lrdt', [128, 32]); lidt = t0('lidt', [128, 32])
                    a1 = t0('a1', [128, 32]); a2 = t0('a2', [128, 32]); sinv = t0('sinv', [128, 32]); cosv = t0('cosv', [128, 32])
                    ar = t0('ar', [128, 32]); ai = t0('ai', [128, 32]); den = t0('den', [128, 32]); tq = t0('tq', [128, 32])
                    crr = t0('crr', [128, 32]); cii = t0('cii', [128, 32])
                    act(dt_[:], ls[:], AF.Exp, r=['ls'], w=['dt_'])
                    tt('dve', lrdt[:], lr[:], dt_[:], ALU.mult, r=['lr', 'dt_'], w=['lrdt'])
                    tt('dve', lidt[:], li[:], dt_[:], ALU.mult, r=['li', 'dt_'], w=['lidt'])
                    act(magt[:], lrdt[:], AF.Exp, r=['lrdt'], w=['magt'])
                    ts('dve', a1[:], lidt[:], 1.0 / TWO_PI, ALU.mult, r=['lidt'], w=['a1'])
                    ts('dve', a2[:], a1[:], MAGIC, ALU.add, r=['a1'], w=['a2'])
                    stt('dve', thr[:], a2[:], MAGIC, a1[:], ALU.subtract, ALU.subtract, r=['a2', 'a1'], w=['thr'])
                    act(a2[:], thr[:], AF.Abs, r=['thr'], w=['a2'])
                    act(sinv[:], thr[:], AF.Sin, r=['thr'], w=['sinv'], scale=-TWO_PI)
                    act(cosv[:], a2[:], AF.Sin, r=['a2', 'halfpi'], w=['cosv'], bias=halfpi[:, 0:1], scale=-TWO_PI)
                    ts('dve', thr[:], thr[:], -1.0, ALU.mult, r=['thr', 'sinv'], w=['thr'])
                    tt('dve', ar[:], magt[:], cosv[:], ALU.mult, r=['magt', 'cosv'], w=['ar'])
                    tt('dve', ai[:], magt[:], sinv[:], ALU.mult, r=['magt', 'sinv'], w=['ai'])
                    tt('dve', den[:], lr[:], lr[:], ALU.mult, r=['lr'], w=['den'])
                    tt('dve', tq[:], li[:], li[:], ALU.mult, r=['li'], w=['tq'])
                    tt('dve', den[:], den[:], tq[:], ALU.add, r=['den', 'tq'], w=['den'])
                    S.op('dve', lambda e: e.reciprocal(out=den[:], in_=den[:]), ['den'], ['den'])
                    ts('dve', ar[:], ar[:], -1.0, ALU.add, r=['ar'], w=['ar'])
                    tt('dve', crr[:], ar[:], lr[:], ALU.mult, r=['ar', 'lr'], w=['crr'])
                    tt('dve', tq[:], ai[:], li[:], ALU.mult, r=['ai', 'li'], w=['tq'])
                    tt('dve', crr[:], crr[:], tq[:], ALU.add, r=['crr', 'tq'], w=['crr'])
                    tt('dve', crr[:], crr[:], den[:], ALU.mult, r=['crr', 'den'], w=['crr'])
                    tt('dve', cii[:], ai[:], lr[:], ALU.mult, r=['ai', 'lr'], w=['cii'])
                    tt('dve', tq[:], ar[:], li[:], ALU.mult, r=['ar', 'li'], w=['tq'])
                    tt('dve', cii[:], cii[:], tq[:], ALU.subtract, r=['cii', 'tq'], w=['cii'])
                    tt('dve', cii[:], cii[:], den[:], ALU.mult, r=['cii', 'den'], w=['cii'])
                    bbr = t0('bbr', [128, 512]); bbi = t0('bbi', [128, 512]); tb = t0('tb', [128, 512])
                    crb = crr[:].unsqueeze(2).to_broadcast([128, 32, 16]); cib = cii[:].unsqueeze(2).to_broadcast([128, 32, 16])
                    v3 = lambda t: t[:].rearrange("p (a h) -> p a h", h=16)
                    tt('dve', v3(bbr), v3(bre), crb, ALU.mult, r=['bre', 'crr'], w=['bbr'])
                    tt('dve', v3(tb), v3(bim), cib, ALU.mult, r=['bim', 'cii'], w=['tb'])
                    tt('dve', bbr[:], bbr[:], tb[:], ALU.subtract, r=['bbr', 'tb'], w=['bbr'])
                    tt('dve', v3(bbi), v3(bim), crb, ALU.mult, r=['bim', 'crr'], w=['bbi'])
                    tt('dve', v3(tb), v3(bre), cib, ALU.mult, r=['bre', 'cii', 'bbr'], w=['tb'])
                    tt('dve', bbi[:], bbi[:], tb[:], ALU.add, r=['bbi', 'tb'], w=['bbi'])
                    Bst = t0('Bst', [128, 64 * 128])
                    mset('pool', Bst[:], 0.0, w=['Bst'])
                    for d in range(2):
                        for s in range(2):
                            for ri, src in ((0, bbr), (1, bbi)):
                                base = Bst[s * 64:(s + 1) * 64, ((d * 16) * 2 + ri) * 128 + s * 16:((d * 16) * 2 + ri) * 128 + s * 16 + 1]
                                dst = bass.AP(base.tensor, base.offset, [list(base.ap[0]), [1024, 4], [288, 4], [1, 16]])
                                sv = src[s * 64:(s + 1) * 64, d * 256:(d + 1) * 256].rearrange("p (c j h) -> p c j h", c=4, j=4)
                                cp('dve', dst, sv, r=['bbr', 'bbi', 'Bst'], w=['Bst'])
                    for q4 in range(16):
                        ps, pk = nps()
                        for i4 in range(4):
                            idx = q4 * 4 + i4
                            tr(ps[:, i4 * 128:(i4 + 1) * 128], Bst[:, idx * 128:(idx + 1) * 128], identf[:], r=['Bst', 'identf'], w=[pk])
                        cp('act', Bm[:, q4 * 4:(q4 + 1) * 4, :], ps[:].rearrange("p (a c) -> p a c", c=128), r=[pk], w=['Bm'])
                    Xc = t0('Xc', [128, 16 * 128])
                    Xv = Xc[:].rearrange("p (a r c) -> p a r c", r=2, c=128)
                    c3 = lambda t: t[:].rearrange("p (a q) -> p a q", q=64)
                    for s in range(2):
                        ts('dve', Xv[:, :, 0, s * 64:(s + 1) * 64], c3(cre), par[:, s:s + 1], ALU.mult, r=['cre', 'par', 'Xc'], w=['Xc'])
                        ts('dve', Xv[:, :, 1, s * 64:(s + 1) * 64], c3(cim), par[:, 2 + s:3 + s], ALU.mult, r=['cim', 'par', 'Xc'], w=['Xc'])
                    mset('pool', Cm[:], 0.0, w=['Cm'])
                    for q4 in range(4):
                        ps, pk = nps()
                        for i4 in range(4):
                            idx = q4 * 4 + i4
                            tr(ps[:, i4 * 128:(i4 + 1) * 128], Xc[:, idx * 128:(idx + 1) * 128], identf[:], r=['Xc', 'identf'], w=[pk])
                        for i4 in range(4):
                            idx = q4 * 4 + i4
                            dct, ri = idx // 2, idx % 2
                            d, ct = dct // 4, dct % 4
                            base = Cm[:, (d * 16 + ct * 4) * 2 + ri, 0:1]
                            dst = bass.AP(base.tensor, base.offset, [list(base.ap[0]), [2 * 128 + 32, 4], [1, 32]])
                            cp('dve', dst, ps[:, i4 * 128:(i4 + 1) * 128].rearrange("p (j c) -> p j c", c=32), r=[pk, 'Cm'], w=['Cm'])

                S.barrier()

            gates = sb('gates', [128, 16, NE])
            yacc = sb('yacc', [128, 4, NTOK])
            es13 = contextlib.ExitStack()
            es13.__enter__()
            OPEN.append(es13)
            sT_own = es13.enter_context(nc.sbuf_tensor('s_sT_own', [128, 4, NTOK], BF16))
            sT_ctx = es13.enter_context(nc.sbuf_tensor('s_sT_ctx', [128, 4, 256], BF16))
            es1 = contextlib.ExitStack()
            es1.__enter__()
            OPEN.append(es1)

            def t1(name, shape, dt=F32):
                return es1.enter_context(nc.sbuf_tensor('s_' + name, list(shape), dt))
            qT = t1('qT', [128, 4, NTOK], BF16)
            kT = t1('kT', [128, 2, 2432], BF16)
            vv = t1('vv', [128, 19, 128], BF16)
            es1b = contextlib.ExitStack()
            es1b.__enter__()
            OPEN.append(es1b)

            def t1b(name, shape, dt=F32):
                return es1b.enter_context(nc.sbuf_tensor('s_' + name, list(shape), dt))
            gbc = t1b('gbc', [128, DM]); bbc = t1b('bbc', [128, DM])
            dma('sp', gbc[:], pbc(D['ln_in_g']), 'c_gbc', w=['gbc'])
            dma('sp', bbc[:], pbc(D['ln_in_b']), 'c_bbc', w=['bbc'])
            ropeCb = [t1b('ropeC%d' % i, [128, 512]) for i in range(2)]
            ropeSb = [t1b('ropeS%d' % i, [128, 512]) for i in range(2)]
            ropecur = [None, None, None, None]
            win_v = D['w_in'].rearrange("(k p) c -> p k c", p=128)
            w_s = t1b('w_s', [128, 8, 512], BF16); w_q = t1b('w_q', [128, 8, 512], BF16)
            w_k = t1b('w_k', [128, 8, 256], BF16); w_v = t1b('w_v', [128, 8, 128], BF16)
            w_gs = yacc[:, 0:2, :].rearrange('p a b -> p (a b)').bitcast(BF16).rearrange('p (k c) -> p k c', c=1024)
            w_ga = yacc[:, 2:4, :].rearrange('p a b -> p (a b)').bitcast(BF16).rearrange('p (k c) -> p k c', c=1024)
            dma('pool', w_s[:], win_v[:, :, 0:512], 'c_ws', w=['w_s'])
            dma('pool', w_q[:], win_v[:, :, 512:1024], 'c_wq', w=['w_q'])
            for kvh in range(2):
                for dup in range(2):
                    dma('pool', w_k[:, :, kvh * 128 + dup * 64:kvh * 128 + dup * 64 + 64],
                        win_v[:, :, 1024 + kvh * 64:1024 + kvh * 64 + 64], 'c_wk%d%d' % (kvh, dup), w=['w_k%d%d' % (kvh, dup)])
            wk_keys = ['w_k00', 'w_k01', 'w_k10', 'w_k11']
            dma('pool', w_v[:], win_v[:, :, 1152:1280], 'c_wv', w=['w_v'])
            dma('pool', w_gs, win_v[:, :, 1280:2304], 'c_wgs', w=['w_gs'])
            dma('pool', w_ga, win_v[:, :, 2304:3328], 'c_wga', w=['w_ga'])
            xt = [t1b('xt%d' % i, [128, DM]) for i in range(3)]
            ht = [t1b('ht%d' % i, [128, DM]) for i in range(2)]
            stt_ = [t1b('st%d' % i, [128, 16]) for i in range(2)]
            uT = [t1b('uT%d' % i, [128, 8, 512], BF16) for i in range(2)]
            rA = [t1b('rA%d' % i, [128, 512]) for i in range(2)]
            rB = [t1b('rB%d' % i, [128, 512]) for i in range(2)]
            stg = [t1b('stg%d' % i, [128, 512], BF16) for i in range(4)]

            LNENG = _cfg('LNENG', 'dve')
            ROPENG = _cfg('ROPENG', 'pool')
            tilectr = [0]
            stgctr = [0]
            ropectr = [0]

            def ln_tile(src_ap, which, ug, ugk, col0, spill_row=None):
                i = tilectr[0]
                tilectr[0] += 1
                x = xt[i % 3]; xk = 'xt%d' % (i % 3)
                h = ht[i % 2]; hk = 'ht%d' % (i % 2)
                st = stt_[i % 2]; sk = 'st%d' % (i % 2)
                dma('sp', x[:], src_ap, xk, w=[xk])
                S.op('dve', lambda e: e.bn_stats(out=st[:, 0:6], in_=x[:, 0:512]), [xk], [sk])
                S.op('dve', lambda e: e.bn_stats(out=st[:, 6:12], in_=x[:, 512:1024]), [xk, sk], [sk])
                S.op('dve', lambda e: e.bn_aggr(out=st[:, 12:14], in_=st[:, 0:12]), [sk], [sk])
                rstd(st, sk)
                ts('dve', x[:], x[:], st[:, 12:13], ALU.subtract, r=[xk, sk], w=[xk], s2=st[:, 14:15], op1=ALU.mult)
                tt(LNENG, h[:], x[:], gbc[:], ALU.mult, r=[xk, 'gbc'], w=[hk])
                tt(LNENG, h[:], h[:], bbc[:], ALU.add, r=[hk, 'bbc'], w=[hk])
                if spill_row is not None:
                    t = spill_row // 128
                    dma('sp', D['h_scr'][spill_row:spill_row + 128, :], h[:], 'hs%d' % (t % 4), r=[hk], w=[('hscr', t)])
                for half in range(2):
                    ps, pk = nps()
                    for kk in range(4):
                        k = half * 4 + kk
                        tr(ps[:, kk * 128:(kk + 1) * 128], h[:, k * 128:(k + 1) * 128], identf[:], r=[hk, 'identf'], w=[pk])
                    for kk in range(4):
                        k = half * 4 + kk
                        act(uT[ug][:, k, col0:col0 + 128], ps[:, kk * 128:(kk + 1) * 128], AF.Identity,
                            r=[pk, 'onep', 'modT'], w=[ugk], bias=sh13[:, k, which:which + 1], scale=onep3[:, k, which:which + 1])

            def proj(ug, ugk, wt, wkeys, c0, n):
                ps, pk = nps()
                for k in range(8):
                    mm(ps[:, 0:n], wt[:, k, c0:c0 + 128], uT[ug][:, k, 0:n], k == 0, k == 7, r=[ugk] + list(wkeys), w=[pk])
                return ps, pk

            def rope_load(slot, rc0, n):
                dma('sp', ropeCb[slot][:, 0:n], D['ropeC'][:, rc0:rc0 + n], 'ropeC%d' % slot, w=['ropeC%d' % slot])
                dma('sp', ropeSb[slot][:, 0:n], D['ropeS'][:, rc0:rc0 + n], 'ropeS%d' % slot, w=['ropeS%d' % slot])
                ropecur[0] = ropeCb[slot]; ropecur[1] = ropeSb[slot]; ropecur[2] = 'ropeC%d' % slot; ropecur[3] = 'ropeS%d' % slot

            def rope(ps, pk, pc0, n, dst, dkey):
                i = ropectr[0] % 2
                ropectr[0] += 1
                ropeC, ropeS, rck, rsk = ropecur
                A = rA[i]; Ak = 'rA%d' % i; B = rB[i]; Bk = 'rB%d' % i
                tt('dve', A[:, 0:n], ps[:, pc0:pc0 + n], ropeC[:, 0:n], ALU.mult, r=[pk, rck], w=[Ak])
                for (o, s_) in ((0, 32), (32, 0), (64, 96), (96, 64)):
                    tt('dve', B[o:o + 32, 0:n], ps[s_:s_ + 32, pc0:pc0 + n], ropeS[s_:s_ + 32, 0:n], ALU.mult,
                       r=[pk, rsk], w=[Bk])
                tt(ROPENG, dst, A[:, 0:n], B[:, 0:n], ALU.add, r=[Ak, Bk], w=[dkey])

            def group(kind, gi):
                ug = (0 if kind == 'ctx' else 1 + gi + (4 if kind == 'oth' else 0)) % 2
                ugk = 'uT%d' % ug
                n = 256 if kind == 'ctx' else 512
                for t in range(n // 128):
                    if kind == 'ctx':
                        ln_tile(D['ctxb'][t * 128:(t + 1) * 128, :], 1, ug, ugk, t * 128)
                    elif kind == 'own':
                        row = gi * 512 + t * 128
                        ln_tile(D['x_own'][row:row + 128, :], 0, ug, ugk, t * 128, spill_row=row)
                    else:
                        row = gi * 512 + t * 128
                        ln_tile(D['x_oth'][row:row + 128, :], 0, ug, ugk, t * 128)
                for ct in range(4):
                    ps, pk = proj(ug, ugk, w_s, ['w_s'], ct * 128, n)
                    if kind == 'own':
                        cp('act', sT_own[:, ct, gi * 512:(gi + 1) * 512], ps[:, 0:n], r=[pk], w=[('sT_own', gi)])
                    elif kind == 'ctx':
                        cp('act', sT_ctx[:, ct, :], ps[:, 0:n], r=[pk], w=['sT_ctx'])
                    else:
                        si = stgctr[0] % 4
                        stgctr[0] += 1
                        cp('act', stg[si][:], ps[:, 0:n], r=[pk], w=['stg%d' % si])
                        dma('sp', D['soth_scr'][ct, :, gi * 512:(gi + 1) * 512], stg[si][:], 'stg%d' % si,
                            r=['stg%d' % si], w=[('soth', ct, gi)])
                halo = (kind == 'oth' and gi == 3)
                if kind == 'own':
                    rope_load(gi % 2, gi * 512, 512)
                elif halo:
                    rope_load(0, 2048, 128)
                if kind != 'oth' or halo:
                    for kvh in range(2):
                        ps, pk = proj(ug, ugk, w_k, wk_keys, kvh * 128, n)
                        if kind == 'ctx':
                            cp('act', kT[:, kvh, 2176:2432], ps[:, 0:256], r=[pk], w=[('kT', kvh, 'ctx')])
                        elif kind == 'own':
                            rope(ps, pk, 0, 512, kT[:, kvh, gi * 512:(gi + 1) * 512], ('kT', kvh, gi))
                        else:
                            rope(ps, pk, 384, 128, kT[:, kvh, 2048:2176], ('kT', kvh, 'halo'))
                    tl = range(n // 128) if not halo else [3]
                    for t in tl:
                        vt = {'ctx': 17 + t, 'own': gi * 4 + t, 'oth': 16}[kind]
                        ps, pk = nps()
                        for k in range(8):
                            mm(ps[:, 0:128], uT[ug][:, k, t * 128:(t + 1) * 128], w_v[:, k, :], k == 0, k == 7,
                               r=[ugk, 'w_v'], w=[pk])
                        cp('act', vv[:, vt, :], ps[:, 0:128], r=[pk], w=[('vv', vt)])
                if kind == 'own':
                    for qt in range(4):
                        ps, pk = proj(ug, ugk, w_q, ['w_q'], qt * 128, n)
                        rope(ps, pk, 0, 512, qT[:, qt, gi * 512:(gi + 1) * 512], ('qT', gi))
                    for (wt, wkey, scr, nm) in ((w_gs, 'w_gs', 'sgs_scr', 'sgs'), (w_ga, 'w_ga', 'sga_scr', 'sga')):
                        for ot in range(8):
                            ps, pk = proj(ug, ugk, wt, [wkey], ot * 128, n)
                            si = stgctr[0] % 4
                            stgctr[0] += 1
                            act(stg[si][:], ps[:, 0:n], AF.Sigmoid, r=[pk], w=['stg%d' % si])
                            dma('sp', D[scr][ot, :, gi * 512:(gi + 1) * 512], stg[si][:], 'stg%d' % si,
                                r=['stg%d' % si], w=[(nm, ot, gi)])

            group('ctx', 0)
            for gi in range(4):
                group('own', gi)
            for gi in range(4):
                group('oth', gi)
            es1b.__exit__(None, None, None)
            OPEN.remove(es1b)
            S.barrier()
            debug_out('dbg_sT', sT_own[:, :, :], [('sT_own', g) for g in range(4)])
            debug_out('dbg_qT', qT[:, :, :], [('qT', g) for g in range(4)])
            debug_out('dbg_kT', kT[:, :, :], [('kT', a, b) for a in range(2) for b in (0, 1, 2, 3, 'halo', 'ctx')])
            debug_out('dbg_vv', vv[:, :, :], [('vv', t) for t in range(19)])
            checkpoint('p1')

            with contextlib.ExitStack() as es2:
                pT = [es2.enter_context(nc.sbuf_tensor('s_pT%d' % i, [128, 512], BF16)) for i in range(3)]
                rec = [es2.enter_context(nc.sbuf_tensor('s_rec%d' % i, [64, 512], F32)) for i in range(2)]
                ost = [es2.enter_context(nc.sbuf_tensor('s_ost%d' % i, [128, 2, 128], BF16)) for i in range(2)]
                GORD = [0, 2, 1, 3]
                items = []
                for kvh in range(2):
                    for qt in range(16):
                        gi = qt // 4
                        tiles = []
                        if qt > 0:
                            tiles.append(((qt - 1) * 128, qt - 1, 0, ('kT', kvh, (qt - 1) // 4), ('vv', qt - 1)))
                        tiles.append((qt * 128, qt, None, ('kT', kvh, gi), ('vv', qt)))
                        if qt < 15:
                            tiles.append(((qt + 1) * 128, qt + 1, 1, ('kT', kvh, (qt + 1) // 4), ('vv', qt + 1)))
                        else:
                            tiles.append((2048, 16, 2, ('kT', kvh, 'halo'), ('vv', 16)))
                        tiles.append((2176, 17, None, ('kT', kvh, 'ctx'), ('vv', 17)))
                        tiles.append((2304, 18, None, ('kT', kvh, 'ctx'), ('vv', 18)))
                        for ti, tl in enumerate(tiles):
                            items.append((kvh, qt, ti, len(tiles), tl))

                def emit_scores(idx):
                    kvh, qt, ti, nt, (kc0, vt, mk, kkey, vkey) = items[idx]
                    gi = qt // 4
                    sb_ = 4 + (idx % 2) * 2
                    pssA, pskA = psb[sb_], 'ps%d' % sb_
                    pssB, pskB = psb[sb_ + 1], 'ps%d' % (sb_ + 1)
                    for s_ in range(4):
                        g = GORD[s_]
                        h = kvh * 4 + g
                        hh = h % 2
                        pss, psk = (pssA, pskA) if hh == 0 else (pssB, pskB)
                        c0_ = (s_ % 2) * 128
                        mm(pss[:, c0_:c0_ + 128], kT[hh * 64:(hh + 1) * 64, kvh, kc0:kc0 + 128],
                           qT[hh * 64:(hh + 1) * 64, h // 2, qt * 128:(qt + 1) * 128], True, True,
                           r=[kkey, ('qT', gi)], w=[psk])
                    p = pT[idx % 3]; pkey = 'pT%d' % (idx % 3)
                    act(p[:, 0:256], pssA[:, 0:256], AF.Exp, r=[pskA], w=[pkey], scale=0.125)
                    act(p[:, 256:512], pssB[:, 0:256], AF.Exp, r=[pskB, pkey], w=[pkey], scale=0.125)
                    if mk is not None:
                        mv = masks[:, mk * 128:(mk + 1) * 128].unsqueeze(1).to_broadcast([128, 4, 128])
                        tt('dve', p[:].rearrange("p (g q) -> p g q", g=4), p[:].rearrange("p (g q) -> p g q", g=4), mv,
                           ALU.mult, r=[pkey, 'masks'], w=[pkey])

                def emit_pv(idx):
                    kvh, qt, ti, nt, (kc0, vt, mk, kkey, vkey) = items[idx]
                    it = kvh * 16 + qt
                    ab_ = (it % 2) * 2
                    pso, pok = psb[ab_], 'ps%d' % ab_
                    psd, pdk = psb[ab_ + 1], 'ps%d' % (ab_ + 1)
                    p = pT[idx % 3]; pkey = 'pT%d' % (idx % 3)
                    mm(pso[0:64, :], vv[:, vt, kvh * 64:(kvh + 1) * 64], p[:], ti == 0, ti == nt - 1, r=[vkey, pkey], w=[pok])
                    mm(psd[0:64, :], ones_b[:, 0:64], p[:], ti == 0, False, r=['ones_b', pkey], w=[pdk])
                    if ti < nt - 1:
                        return
                    mm(psd[0:64, :], ones_b[0:1, 0:64], esink[0:1, kvh * 512:(kvh + 1) * 512], False, True,
                       r=['ones_b', 'esink'], w=[pdk])
                    rc = rec[it % 2]; rk = 'rec%d' % (it % 2)
                    S.op('dve', lambda e: e.reciprocal(out=rc[:], in_=psd[0:64, :]), [pdk], [rk])
                    osl = it % 2
                    for s_ in range(4):
                        g = GORD[s_]
                        hh = g % 2
                        tt('dve', ost[osl][hh * 64:(hh + 1) * 64, g // 2, :],
                           pso[0:64, s_ * 128:(s_ + 1) * 128], rc[:, s_ * 128:(s_ + 1) * 128], ALU.mult,
                           r=[pok, rk], w=['ost%d' % osl])
                    dma('sp', D['o_scr'][kvh * 2:kvh * 2 + 2, :, qt * 128:(qt + 1) * 128].rearrange("k p t -> p k t"),
                        ost[osl][:], 'ost%d' % osl, r=['ost%d' % osl], w=[('oscr', kvh, qt)])

                for idx in range(len(items)):
                    emit_scores(idx)
                    if idx > 0:
                        emit_pv(idx - 1)
                emit_pv(len(items) - 1)
            es1.__exit__(None, None, None)
            OPEN.remove(es1)
            S.barrier()
            debug_out('dbg_o', D['o_scr'], [('oscr', a, b) for a in range(2) for b in range(16)], 'sp')
            checkpoint('p2')

            with contextlib.ExitStack() as es3:
                def t3(name, shape, dt=F32):
                    return es3.enter_context(nc.sbuf_tensor('s_' + name, list(shape), dt))
                Bm = t3('Bm', [128, 64, 128], BF16)
                Cm = t3('Cm', [128, 64, 128], BF16)
                magt = t3('magt', [128, 32]); thr = t3('thr', [128, 32])
                s5_setup(Bm, Cm, magt, thr)
                nmU = ['XR', 'XI', 'M1', 'M2', 'TR', 'TI']
                ub = [{nm: t3('%s%d' % (nm, i), [128, 512]) for nm in nmU} for i in range(4)]
                tabC = [t3('tabC%d' % i, [128, 512]) for i in range(4)]
                tabS = [t3('tabS%d' % i, [128, 512]) for i in range(4)]
                sre = [[t3('sre%d_%d' % (i, jj), [128, 512], BF16) for jj in range(4)] for i in range(2)]
                sim = [[t3('sim%d_%d' % (i, jj), [128, 512], BF16) for jj in range(4)] for i in range(2)]
                soth = [t3('soth%d' % i, [128, 512], BF16) for i in range(2)]
                carry = t3('carry', [128, 64])
                ctmp = t3('ctmp', [128, 8])
                sctr = 0
                octr = 0

                def rev(ap2):
                    n = ap2.ap[-1][1]
                    return bass.AP(ap2.tensor, ap2.offset + (n - 1) * ap2.ap[-1][0], [list(ap2.ap[0]), [-ap2.ap[-1][0], n]])

                ENGJ = ['dve', 'dve', 'dve', 'dve']
                for d in range(2):
                    if d == 0:
                        segs = [('ctx', 0, False)] + [('own', c, False) for c in range(4)]
                    else:
                        segs = [('ctx', 0, True)] + [('oth', c, False) for c in range(4)] + [('own', c, True) for c in (3, 2, 1, 0)]
                    for ct in range(4):
                        for jj in range(4):
                            dj = d * 16 + ct * 4 + jj
                            E = ENGJ[jj]
                            u = ub[jj]
                            kM1 = 'M1_%d' % jj; kM2 = 'M2_%d' % jj; kC = 'tabC%d' % jj; kS = 'tabS%d' % jj
                            ts(E, u['M1'][:], iota1[:], thr[:, dj:dj + 1], ALU.mult, r=['iota1', 'thr'], w=[kM1])
                            ts(E, u['M2'][:], u['M1'][:], MAGIC, ALU.add, r=[kM1], w=[kM2])
                            ts(E, u['M2'][:], u['M2'][:], -MAGIC, ALU.add, r=[kM2], w=[kM2])
                            tt(E, tabS[jj][:], u['M2'][:], u['M1'][:], ALU.subtract, r=[kM2, kM1], w=[kS])
                            act(tabC[jj][:], tabS[jj][:], AF.Abs, r=[kS], w=[kC])
                            act(tabS[jj][:], tabS[jj][:], AF.Sin, r=[kS, kC], w=[kS], scale=-TWO_PI)
                            act(tabC[jj][:], tabC[jj][:], AF.Sin, r=[kC, 'halfpi'], w=[kC], bias=halfpi[:, 0:1], scale=-TWO_PI)
                        for si_, (kind, c, rv) in enumerate(segs):
                            n = 256 if kind == 'ctx' else 512
                            if kind == 'ctx':
                                src = sT_ctx[:, ct, :]; skey = 'sT_ctx'
                            elif kind == 'own':
                                src = sT_own[:, ct, c * 512:(c + 1) * 512]; skey = ('sT_own', c)
                            else:
                                so = soth[octr % 2]; skey = 'soth%d' % (octr % 2)
                                octr += 1
                                dma('sp', so[:], D['soth_scr'][ct, :, c * 512:(c + 1) * 512], skey, r=[('soth', ct, c)], w=[skey])
                                src = so[:]
                            if rv:
                                src = rev(src)
                            sslot = sctr % 2
                            if kind == 'own':
                                sctr += 1
                            K = lambda nm, jj: '%s_%d' % (nm, jj)
                            for jj in range(4):
                                dj = d * 16 + ct * 4 + jj
                                u = ub[jj]
                                psr, prk = nps()
                                psi, pik = nps()
                                mm(psr[:, 0:n], Bm[:, dj * 2, :], src, True, True, r=[skey, 'Bm'], w=[prk])
                                mm(psi[:, 0:n], Bm[:, dj * 2 + 1, :], src, True, True, r=[skey, 'Bm'], w=[pik])
                                cp('act', u['XR'][:, 0:n], psr[:, 0:n], r=[prk], w=[K('XR', jj)])
                                cp('act', u['XI'][:, 0:n], psi[:, 0:n], r=[pik], w=[K('XI', jj)])
                            for jj in range(4):
                                E = ENGJ[jj]; u = ub[jj]
                                tt(E, u['M1'][:, 0:n], u['XR'][:, 0:n], tabC[jj][:, 0:n], ALU.mult, r=[K('XR', jj), 'tabC%d' % jj], w=[K('M1', jj)])
                                tt(E, u['M2'][:, 0:n], u['XI'][:, 0:n], tabS[jj][:, 0:n], ALU.mult, r=[K('XI', jj), 'tabS%d' % jj], w=[K('M2', jj)])
                            for jj in range(4):
                                E = ENGJ[jj]; u = ub[jj]
                                tt(E, u['TR'][:, 0:n], u['M1'][:, 0:n], u['M2'][:, 0:n], ALU.add, r=[K('M1', jj), K('M2', jj)], w=[K('TR', jj)])
                            for jj in range(4):
                                E = ENGJ[jj]; u = ub[jj]
                                tt(E, u['M1'][:, 0:n], u['XI'][:, 0:n], tabC[jj][:, 0:n], ALU.mult, r=[K('XI', jj), 'tabC%d' % jj], w=[K('M1', jj)])
                                tt(E, u['M2'][:, 0:n], u['XR'][:, 0:n], tabS[jj][:, 0:n], ALU.mult, r=[K('XR', jj), 'tabS%d' % jj], w=[K('M2', jj)])
                            for jj in range(4):
                                E = ENGJ[jj]; u = ub[jj]
                                tt(E, u['TI'][:, 0:n], u['M1'][:, 0:n], u['M2'][:, 0:n], ALU.subtract, r=[K('M1', jj), K('M2', jj)], w=[K('TI', jj)])
                            for jj in range(4):
                                dj = d * 16 + ct * 4 + jj
                                u = ub[jj]
                                ck = ('carry', dj)
                                mg = magt[:, dj:dj + 1].to_broadcast([128, n])
                                if si_ == 0:
                                    scan('dve', u['XR'][:, 0:n], mg, u['TR'][:, 0:n], 0.0, r=['magt', K('TR', jj)], w=[K('XR', jj)])
                                    scan('dve', u['XI'][:, 0:n], mg, u['TI'][:, 0:n], 0.0, r=['magt', K('TI', jj)], w=[K('XI', jj)])
                                else:
                                    scan('dve', u['XR'][:, 0:n], mg, u['TR'][:, 0:n], carry[:, 2 * dj:2 * dj + 1], r=['magt', K('TR', jj), ck], w=[K('XR', jj)])
                                    scan('dve', u['XI'][:, 0:n], mg, u['TI'][:, 0:n], carry[:, 2 * dj + 1:2 * dj + 2], r=['magt', K('TI', jj), ck], w=[K('XI', jj)])
                            if si_ < len(segs) - 1:
                                for jj in range(4):
                                    dj = d * 16 + ct * 4 + jj
                                    u = ub[jj]
                                    ck = ('carry', dj)
                                    cn = tabC[jj][:, n - 1:n]; sn_ = tabS[jj][:, n - 1:n]
                                    rl = u['XR'][:, n - 1:n]; il = u['XI'][:, n - 1:n]
                                    tk_ = 'ctmp%d' % jj
                                    tt('dve', ctmp[:, 2 * jj:2 * jj + 1], il, sn_, ALU.mult, r=[K('XI', jj), 'tabS%d' % jj], w=[tk_])
                                    tt('dve', ctmp[:, 2 * jj + 1:2 * jj + 2], il, cn, ALU.mult, r=[K('XI', jj), 'tabC%d' % jj, tk_], w=[tk_])
                                    stt('dve', carry[:, 2 * dj:2 * dj + 1], rl, cn, ctmp[:, 2 * jj:2 * jj + 1], ALU.mult, ALU.subtract,
                                        r=[K('XR', jj), 'tabC%d' % jj, tk_, ck], w=[ck])
                                    stt('dve', carry[:, 2 * dj + 1:2 * dj + 2], rl, sn_, ctmp[:, 2 * jj + 1:2 * jj + 2], ALU.mult, ALU.add,
                                        r=[K('XR', jj), 'tabS%d' % jj, tk_, ck], w=[ck])
                            if kind == 'own':
                                for jj in range(4):
                                    E = ENGJ[jj]; u = ub[jj]
                                    tt(E, u['M1'][:, 0:n], u['XR'][:, 0:n], tabC[jj][:, 0:n], ALU.mult, r=[K('XR', jj), 'tabC%d' % jj], w=[K('M1', jj)])
                                    tt(E, u['M2'][:, 0:n], u['XI'][:, 0:n], tabS[jj][:, 0:n], ALU.mult, r=[K('XI', jj), 'tabS%d' % jj], w=[K('M2', jj)])
                                for jj in range(4):
                                    E = ENGJ[jj]; u = ub[jj]
                                    tt(E, sre[sslot][jj][:], u['M1'][:, 0:n], u['M2'][:, 0:n], ALU.subtract, r=[K('M1', jj), K('M2', jj)], w=[('sre', sslot, jj)])
                                for jj in range(4):
                                    E = ENGJ[jj]; u = ub[jj]
                                    tt(E, u['M1'][:, 0:n], u['XR'][:, 0:n], tabS[jj][:, 0:n], ALU.mult, r=[K('XR', jj), 'tabS%d' % jj], w=[K('M1', jj)])
                                    tt(E, u['M2'][:, 0:n], u['XI'][:, 0:n], tabC[jj][:, 0:n], ALU.mult, r=[K('XI', jj), 'tabC%d' % jj], w=[K('M2', jj)])
                                for jj in range(4):
                                    E = ENGJ[jj]; u = ub[jj]
                                    tt(E, sim[sslot][jj][:], u['M1'][:, 0:n], u['M2'][:, 0:n], ALU.add, r=[K('M1', jj), K('M2', jj)], w=[('sim', sslot, jj)])
                                psy, pyk = nps()
                                for jj in range(4):
                                    dj = d * 16 + ct * 4 + jj
                                    a_re = sre[sslot][jj][:]; a_im = sim[sslot][jj][:]
                                    if rv:
                                        a_re = rev(a_re); a_im = rev(a_im)
                                    mm(psy[:], Cm[:, dj * 2, :], a_re, jj == 0, False, r=['Cm', ('sre', sslot, jj)], w=[pyk])
                                    mm(psy[:], Cm[:, dj * 2 + 1, :], a_im, False, jj == 3, r=['Cm', ('sim', sslot, jj)], w=[pyk])
                                ysl = yacc[:, ct, c * 512:(c + 1) * 512]
                                if d == 0:
                                    stt('dve', ysl, sT_own[:, ct, c * 512:(c + 1) * 512], dcol[:, ct:ct + 1], psy[:], ALU.mult, ALU.add,
                                        r=[pyk, ('sT_own', c), 'dcol'], w=[('yacc', ct, c)])
                                else:
                                    tt('dve', ysl, ysl, psy[:], ALU.add, r=[pyk, ('yacc', ct, c)], w=[('yacc', ct, c)])
            es13.__exit__(None, None, None)
            OPEN.remove(es13)
            S.barrier()
            debug_out('dbg_y', yacc[:, :, :], [('yacc', a, b) for a in range(4) for b in range(4)], 'sp')
            checkpoint('p3')

            GELUENG = _cfg('GELUENG', 'dve'); MTENG = _cfg('MTENG', 'dve'); LN1ENG = _cfg('LN1ENG', 'dve')
            HMENG = _cfg('HMENG', 'dve'); HBENG = _cfg('HBENG', 'dve')
            with contextlib.ExitStack() as es4:
                def t4(name, shape, dt=F32):
                    return es4.enter_context(nc.sbuf_tensor('s_' + name, list(shape), dt))
                wglu = t4('wglu', [128, 4, 512], BF16); wso = t4('wso', [128, 4, DM], BF16); wao = t4('wao', [128, 4, DM], BF16)
                wo = t4('wo', [128, 8, DM], BF16); wr = t4('wr', [128, 8, NE]); brt = t4('brt', [1, NE])
                dma('pool', wglu[:], D['w_glu'].rearrange("(k p) c -> p k c", p=128), 'c_wglu', w=['wglu'])
                dma('pool', wso[:], D['w_ssm_out'].rearrange("(k p) c -> p k c", p=128), 'c_wso', w=['wso'])
                dma('pool', wao[:], D['w_att_out'].rearrange("(k p) c -> p k c", p=128), 'c_wao', w=['wao'])
                dma('pool', wo[:], D['w_o'].rearrange("(k p) c -> p k c", p=128), 'c_wo', w=['wo'])
                dma('sp', wr[:], D['w_router'].rearrange("(k p) c -> p k c", p=128), 'c_wr', w=['wr'])
                dma('sp', brt[:], D['b_router'], 'c_br', w=['brt'])
                l1g = t4('l1g', [128, DM]); l1b = t4('l1b', [128, DM])
                dma('sp', l1g[:], pbc(D['ln1_g']), 'c_l1g', w=['l1g'])
                dma('sp', l1b[:], pbc(D['ln1_b']), 'c_l1b', w=['l1b'])
                sgs = [t4('sgs%d' % i, [128, 8, 512], BF16) for i in range(1)]
                sga = [t4('sga%d' % i, [128, 8, 512], BF16) for i in range(1)]
                oTc = [t4('oTc%d' % i, [128, 4, 512], BF16) for i in range(2)]
                g_t = [t4('g_t%d' % i, [128, 512]) for i in range(2)]
                g_w = [t4('g_w%d' % i, [128, 512]) for i in range(2)]
                zT = t4('zT', [128, 4, 512], BF16); z2T = t4('z2T', [128, 4, 512], BF16)
                sgl = [t4('sgl%d' % i, [128, 512], BF16) for i in range(2)]
                m1 = [t4('m1_%d' % i, [128, 512]) for i in range(2)]
                m2 = [t4('m2_%d' % i, [128, 512]) for i in range(2)]
                mT = t4('mT', [128, 8, 512], BF16)
                hr = [t4('hr%d' % i, [128, DM]) for i in range(2)]
                tk = [t4('tk%d' % i, [128, DM]) for i in range(2)]
                hmf = [t4('hmf%d' % i, [128, 8, 128]) for i in range(2)]
                hmb = [t4('hmb%d' % i, [128, 8, 128], BF16) for i in range(2)]
                st4 = [t4('st4_%d' % i, [128, 16]) for i in range(2)]
                lg = [t4('lg%d' % i, [128, NE]) for i in range(2)]
                mx = [t4('mx%d' % i, [128, 16]) for i in range(2)]
                msk = [t4('msk%d' % i, [128, NE]) for i in range(2)]
                P4STOP = _cfg('P4STOP', '')
                for c in range(4):
                    if P4STOP and c > 0:
                        break
                    cs_ = slice(c * 512, (c + 1) * 512)
                    b2 = 0
                    oc = oTc[c % 2]; ock = 'oTc%d' % (c % 2)
                    for kvh in range(2):
                        dma('sp', oc[:, kvh * 2:kvh * 2 + 2, :], D['o_scr'][kvh * 2:kvh * 2 + 2, :, cs_].rearrange("k p t -> p k t"),
                            ock, r=[('oscr', kvh, qt) for qt in range(c * 4, c * 4 + 4)], w=[ock])
                    for ot in range(8):
                        dma('sp', sgs[b2][:, ot, :], D['sgs_scr'][ot, :, cs_], 'sgsl%d' % b2, r=[('sgs', ot, c)], w=['sgs%d' % b2])
                        dma('sp', sga[b2][:, ot, :], D['sga_scr'][ot, :, cs_], 'sgal%d' % b2, r=[('sga', ot, c)], w=['sga%d' % b2])
                    if P4STOP == 'loads':
                        break
                    for ct in range(4):
                        i = ct % 2
                        y = yacc[:, ct, cs_]; yk = ('yacc', ct, c)
                        tt(GELUENG, g_t[i][:], y, y, ALU.mult, r=[yk], w=['g_t%d' % i])
                        ts(GELUENG, g_t[i][:], g_t[i][:], 0.044715, ALU.mult, r=['g_t%d' % i], w=['g_t%d' % i], s2=1.0, op1=ALU.add)
                        tt(GELUENG, g_w[i][:], g_t[i][:], y, ALU.mult, r=['g_t%d' % i, yk], w=['g_w%d' % i])
                        act(g_w[i][:], g_w[i][:], AF.Sigmoid, r=['g_w%d' % i], w=['g_w%d' % i], scale=1.5957691216057308)
                        tt(GELUENG, zT[:, ct, :], g_w[i][:], y, ALU.mult, r=['g_w%d' % i, yk], w=[('zT', ct)])
                    if P4STOP == 'gelu':
                        break
                    for ct in range(4):
                        ps, pk = nps()
                        for k in range(4):
                            mm(ps[:], wglu[:, k, ct * 128:(ct + 1) * 128], zT[:, k, :], k == 0, k == 3, r=['wglu', ('zT', k)], w=[pk])
                        i = ct % 2
                        act(sgl[i][:], ps[:], AF.Sigmoid, r=[pk, 'bgluT'], w=['sgl%d' % i], bias=bgluT[:, ct:ct + 1])
                        tt('dve', z2T[:, ct, :], zT[:, ct, :], sgl[i][:], ALU.mult, r=[('zT', ct), 'sgl%d' % i], w=[('z2T', ct)])
                    if P4STOP == 'glu':
                        break
                    for ot in range(8):
                        i = ot % 2
                        psa, pak = nps()
                        for k in range(4):
                            mm(psa[:], wso[:, k, ot * 128:(ot + 1) * 128], z2T[:, k, :], k == 0, k == 3, r=['wso', ('z2T', k)], w=[pak])
                        psb_, pbk = nps()
                        for k in range(4):
                            mm(psb_[:], wao[:, k, ot * 128:(ot + 1) * 128], oc[:, k, :], k == 0, k == 3, r=['wao', ock], w=[pbk])
                        tt('dve', m1[i][:], psa[:], sgs[b2][:, ot, :], ALU.mult, r=[pak, 'sgs%d' % b2], w=['m1_%d' % i])
                        tt('dve', m2[i][:], psb_[:], sga[b2][:, ot, :], ALU.mult, r=[pbk, 'sga%d' % b2], w=['m2_%d' % i])
                        tt(MTENG, mT[:, ot, :], m1[i][:], m2[i][:], ALU.add, r=['m1_%d' % i, 'm2_%d' % i], w=[('mT', ot)])
                    if P4STOP == 'branch':
                        break
                    for t in range(4):
                        tg = c * 4 + t
                        i = tg % 2
                        row = tg * 128
                        h = hr[i]; hk = 'hr%d' % i
                        x = tk[i]; xk = 'tk%d' % i
                        st = st4[i]; sk = 'st4_%d' % i
                        dma('sp', h[:], D['h_scr'][row:row + 128, :], hk, r=[('hscr', tg)], w=[hk])
                        for half in range(2):
                            ps, pk = nps()
                            for k in range(8):
                                mm(ps[:], mT[:, k, t * 128:(t + 1) * 128], wo[:, k, half * 512:(half + 1) * 512], k == 0, k == 7,
                                   r=[('mT', k), 'wo'], w=[pk])
                            tt('dve', x[:, half * 512:(half + 1) * 512], ps[:], G1[:, half * 512:(half + 1) * 512], ALU.mult,
                               r=[pk, 'modbc'], w=[xk])
                        stt('dve', x[:], h[:], ALPHA, x[:], ALU.mult, ALU.add, r=[hk, xk], w=[xk])
                        if P4STOP == 'mix':
                            continue
                        S.op('dve', lambda e, st=st, x=x: e.bn_stats(out=st[:, 0:6], in_=x[:, 0:512]), [xk], [sk])
                        S.op('dve', lambda e, st=st, x=x: e.bn_stats(out=st[:, 6:12], in_=x[:, 512:1024]), [xk, sk], [sk])
                        S.op('dve', lambda e, st=st: e.bn_aggr(out=st[:, 12:14], in_=st[:, 0:12]), [sk], [sk])
                        rstd(st, sk)
                        ts('dve', x[:], x[:], st[:, 12:13], ALU.subtract, r=[xk, sk], w=[xk], s2=st[:, 14:15], op1=ALU.mult)
                        tt(LN1ENG, x[:], x[:], l1g[:], ALU.mult, r=[xk, 'l1g'], w=[xk])
                        tt(LN1ENG, h[:], x[:], l1b[:], ALU.add, r=[xk, 'l1b', hk], w=[hk])
                        if P4STOP == 'ln':
                            continue
                        dma('sp', D['h1_scr'][row:row + 128, :], h[:], 'h1s%d' % (tg % 4), r=[hk], w=[('h1scr', tg)])
                        tt(HMENG, x[:], h[:], ONESC2, ALU.mult, r=[hk, 'modbc'], w=[xk])
                        tt(HMENG, x[:], x[:], SH2, ALU.add, r=[xk, 'modbc'], w=[xk])
                        if P4STOP == 'spill':
                            continue
                        hf_ = hmf[i]; hfk = 'hmf%d' % i
                        hb_ = hmb[i]; hbk = 'hmb%d' % i
                        for half in range(2):
                            ps, pk = nps()
                            for kk in range(4):
                                k = half * 4 + kk
                                tr(ps[:, kk * 128:(kk + 1) * 128], x[:, k * 128:(k + 1) * 128], identf[:], r=[xk, 'identf'], w=[pk])
                            cp('act', hf_[:, half * 4:(half + 1) * 4, :], ps[:].rearrange("p (a c) -> p a c", c=128), r=[pk], w=[hfk])
                        cp(HBENG, hb_[:], hf_[:], r=[hfk], w=[hbk])
                        for k in range(8):
                            dma('sp', D['hmT_scr'][k, :, row:row + 128], hb_[:, k, :], 'hmts%d' % i, r=[hbk], w=[('hmT', tg, k)])
                        if 'router' in _cfg('P4SKIP', ''):
                            mset('dve', gates[:, tg, :], 0.25, w=[('gates', tg)])
                            continue
                        ps, pk = nps()
                        for k in range(8):
                            mm(ps[:, 0:NE], hf_[:, k, :], wr[:, k, :], k == 0, False, r=[hfk, 'wr'], w=[pk])
                        mm(ps[:, 0:NE], ones_f[0:1, :], brt[0:1, :], False, True, r=['ones_f', 'brt'], w=[pk])
                        L = lg[i]; lk = 'lg%d' % i
                        M = mx[i]; mk_ = 'mx%d' % i
                        K_ = msk[i]; kk_ = 'msk%d' % i
                        cp('act', L[:], ps[:, 0:NE], r=[pk], w=[lk])
                        S.op('dve', lambda e, M=M, L=L: e.max(out=M[:, 0:8], in_=L[:]), [lk], [mk_])
                        ts('dve', K_[:], L[:], M[:, 3:4], ALU.is_ge, r=[lk, mk_], w=[kk_])
                        ts('dve', M[:, 8:9], M[:, 0:1], -1.0, ALU.mult, r=[mk_], w=[mk_])
                        act(L[:], L[:], AF.Exp, r=[lk, mk_], w=[lk], bias=M[:, 8:9])
                        tt('dve', L[:], L[:], K_[:], ALU.mult, r=[lk, kk_], w=[lk])
                        S.op('dve', lambda e, M=M, L=L: e.reduce_sum(out=M[:, 9:10], in_=L[:], axis=mybir.AxisListType.X), [lk, mk_], [mk_])
                        S.op('dve', lambda e, M=M: e.reciprocal(out=M[:, 10:11], in_=M[:, 9:10]), [mk_], [mk_])
                        ts('dve', gates[:, tg, :], L[:], M[:, 10:11], ALU.mult, r=[lk, mk_], w=[('gates', tg)])

            S.barrier()
            debug_out('dbg_gates', gates[:, :, :], [('gates', t) for t in range(16)], 'sp')
            debug_out('dbg_h1', D['h1_scr'], [('h1scr', t) for t in range(16)], 'sp')
            checkpoint('p4')
            finals = []
            with contextlib.ExitStack() as es5:
                def t5(name, shape, dt=F32):
                    return es5.enter_context(nc.sbuf_tensor('s_' + name, list(shape), dt))
                bguT = t5('bguT', [128, NE * 16]); bgu1 = t5('bgu1', [128, NE * 16])
                dma('sp', bguT[:], D['b_guT'], 'c_bgu', w=['bguT'])
                ts('pool', bgu1[:], bguT[:], 1.0, ALU.add, r=['bguT'], w=['bgu1'])
                l2g = t5('l2g', [128, DM]); l2b = t5('l2b', [128, DM])
                dma('sp', l2g[:], pbc(D['ln2_g']), 'c_l2g', w=['l2g'])
                dma('sp', l2b[:], pbc(D['ln2_b']), 'c_l2b', w=['l2b'])
                acc = yacc[:, :, :].rearrange('p a (b c) -> p (a b) c', c=1024)
                hmT = t5('hmT', [128, 8, 1024], BF16)
                NR = 8
                ring = [t5('ring%d' % i, [128, 8, 512], BF16) for i in range(NR)]
                bdall = t5('bdall', [NE, DM])
                dma('sp', bdall[:], D['b_down'], 'c_bdall', w=['bdall'])
                gTt = [t5('gTt%d' % i, [NE, 128]) for i in range(2)]
                actT = t5('actT', [128, 8, 1024], BF16)
                NGS = 4
                Gt = [t5('Gt%d' % i, [128, 512]) for i in range(NGS)]
                Lt = [t5('Lt%d' % i, [128, 512]) for i in range(NGS)]
                Sg = [t5('Sg%d' % i, [128, 512]) for i in range(NGS)]
                pend = []
                h1t = [t5('h1t%d' % i, [128, DM]) for i in range(2)]
                st6 = [t5('st6_%d' % i, [128, 16]) for i in range(2)]
                rctr = 0
                ectr = 0
                bctr = 0
                for half in range(2):
                    for k in range(8):
                        dma('sp', hmT[:, k, :], D['hmT_scr'][k, :, half * 1024:(half + 1) * 1024], 'hmTl',
                            r=[('hmT', tg, k) for tg in range(half * 8, half * 8 + 8)], w=['hmT'])
                    for t in range(8):
                        tg = half * 8 + t
                        gT = gTt[t % 2]; gTk = 'gTt%d' % (t % 2)
                        ps, pk = nps()
                        tr(ps[0:NE, 0:128], gates[:, tg, :], identf[:], r=[('gates', tg), 'identf'], w=[pk])
                        cp('act', gT[:], ps[0:NE, 0:128], r=[pk], w=[gTk])
                        for dh in range(2):
                            ps2, pk2 = nps()
                            mm(ps2[:], gT[:], bdall[:, dh * 512:(dh + 1) * 512], True, True, r=[gTk, 'bdall'], w=[pk2])
                            cp('act', acc[:, t, dh * 512:(dh + 1) * 512], ps2[:], r=[pk2], w=[('acc', t, dh)])
                    NODMA = bool(_cfg('MOE_NODMA'))
                    for e in range(NE):
                        if NODMA and (e > 0 or half > 0):
                            units = list(range(6))
                        wgu_v = D['w_gate_up'][e].rearrange("(k p) c -> p k c", p=128)
                        wd_v = D['w_down'][e].rearrange("(k p) c -> p k c", p=128)
                        units = [] if not (NODMA and (e > 0 or half > 0)) else units
                        for c in range(4):
                            if NODMA and (e > 0 or half > 0):
                                break
                            ri_ = rctr % NR
                            rctr += 1
                            dma('pool', ring[ri_][:, :, 0:256], wgu_v[:, :, c * 256:(c + 1) * 256], 'ringa%d' % ri_, w=[('ring', ri_, 0)])
                            dma('pool', ring[ri_][:, :, 256:512], wgu_v[:, :, 1024 + c * 256:1024 + (c + 1) * 256], 'ringb%d' % ri_, w=[('ring', ri_, 1)])
                            units.append(ri_)
                        for dh in range(2):
                            if NODMA and (e > 0 or half > 0):
                                break
                            ri_ = rctr % NR
                            rctr += 1
                            dma('pool', ring[ri_][:, :, 0:256], wd_v[:, :, dh * 512:dh * 512 + 256], 'ringa%d' % ri_, w=[('ring', ri_, 0)])
                            dma('pool', ring[ri_][:, :, 256:512], wd_v[:, :, dh * 512 + 256:(dh + 1) * 512], 'ringb%d' % ri_, w=[('ring', ri_, 1)])
                            units.append(ri_)
                        for c in range(4):
                            ru = units[c]
                            for sub in range(2):
                                fi = 2 * c + sub
                                for tch in range(2):
                                    tok = slice(tch * 512, (tch + 1) * 512)
                                    psg, pgk = nps()
                                    psl, plk = nps()
                                    for k in range(8):
                                        mm(psg[:], ring[ru][:, k, sub * 128:(sub + 1) * 128], hmT[:, k, tok], k == 0, k == 7,
                                           r=[('ring', ru, 0), 'hmT'], w=[pgk])
                                    for k in range(8):
                                        mm(psl[:], ring[ru][:, k, 256 + sub * 128:256 + (sub + 1) * 128], hmT[:, k, tok], k == 0, k == 7,
                                           r=[('ring', ru, 1), 'hmT'], w=[plk])
                                    i = ectr % NGS
                                    ectr += 1
                                    G = Gt[i]; gk = 'Gt%d' % i
                                    L = Lt[i]; lk = 'Lt%d' % i
                                    Sg_ = Sg[i]; sgk = 'Sg%d' % i
                                    ts('dve', G[:], psg[:], bguT[:, e * 16 + fi:e * 16 + fi + 1], ALU.add, r=[pgk, 'bguT'], w=[gk], s2=7.0, op1=ALU.min)
                                    if not _cfg('MOE_SKIPL'):
                                        ts('dve', L[:], psl[:], bgu1[:, e * 16 + 8 + fi:e * 16 + 8 + fi + 1], ALU.add, r=[plk, 'bgu1'], w=[lk], s2=8.0, op1=ALU.min)
                                    act(Sg_[:], G[:], AF.Sigmoid, r=[gk], w=[sgk], scale=1.702)
                                    tt(_cfg('MOEGENG', 'pool'), G[:], G[:], Sg_[:], ALU.mult, r=[gk, sgk], w=[gk])
                                    pend.append((actT[:, fi, tok], L[:], G[:], lk, gk, ('actT', fi, tch)))
                                    if len(pend) > 2:
                                        o_, l_, g_, lk_, gk_, ak_ = pend.pop(0)
                                        stt('dve', o_, l_, -6.0, g_, ALU.max, ALU.mult, r=[lk_, gk_], w=[ak_])
                        while pend:
                            o_, l_, g_, lk_, gk_, ak_ = pend.pop(0)
                            stt('dve', o_, l_, -6.0, g_, ALU.max, ALU.mult, r=[lk_, gk_], w=[ak_])
                        for t in range(8):
                            tg = half * 8 + t
                            tch = t // 4
                            for dh in range(2):
                                ru = units[4 + dh]
                                ps, pk = nps()
                                for k in range(8):
                                    mm(ps[:], actT[:, k, t * 128:(t + 1) * 128], ring[ru][:, k, :], k == 0, k == 7,
                                       r=[('actT', k, tch), ('ring', ru, 0), ('ring', ru, 1)], w=[pk])
                                asl = acc[:, t, dh * 512:(dh + 1) * 512]
                                ak = ('acc', t, dh)
                                stt('dve', asl, ps[:], gates[:, tg, e:e + 1], asl, ALU.mult, ALU.add, r=[pk, ('gates', tg), ak], w=[ak])
                    for t in range(8):
                        tg = half * 8 + t
                        i = tg % 2
                        row = tg * 128
                        h = h1t[i]; hk = 'h1t%d' % i
                        st = st6[i]; sk = 'st6_%d' % i
                        x = acc[:, t, :]
                        xk0 = ('acc', t, 0); xk1 = ('acc', t, 1)
                        dma('sp', h[:], D['h1_scr'][row:row + 128, :], hk, r=[('h1scr', tg)], w=[hk])
                        tt(_cfg('LN2ENG', 'dve'), x, x, G2, ALU.mult, r=[xk0, xk1, 'modbc'], w=[xk0, xk1])
                        stt('dve', x, h[:], ALPHA, x, ALU.mult, ALU.add, r=[hk, xk0, xk1], w=[xk0, xk1])
                        S.op('dve', lambda e, st=st, x=x: e.bn_stats(out=st[:, 0:6], in_=x[:, 0:512]), [xk0, xk1], [sk])
                        S.op('dve', lambda e, st=st, x=x: e.bn_stats(out=st[:, 6:12], in_=x[:, 512:1024]), [xk0, xk1, sk], [sk])
                        S.op('dve', lambda e, st=st: e.bn_aggr(out=st[:, 12:14], in_=st[:, 0:12]), [sk], [sk])
                        rstd(st, sk)
                        ts('dve', x, x, st[:, 12:13], ALU.subtract, r=[xk0, xk1, sk], w=[xk0, xk1], s2=st[:, 14:15], op1=ALU.mult)
                        tt('dve', x, x, l2g[:], ALU.mult, r=[xk0, xk1, 'l2g'], w=[xk0, xk1])
                        tt(_cfg('LN2ENG', 'dve'), x, x, l2b[:], ALU.add, r=[xk0, xk1, 'l2b'], w=[xk0, xk1])
                        ev = dma('sp', D['out'][row:row + 128, :], x, 'outs%d' % i, r=[xk0, xk1])
                        finals.append(ev)
            return finals

        try:
            finals_ = body()
        except _Stop:
            finals_ = []
            for st_ in reversed(OPEN):
                st_.__exit__(None, None, None)
        finish(finals_)
    return nc


def _rope_tables(local_real):
    pos = np.asarray(local_real, dtype=np.int64)
    row = (pos // 64).astype(np.float64)
    col = (pos % 64).astype(np.float64)
    inv = 10000.0 ** (-np.arange(16, dtype=np.float64) / 16.0)
    ang = np.concatenate([row[None, :] * inv[:, None], col[None, :] * inv[:, None]], axis=0)
    c = np.cos(ang); s = np.sin(ang)
    C = np.concatenate([c, c, c, c], axis=0)
    Sg = np.concatenate([s, -s, s, -s], axis=0)
    return C.astype(np.float32), Sg.astype(np.float32)


def _make_inputs(inp, r):
    b, hf = r // 2, r % 2
    f = np.float32
    x = inp['x'][b]
    L = np.arange(4096) if hf == 0 else np.arange(4095, -1, -1)
    own = L[:2048]
    oth = L[2048:][::-1]
    m = {}
    m['x_own'] = np.ascontiguousarray(x[own])
    m['x_oth'] = np.ascontiguousarray(x[oth])
    cx = inp['ctx'][b]
    m['ctxb'] = np.ascontiguousarray(cx if hf == 0 else cx[::-1])
    cv = np.zeros((128, 8, 2), f)
    cv[:, :, 0] = inp['c'][b].reshape(8, 128).T
    cv[:, :, 1] = inp['c_ctx'].reshape(8, 128).T
    m['cvec'] = cv.reshape(128, 16)
    m['ln_in_g'] = inp['ln_in_g'].reshape(1, -1); m['ln_in_b'] = inp['ln_in_b'].reshape(1, -1)
    m['w_mod'] = inp['w_mod'][0]
    m['b_modT'] = np.ascontiguousarray(inp['b_mod'][0].reshape(48, 128).T)
    m['b_mod_row'] = inp['b_mod'][0].reshape(1, -1)
    m['w_in'] = inp['w_in'][0]
    dsel = [0, 1] if hf == 0 else [1, 0]

    def sp_layout(a):
        a = a[dsel].reshape(2, 16, 2, 64)
        return np.ascontiguousarray(a.transpose(2, 3, 0, 1).reshape(128, 32))
    m['lamre'] = sp_layout(inp['ssm_lam_re'][0]); m['lamim'] = sp_layout(inp['ssm_lam_im'][0])
    m['lstep'] = sp_layout(np.broadcast_to(inp['ssm_log_step'][0][:, :, None], (2, 32, 64)))

    def b_layout(a):
        a = a[dsel].reshape(2, 16, 2, 64, 16)
        return np.ascontiguousarray(a.transpose(2, 3, 0, 1, 4).reshape(128, 512))
    m['bre'] = b_layout(inp['ssm_b_re'][0]); m['bim'] = b_layout(inp['ssm_b_im'][0])

    def c_layout(a):
        a = a[dsel].reshape(2, 4, 8, 16, 64)
        return np.ascontiguousarray(a.transpose(2, 3, 0, 1, 4).reshape(128, 512))
    m['cre'] = c_layout(inp['ssm_c_re'][0]); m['cim'] = c_layout(inp['ssm_c_im'][0])
    m['dcol'] = np.ascontiguousarray(inp['ssm_d'][0].reshape(4, 128).T)
    gl = np.arange(128) // 16
    par = np.zeros((128, 4), f)
    par[:, 0] = (gl % 2 == 0); par[:, 1] = (gl % 2 == 1); par[:, 2] = -par[:, 0]; par[:, 3] = -par[:, 1]
    m['par'] = par
    m['w_glu'] = inp['w_glu'][0]
    m['b_gluT'] = np.ascontiguousarray(inp['b_glu'][0].reshape(4, 128).T)
    m['sinkrow'] = np.ascontiguousarray(np.repeat(inp['attn_sink'][0][[0, 2, 1, 3, 4, 6, 5, 7]], 128).reshape(1, 1024))
    m['w_ssm_out'] = inp['w_ssm_out'][0]; m['w_att_out'] = inp['w_att_out'][0]; m['w_o'] = inp['w_o'][0]
    m['ln1_g'] = inp['ln1_g'][0].reshape(1, -1); m['ln1_b'] = inp['ln1_b'][0].reshape(1, -1)
    m['w_router'] = inp['w_router'][0]; m['b_router'] = inp['b_router'][0].reshape(1, -1)
    m['w_gate_up'] = inp['w_gate_up'][0]
    m['b_guT'] = np.ascontiguousarray(inp['b_gate_up'][0].reshape(32, 16, 128).transpose(2, 0, 1).reshape(128, 512))
    m['w_down'] = inp['w_down'][0]; m['b_down'] = inp['b_down'][0]
    m['ln2_g'] = inp['ln2_g'][0].reshape(1, -1); m['ln2_b'] = inp['ln2_b'][0].reshape(1, -1)
    m['ident'] = np.eye(128, dtype=f)
    halo = oth[1920:2048]
    C, Sg = _rope_tables(np.concatenate([own, halo]))
    m['ropeC'] = C; m['ropeS'] = Sg
    ki = np.arange(128)[:, None]; qi = np.arange(128)[None, :]
    mk = np.zeros((128, 3, 128), f)
    mk[:, 0] = (qi <= ki); mk[:, 1] = (ki <= qi); mk[:, 2] = (ki + qi >= 127)
    m['masks'] = mk.reshape(128, 384)
    m['iota1'] = np.ascontiguousarray(np.broadcast_to(np.arange(1, 513, dtype=f)[None, :], (128, 512)))
    return {k: np.ascontiguousarray(v, dtype=f) for k, v in m.items()}


_NC_CACHE = {}


def kernel(**inputs):
    inp = {k: np.asarray(v) for k, v in inputs.items()}
    if 'nc' not in _NC_CACHE:
        _NC_CACHE['nc'] = build_nc()
    nc = _NC_CACHE['nc']
    in_maps = [_make_inputs(inp, r) for r in range(8)]
    res = run_bass_kernel_spmd(nc, in_maps, core_ids=list(range(8)))
    out = np.zeros((4, 4096, 1024), np.float32)
    for r in range(8):
        b, hf = r // 2, r % 2
        o = np.asarray(res.results[r]['out'])
        if hf == 0:
            out[b, :2048] = o
        else:
            out[b, 2048:] = o[::-1]
    return out
```

```python
import math
import contextlib
import numpy as np
import concourse.bass as bass
import concourse.mybir as mybir
from concourse.bass_utils import run_bass_kernel_spmd

F32 = mybir.dt.float32
BF16 = mybir.dt.bfloat16
ALU = mybir.AluOpType
AF = mybir.ActivationFunctionType

NTOK = 2048
DM = 1024
NE = 32
TWO_PI = 2.0 * math.pi
MAGIC = 12582912.0
ALPHA = 2.0 ** 0.25
LN_EPS = 1e-5
_CFG = {}


def _cfg(name, default=None):
    return _CFG.get(name, default)


class Sched:
    ENG = ('pe', 'dve', 'act', 'pool', 'sp')

    def __init__(self, nc):
        self.nc = nc
        self.ops = {e: [] for e in self.ENG}
        self.state = {}
        self.dma_cnt = {}
        self.nsig = {}
        self.barrier_deps = set()

    def barrier(self):
        b = set()
        for e in self.ENG:
            for idx in range(len(self.ops[e]) - 1, -1, -1):
                if self.ops[e][idx]['dma'] is None:
                    b.add(('c', e, idx))
                    break
        for sname, c in self.dma_cnt.items():
            b.add(('d', sname, c))
        self.barrier_deps = b

    def _deps(self, reads, writes):
        deps = set()
        for k in writes:
            if k not in self.state:
                deps.update(self.barrier_deps)
        for k in reads:
            st = self.state.get(k)
            if st and st[0] is not None:
                deps.add(st[0])
        for k in writes:
            st = self.state.get(k)
            if st:
                if st[0] is not None:
                    deps.add(st[0])
                deps.update(st[1])
        return deps

    def _update(self, ev, reads, writes):
        for k in reads:
            st = self.state.setdefault(k, [None, []])
            st[1].append(ev)
        for k in writes:
            self.state[k] = [ev, []]

    def op(self, eng, fn, reads=(), writes=()):
        deps = self._deps(reads, writes)
        idx = len(self.ops[eng])
        ev = ('c', eng, idx)
        if eng == 'pe':
            deps = {d for d in deps if not (d[0] == 'c' and d[1] == 'pe')}
        self.ops[eng].append(dict(fn=fn, deps=deps, ev=ev, sig=False, dma=None))
        self._update(ev, reads, writes)
        return ev

    def dma(self, eng, fn, sem, reads=(), writes=()):
        deps = self._deps(reads, writes)
        c = self.dma_cnt.get(sem, 0) + 1
        self.dma_cnt[sem] = c
        if c > 1:
            deps.add(('d', sem, c - 1))
        ev = ('d', sem, c)
        self.ops[eng].append(dict(fn=fn, deps=deps, ev=ev, sig=False, dma=sem))
        self._update(ev, reads, writes)
        return ev

    def emit(self, final_waits=()):
        nc = self.nc
        for e in self.ENG:
            for o in self.ops[e]:
                for d in o['deps']:
                    if d[0] == 'c':
                        self.ops[d[1]][d[2]]['sig'] = True
        for e in self.ENG:
            n = 0
            for o in self.ops[e]:
                if o['dma'] is None and o['sig']:
                    n += 1
                    o['n'] = n
            self.nsig[e] = n
        with contextlib.ExitStack() as es:
            csem = {e: es.enter_context(nc.semaphore('c_' + e)) for e in self.ENG if self.nsig[e] > 0}
            dsem = {s: es.enter_context(nc.semaphore('d_' + str(s))) for s in self.dma_cnt}
            block = es.enter_context(nc.Block())
            ops = self.ops

            def run(e, engobj):
                waited = {}
                for o in ops[e]:
                    need = {}
                    for d in o['deps']:
                        if d[0] == 'c':
                            tgt = ops[d[1]][d[2]]['n']
                            key = ('c', d[1])
                        else:
                            tgt = 16 * d[2]
                            key = ('d', d[1])
                        if tgt > need.get(key, 0):
                            need[key] = tgt
                    todo = []
                    for key in sorted(need):
                        tgt = need[key]
                        if waited.get(key, 0) >= tgt:
                            continue
                        waited[key] = tgt
                        todo.append((csem[key[1]] if key[0] == 'c' else dsem[key[1]], tgt))
                    attach = None
                    if todo and _cfg('ATTACH', True) and o['dma'] is None:
                        attach = todo.pop()
                    for sem_, tgt in todo:
                        engobj.wait_ge(sem_, tgt)
                    ins = o['fn'](engobj)
                    if attach is not None:
                        ins._wait_ge(attach[0], attach[1])
                    if o['dma'] is not None:
                        ins.then_inc(dsem[o['dma']], 16)
                    elif o['sig']:
                        ins.then_inc(csem[e], 1)
                if e == 'sp':
                    for ev in final_waits:
                        engobj.wait_ge(dsem[ev[1]], 16 * ev[2])

            @block.tensor
            def _(pe):
                run('pe', pe)

            @block.vector
            def _(v):
                run('dve', v)

            @block.scalar
            def _(a):
                run('act', a)

            @block.gpsimd
            def _(g):
                run('pool', g)

            @block.sync
            def _(s):
                run('sp', s)


class _Stop(Exception):
    pass


def build_nc(dbg=(), stop=None):
    nc = bass.Bass("TRN2", target_bir_lowering=False)
    S = Sched(nc)
    D = {}

    def din(name, shape, dt=F32):
        D[name] = nc.dram_tensor(name, list(shape), dt, kind="ExternalInput").ap()

    def dscr(name, shape, dt):
        D[name] = nc.dram_tensor(name, list(shape), dt, kind="Internal").ap()

    def dout(name, shape, dt=F32):
        D[name] = nc.dram_tensor(name, list(shape), dt, kind="ExternalOutput").ap()

    din('x_own', [NTOK, DM]); din('x_oth', [NTOK, DM]); din('ctxb', [256, DM])
    din('cvec', [128, 16]); din('ln_in_g', [1, DM]); din('ln_in_b', [1, DM])
    din('w_mod', [DM, 6144]); din('b_modT', [128, 48]); din('b_mod_row', [1, 6144])
    din('w_in', [DM, 3328])
    din('lamre', [128, 32]); din('lamim', [128, 32]); din('lstep', [128, 32])
    din('bre', [128, 512]); din('bim', [128, 512]); din('cre', [128, 512]); din('cim', [128, 512])
    din('dcol', [128, 4]); din('par', [128, 4])
    din('w_glu', [512, 512]); din('b_gluT', [128, 4]); din('sinkrow', [1, 1024])
    din('w_ssm_out', [512, DM]); din('w_att_out', [512, DM]); din('w_o', [DM, DM])
    din('ln1_g', [1, DM]); din('ln1_b', [1, DM]); din('w_router', [DM, NE]); din('b_router', [1, NE])
    din('w_gate_up', [NE, DM, 2048]); din('b_guT', [128, NE * 16]); din('w_down', [NE, DM, DM]); din('b_down', [NE, DM])
    din('ln2_g', [1, DM]); din('ln2_b', [1, DM])
    din('ident', [128, 128]); din('ropeC', [128, 2176]); din('ropeS', [128, 2176])
    din('masks', [128, 384]); din('iota1', [128, 512])
    dscr('h_scr', [NTOK, DM], F32); dscr('h1_scr', [NTOK, DM], F32)
    dscr('sgs_scr', [8, 128, NTOK], BF16); dscr('sga_scr', [8, 128, NTOK], BF16)
    dscr('soth_scr', [4, 128, NTOK], BF16); dscr('hmT_scr', [8, 128, NTOK], BF16); dscr('o_scr', [4, 128, NTOK], BF16)
    dout('out', [NTOK, DM])
    for item in dbg:
        dout(item[0], item[1], item[2] if len(item) > 2 else F32)

    es = contextlib.ExitStack()
    with es:
        def sb(name, shape, dt=F32):
            return es.enter_context(nc.sbuf_tensor('s_' + name, list(shape), dt))

        psb = [es.enter_context(nc.psum_tensor('psb%d' % i, [128, 512], F32)) for i in range(8)]
        psctr = [0]

        def nps():
            i = psctr[0] % 8
            psctr[0] += 1
            return psb[i], 'ps%d' % i

        def dma(eng, out, in_, sem, r=(), w=()):
            return S.dma(eng, lambda e: e.dma_start(out=out, in_=in_), sem, r, w)

        def mm(out, lhsT, rhs, start, stop, r=(), w=()):
            S.op('pe', lambda e: e.matmul(out, lhsT=lhsT, rhs=rhs, start=start, stop=stop), r, w)

        def tr(out, in_, ident, r=(), w=()):
            S.op('pe', lambda e: e.transpose(out, in_, ident), r, w)

        def act(out, in_, func, r=(), w=(), bias=None, scale=None, eng='act'):
            kw = {}
            if bias is not None:
                kw['bias'] = bias
            if scale is not None:
                kw['scale'] = scale
            S.op(eng, lambda e: e.activation(out=out, in_=in_, func=func, **kw), r, w)

        def tt(eng, out, in0, in1, op, r=(), w=()):
            S.op(eng, lambda e: e.tensor_tensor(out=out, in0=in0, in1=in1, op=op), r, w)

        def ts(eng, out, in0, s1, op0, r=(), w=(), s2=None, op1=None):
            if op1 is None:
                S.op(eng, lambda e: e.tensor_scalar(out=out, in0=in0, scalar1=s1, scalar2=None, op0=op0), r, w)
            else:
                S.op(eng, lambda e: e.tensor_scalar(out=out, in0=in0, scalar1=s1, scalar2=s2, op0=op0, op1=op1), r, w)

        def stt(eng, out, in0, scalar, in1, op0, op1, r=(), w=()):
            S.op(eng, lambda e: e.scalar_tensor_tensor(out=out, in0=in0, scalar=scalar, in1=in1, op0=op0, op1=op1), r, w)

        def cp(eng, out, in_, r=(), w=()):
            if eng == 'act':
                S.op(eng, lambda e: e.copy(out=out, in_=in_), r, w)
            else:
                S.op(eng, lambda e: e.tensor_copy(out=out, in_=in_), r, w)

        def mset(eng, ap, val, w=()):
            S.op(eng, lambda e: e.memset(ap, val), (), w)

        def scan(eng, out, d0, d1, init, r=(), w=()):
            S.op(eng, lambda e: e.tensor_tensor_scan(out=out, data0=d0, data1=d1, initial=init,
                                                     op0=ALU.mult, op1=ALU.add), r, w)

        def pbc(ap):
            return ap.partition_broadcast(128)

        CONST = {}
        OPEN = []

        def rstd(st, sk):
            act(st[:, 14:15], st[:, 13:14], AF.Sqrt, r=[sk, 'epsT'], w=[sk], bias=CONST['epsT'][:, 0:1])
            S.op('dve', lambda e: e.reciprocal(out=st[:, 14:15], in_=st[:, 14:15]), [sk], [sk])

        def debug_out(name, src_ap, keys, eng='pool'):
            if any(it[0] == name for it in dbg):
                dma(eng, D[name], src_ap, 'dbg_' + name, r=list(keys))

        def finish(finals=()):
            fw = {}
            for ev in finals:
                fw[ev[1]] = max(fw.get(ev[1], 0), ev[2])
            for it in dbg:
                name = it[0]
                s_ = 'dbg_' + name
                if s_ in S.dma_cnt:
                    fw[s_] = S.dma_cnt[s_]
            S.emit(final_waits=[('d', k, v) for k, v in fw.items()])

        def checkpoint(tag):
            if stop == tag:
                raise _Stop()

        def body():
            identf = sb('identf', [128, 128])
            dma('sp', identf[:], D['ident'], 'c_ident', w=['identf'])
            halfpi = sb('halfpi', [128, 1])
            mset('dve', halfpi[:], 0.5 * math.pi, w=['halfpi'])
            epsT = sb('epsT', [128, 1])
            mset('dve', epsT[:], LN_EPS, w=['epsT'])
            CONST['epsT'] = epsT
            ones_f = sb('ones_f', [128, 128])
            mset('dve', ones_f[:], 1.0, w=['ones_f'])
            ones_b = sb('ones_b', [128, 128], BF16)
            mset('dve', ones_b[:], 1.0, w=['ones_b'])
            iota1 = sb('iota1', [128, 512])
            dma('sp', iota1[:], D['iota1'], 'c_iota', w=['iota1'])
            masks = sb('masks', [128, 384], BF16)
            dma('pool', masks[:], D['masks'], 'c_masks', w=['masks'])
            dcol = sb('dcol', [128, 4])
            dma('sp', dcol[:], D['dcol'], 'c_dcol', w=['dcol'])
            par = sb('par', [128, 4])
            dma('sp', par[:], D['par'], 'c_par', w=['par'])
            bgluT = sb('bgluT', [128, 4])
            dma('sp', bgluT[:], D['b_gluT'], 'c_bglu', w=['bgluT'])
            esink = sb('esink', [1, 1024], BF16)
            with nc.sbuf_tensor('s_sinkf', [1, 1024], F32) as sinkf:
                dma('sp', sinkf[:], D['sinkrow'], 'c_sink', w=['sinkf'])
                act(esink[:], sinkf[:], AF.Exp, r=['sinkf'], w=['esink'])
            S.barrier()

            cv = sb('cv', [128, 16])
            dma('sp', cv[:], D['cvec'], 'c_cv', w=['cv'])
            sc_ = sb('sc_', [128, 16])
            act(sc_[:], cv[:], AF.Silu, r=['cv'], w=['sc_'])
            sc3 = sc_[:].rearrange("p (k w) -> p k w", w=2)
            bmodT = sb('bmodT', [128, 48])
            dma('sp', bmodT[:], D['b_modT'], 'c_bmodT', w=['bmodT'])
            modT = sb('modT', [128, 32])
            modbc = sb('modbc', [128, 4096])
            dma('sp', modbc[:], pbc(D['b_mod_row'][:, 2048:6144]), 'c_modbc', w=['modbc'])
            wm_v = D['w_mod'].rearrange("(k p) c -> p k c", p=128)
            with contextlib.ExitStack() as es0:
                sbc = es0.enter_context(nc.sbuf_tensor('s_sbc', [128, 8, 128], BF16))
                sbcx = es0.enter_context(nc.sbuf_tensor('s_sbcx', [128, 8, 128], BF16))
                tmpd = es0.enter_context(nc.sbuf_tensor('s_tmpd', [128, 128], F32))
                mT3 = modT[:].rearrange("p (c w) -> p c w", w=2)
                for k in range(8):
                    cp('dve', sbc[:, k, :], sc3[:, k, 0:1].to_broadcast([128, 128]), r=['sc_'], w=['sbc'])
                    cp('dve', sbcx[:, k, :], sc3[:, k, 1:2].to_broadcast([128, 128]), r=['sc_'], w=['sbcx'])
                wmb = [es0.enter_context(nc.sbuf_tensor('s_wmb%d' % i, [128, 8, 512], BF16)) for i in range(3)]
                for i in range(12):
                    wb = wmb[i % 3]
                    wk = 'wmb%d' % (i % 3)
                    dma('pool', wb[:], wm_v[:, :, i * 512:(i + 1) * 512], wk, w=[wk])
                    if i < 4:
                        for which, sbw, sbk in ((0, sbc, 'sbc'), (1, sbcx, 'sbcx')):
                            ps, pk = nps()
                            for k in range(8):
                                mm(ps[:], sbw[:, k, :], wb[:, k, :], k == 0, k == 7, r=[wk, sbk], w=[pk])
                            for ctl in range(4):
                                ct = i * 4 + ctl
                                tt('dve', tmpd[:], ps[:, ctl * 128:(ctl + 1) * 128], identf[:], ALU.mult, r=[pk, 'identf'], w=['tmpd'])
                                S.op('dve', lambda e, ct=ct, which=which: e.reduce_sum(out=mT3[:, ct, which:which + 1], in_=tmpd[:],
                                                                                     axis=mybir.AxisListType.X), ['tmpd'], ['modT'])
                    else:
                        ps, pk = nps()
                        for k in range(8):
                            mm(ps[:], sbc[:, k, :], wb[:, k, :], k == 0, k == 7, r=[wk, 'sbc'], w=[pk])
                        c0 = (i - 4) * 512
                        tt('dve', modbc[:, c0:c0 + 512], ps[:], modbc[:, c0:c0 + 512], ALU.add, r=[pk, 'modbc'], w=['modbc'])
                tt('dve', mT3, mT3, bmodT[:, 0:16].unsqueeze(2).to_broadcast([128, 16, 2]), ALU.add, r=['modT', 'bmodT'], w=['modT'])
            S.barrier()
            onep = sb('onep', [128, 16])
            ts('dve', onep[:], modT[:, 16:32], 1.0, ALU.add, r=['modT'], w=['onep'])
            onep3 = onep[:].rearrange("p (k w) -> p k w", w=2)
            sh13 = modT[:, 0:16].rearrange("p (k w) -> p k w", w=2)
            ts('pool', modbc[:, 2048:3072], modbc[:, 2048:3072], 1.0, ALU.add, r=['modbc'], w=['modbc'])
            G1 = modbc[:, 0:1024]; SH2 = modbc[:, 1024:2048]; ONESC2 = modbc[:, 2048:3072]; G2 = modbc[:, 3072:4096]
            debug_out('dbg_modT', modT[:], ['modT'], 'sp')
            debug_out('dbg_modbc', modbc[:], ['modbc'], 'sp')
            checkpoint('p0')

            def s5_setup(Bm, Cm, magt, thr):
                with contextlib.ExitStack() as es0:
                    def t0(name, shape, dt=F32):
                        return es0.enter_context(nc.sbuf_tensor('s_' + name, list(shape), dt))
                    lr = t0('lr', [128, 32]); li = t0('li', [128, 32]); ls = t0('ls', [128, 32])
                    dma('sp', lr[:], D['lamre'], 'c_lr', w=['lr']); dma('sp', li[:], D['lamim'], 'c_li', w=['li'])
                    dma('sp', ls[:], D['lstep'], 'c_ls', w=['ls'])
                    bre = t0('bre', [128, 512]); bim = t0('bim', [128, 512]); cre = t0('cre', [128, 512]); cim = t0('cim', [128, 512])
                    dma('sp', bre[:], D['bre'], 'c_bre', w=['bre']); dma('sp', bim[:], D['bim'], 'c_bim', w=['bim'])
                    dma('sp', cre[:], D['cre'], 'c_cre', w=['cre']); dma('sp', cim[:], D['cim'], 'c_cim', w=['cim'])
                    dt_ = t0('dt_', [128, 32]); lrdt = t0('lrdt', [128, 32]); lidt = t0('lidt', [128, 32])
                    a1 = t0('a1', [128, 32]); a2 = t0('a2', [128, 32]); sinv = t0('sinv', [128, 32]); cosv = t0('cosv', [128, 32])
                    ar = t0('ar', [128, 32]); ai = t0('ai', [128, 32]); den = t0('den', [128, 32]); tq = t0('tq', [128, 32])
                    crr = t0('crr', [128, 32]); cii = t0('cii', [128, 32])
                    act(dt_[:], ls[:], AF.Exp, r=['ls'], w=['dt_'])
                    tt('dve', lrdt[:], lr[:], dt_[:], ALU.mult, r=['lr', 'dt_'], w=['lrdt'])
                    tt('dve', lidt[:], li[:], dt_[:], ALU.mult, r=['li', 'dt_'], w=['lidt'])
                    act(magt[:], lrdt[:], AF.Exp, r=['lrdt'], w=['magt'])
                    ts('dve', a1[:], lidt[:], 1.0 / TWO_PI, ALU.mult, r=['lidt'], w=['a1'])
                    ts('dve', a2[:], a1[:], MAGIC, ALU.add, r=['a1'], w=['a2'])
                    stt('dve', thr[:], a2[:], MAGIC, a1[:], ALU.subtract, ALU.subtract, r=['a2', 'a1'], w=['thr'])
                    act(a2[:], thr[:], AF.Abs, r=['thr'], w=['a2'])
                    act(sinv[:], thr[:], AF.Sin, r=['thr'], w=['sinv'], scale=-TWO_PI)
                    act(cosv[:], a2[:], AF.Sin, r=['a2', 'halfpi'], w=['cosv'], bias=halfpi[:, 0:1], scale=-TWO_PI)
                    ts('dve', thr[:], thr[:], -1.0, ALU.mult, r=['thr', 'sinv'], w=['thr'])
                    tt('dve', ar[:], magt[:], cosv[:], ALU.mult, r=['magt', 'cosv'], w=['ar'])
                    tt('dve', ai[:], magt[:], sinv[:], ALU.mult, r=['magt', 'sinv'], w=['ai'])
                    tt('dve', den[:], lr[:], lr[:], ALU.mult, r=['lr'], w=['den'])
                    tt('dve', tq[:], li[:], li[:], ALU.mult, r=['li'], w=['tq'])
                    tt('dve', den[:], den[:], tq[:], ALU.add, r=['den', 'tq'], w=['den'])
                    S.op('dve', lambda e: e.reciprocal(out=den[:], in_=den[:]), ['den'], ['den'])
                    ts('dve', ar[:], ar[:], -1.0, ALU.add, r=['ar'], w=['ar'])
                    tt('dve', crr[:], ar[:], lr[:], ALU.mult, r=['ar', 'lr'], w=['crr'])
                    tt('dve', tq[:], ai[:], li[:], ALU.mult, r=['ai', 'li'], w=['tq'])
                    tt('dve', crr[:], crr[:], tq[:], ALU.add, r=['crr', 'tq'], w=['crr'])
                    tt('dve', crr[:], crr[:], den[:], ALU.mult, r=['crr', 'den'], w=['crr'])
                    tt('dve', cii[:], ai[:], lr[:], ALU.mult, r=['ai', 'lr'], w=['cii'])
                    tt('dve', tq[:], ar[:], li[:], ALU.mult, r=['ar', 'li'], w=['tq'])
                    tt('dve', cii[:], cii[:], tq[:], ALU.subtract, r=['cii', 'tq'], w=['cii'])
                    tt('dve', cii[:], cii[:], den[:], ALU.mult, r=['cii', 'den'], w=['cii'])
                    bbr = t0('bbr', [128, 512]); bbi = t0('bbi', [128, 512]); tb = t0('tb', [128, 512])
                    crb = crr[:].unsqueeze(2).to_broadcast([128, 32, 16]); cib = cii[:].unsqueeze(2).to_broadcast([128, 32, 16])
                    v3 = lambda t: t[:].rearrange("p (a h) -> p a h", h=16)
                    tt('dve', v3(bbr), v3(bre), crb, ALU.mult, r=['bre', 'crr'], w=['bbr'])
                    tt('dve', v3(tb), v3(bim), cib, ALU.mult, r=['bim', 'cii'], w=['tb'])
                    tt('dve', bbr[:], bbr[:], tb[:], ALU.subtract, r=['bbr', 'tb'], w=['bbr'])
                    tt('dve', v3(bbi), v3(bim), crb, ALU.mult, r=['bim', 'crr'], w=['bbi'])
                    tt('dve', v3(tb), v3(bre), cib, ALU.mult, r=['bre', 'cii', 'bbr'], w=['tb'])
                    tt('dve', bbi[:], bbi[:], tb[:], ALU.add, r=['bbi', 'tb'], w=['bbi'])
                    Bst = t0('Bst', [128, 64 * 128])
                    mset('pool', Bst[:], 0.0, w=['Bst'])
                    for d in range(2):
                        for s in range(2):
                            for ri, src in ((0, bbr), (1, bbi)):
                                base = Bst[s * 64:(s + 1) * 64, ((d * 16) * 2 + ri) * 128 + s * 16:((d * 16) * 2 + ri) * 128 + s * 16 + 1]
                                dst = bass.AP(base.tensor, base.offset, [list(base.ap[0]), [1024, 4], [288, 4], [1, 16]])
                                sv = src[s * 64:(s + 1) * 64, d * 256:(d + 1) * 256].rearrange("p (c j h) -> p c j h", c=4, j=4)
                                cp('dve', dst, sv, r=['bbr', 'bbi', 'Bst'], w=['Bst'])
                    for q4 in range(16):
                        ps, pk = nps()
                        for i4 in range(4):
                            idx = q4 * 4 + i4
                            tr(ps[:, i4 * 128:(i4 + 1) * 128], Bst[:, idx * 128:(idx + 1) * 128], identf[:], r=['Bst', 'identf'], w=[pk])
                        cp('act', Bm[:, q4 * 4:(q4 + 1) * 4, :], ps[:].rearrange("p (a c) -> p a c", c=128), r=[pk], w=['Bm'])
                    Xc = t0('Xc', [128, 16 * 128])
                    Xv = Xc[:].rearrange("p (a r c) -> p a r c", r=2, c=128)
                    c3 = lambda t: t[:].rearrange("p (a q) -> p a q", q=64)
                    for s in range(2):
                        ts('dve', Xv[:, :, 0, s * 64:(s + 1) * 64], c3(cre), par[:, s:s + 1], ALU.mult, r=['cre', 'par', 'Xc'], w=['Xc'])
                        ts('dve', Xv[:, :, 1, s * 64:(s + 1) * 64], c3(cim), par[:, 2 + s:3 + s], ALU.mult, r=['cim', 'par', 'Xc'], w=['Xc'])
                    mset('pool', Cm[:], 0.0, w=['Cm'])
                    for q4 in range(4):
                        ps, pk = nps()
                        for i4 in range(4):
                            idx = q4 * 4 + i4
                            tr(ps[:, i4 * 128:(i4 + 1) * 128], Xc[:, idx * 128:(idx + 1) * 128], identf[:], r=['Xc', 'identf'], w=[pk])
                        for i4 in range(4):
                            idx = q4 * 4 + i4
                            dct, ri = idx // 2, idx % 2
                            d, ct = dct // 4, dct % 4
                            base = Cm[:, (d * 16 + ct * 4) * 2 + ri, 0:1]
                            dst = bass.AP(base.tensor, base.offset, [list(base.ap[0]), [2 * 128 + 32, 4], [1, 32]])
                            cp('dve', dst, ps[:, i4 * 128:(i4 + 1) * 128].rearrange("p (j c) -> p j c", c=32), r=[pk, 'Cm'], w=['Cm'])

                S.barrier()

            gates = sb('gates', [128, 16, NE])
            yacc = sb('yacc', [128, 4, NTOK])
            es13 = contextlib.ExitStack()
            es13.__enter__()
            OPEN.append(es13)
            sT_own = es13.enter_context(nc.sbuf_tensor('s_sT_own', [128, 4, NTOK], BF16))
            sT_ctx = es13.enter_context(nc.sbuf_tensor('s_sT_ctx', [128, 4, 256], BF16))
            es1 = contextlib.ExitStack()
            es1.__enter__()
            OPEN.append(es1)

            def t1(name, shape, dt=F32):
                return es1.enter_context(nc.sbuf_tensor('s_' + name, list(shape), dt))
            qT = t1('qT', [128, 4, NTOK], BF16)
            kT = t1('kT', [128, 2, 2432], BF16)
            vv = t1('vv', [128, 19, 128], BF16)
            es1b = contextlib.ExitStack()
            es1b.__enter__()
            OPEN.append(es1b)

            def t1b(name, shape, dt=F32):
                return es1b.enter_context(nc.sbuf_tensor('s_' + name, list(shape), dt))
            gbc = t1b('gbc', [128, DM]); bbc = t1b('bbc', [128, DM])
            dma('sp', gbc[:], pbc(D['ln_in_g']), 'c_gbc', w=['gbc'])
            dma('sp', bbc[:], pbc(D['ln_in_b']), 'c_bbc', w=['bbc'])
            ropeCb = [t1b('ropeC%d' % i, [128, 512]) for i in range(2)]
            ropeSb = [t1b('ropeS%d' % i, [128, 512]) for i in range(2)]
            ropecur = [None, None, None, None]
            win_v = D['w_in'].rearrange("(k p) c -> p k c", p=128)
            w_s = t1b('w_s', [128, 8, 512], BF16); w_q = t1b('w_q', [128, 8, 512], BF16)
            w_k = t1b('w_k', [128, 8, 256], BF16); w_v = t1b('w_v', [128, 8, 128], BF16)
            w_gs = yacc[:, 0:2, :].rearrange('p a b -> p (a b)').bitcast(BF16).rearrange('p (k c) -> p k c', c=1024)
            w_ga = yacc[:, 2:4, :].rearrange('p a b -> p (a b)').bitcast(BF16).rearrange('p (k c) -> p k c', c=1024)
            dma('pool', w_s[:], win_v[:, :, 0:512], 'c_ws', w=['w_s'])
            dma('pool', w_q[:], win_v[:, :, 512:1024], 'c_wq', w=['w_q'])
            for kvh in range(2):
                for dup in range(2):
                    dma('pool', w_k[:, :, kvh * 128 + dup * 64:kvh * 128 + dup * 64 + 64],
                        win_v[:, :, 1024 + kvh * 64:1024 + kvh * 64 + 64], 'c_wk%d%d' % (kvh, dup), w=['w_k%d%d' % (kvh, dup)])
            wk_keys = ['w_k00', 'w_k01', 'w_k10', 'w_k11']
            dma('pool', w_v[:], win_v[:, :, 1152:1280], 'c_wv', w=['w_v'])
            dma('pool', w_gs, win_v[:, :, 1280:2304], 'c_wgs', w=['w_gs'])
            dma('pool', w_ga, win_v[:, :, 2304:3328], 'c_wga', w=['w_ga'])
            xt = [t1b('xt%d' % i, [128, DM]) for i in range(3)]
            ht = [t1b('ht%d' % i, [128, DM]) for i in range(2)]
            stt_ = [t1b('st%d' % i, [128, 16]) for i in range(2)]
            uT = [t1b('uT%d' % i, [128, 8, 512], BF16) for i in range(2)]
            rA = [t1b('rA%d' % i, [128, 512]) for i in range(2)]
            rB = [t1b('rB%d' % i, [128, 512]) for i in range(2)]
            stg = [t1b('stg%d' % i, [128, 512], BF16) for i in range(4)]

            LNENG = _cfg('LNENG', 'dve')
            ROPENG = _cfg('ROPENG', 'pool')
            tilectr = [0]
            stgctr = [0]
            ropectr = [0]

            def ln_tile(src_ap, which, ug, ugk, col0, spill_row=None):
                i = tilectr[0]
                tilectr[0] += 1
                x = xt[i % 3]; xk = 'xt%d' % (i % 3)
                h = ht[i % 2]; hk = 'ht%d' % (i % 2)
                st = stt_[i % 2]; sk = 'st%d' % (i % 2)
                dma('sp', x[:], src_ap, xk, w=[xk])
                S.op('dve', lambda e: e.bn_stats(out=st[:, 0:6], in_=x[:, 0:512]), [xk], [sk])
                S.op('dve', lambda e: e.bn_stats(out=st[:, 6:12], in_=x[:, 512:1024]), [xk, sk], [sk])
                S.op('dve', lambda e: e.bn_aggr(out=st[:, 12:14], in_=st[:, 0:12]), [sk], [sk])
                rstd(st, sk)
                ts('dve', x[:], x[:], st[:, 12:13], ALU.subtract, r=[xk, sk], w=[xk], s2=st[:, 14:15], op1=ALU.mult)
                tt(LNENG, h[:], x[:], gbc[:], ALU.mult, r=[xk, 'gbc'], w=[hk])
                tt(LNENG, h[:], h[:], bbc[:], ALU.add, r=[hk, 'bbc'], w=[hk])
                if spill_row is not None:
                    t = spill_row // 128
                    dma('sp', D['h_scr'][spill_row:spill_row + 128, :], h[:], 'hs%d' % (t % 4), r=[hk], w=[('hscr', t)])
                for half in range(2):
                    ps, pk = nps()
                    for kk in range(4):
                        k = half * 4 + kk
                        tr(ps[:, kk * 128:(kk + 1) * 128], h[:, k * 128:(k + 1) * 128], identf[:], r=[hk, 'identf'], w=[pk])
                    for kk in range(4):
                        k = half * 4 + kk
                        act(uT[ug][:, k, col0:col0 + 128], ps[:, kk * 128:(kk + 1) * 128], AF.Identity,
                            r=[pk, 'onep', 'modT'], w=[ugk], bias=sh13[:, k, which:which + 1], scale=onep3[:, k, which:which + 1])

            def proj(ug, ugk, wt, wkeys, c0, n):
                ps, pk = nps()
                for k in range(8):
                    mm(ps[:, 0:n], wt[:, k, c0:c0 + 128], uT[ug][:, k, 0:n], k == 0, k == 7, r=[ugk] + list(wkeys), w=[pk])
                return ps, pk

            def rope_load(slot, rc0, n):
                dma('sp', ropeCb[slot][:, 0:n], D['ropeC'][:, rc0:rc0 + n], 'ropeC%d' % slot, w=['ropeC%d' % slot])
                dma('sp', ropeSb[slot][:, 0:n], D['ropeS'][:, rc0:rc0 + n], 'ropeS%d' % slot, w=['ropeS%d' % slot])
                ropecur[0] = ropeCb[slot]; ropecur[1] = ropeSb[slot]; ropecur[2] = 'ropeC%d' % slot; ropecur[3] = 'ropeS%d' % slot

            def rope(ps, pk, pc0, n, dst, dkey):
                i = ropectr[0] % 2
                ropectr[0] += 1
                ropeC, ropeS, rck, rsk = ropecur
                A = rA[i]; Ak = 'rA%d' % i; B = rB[i]; Bk = 'rB%d' % i
                tt('dve', A[:, 0:n], ps[:, pc0:pc0 + n], ropeC[:, 0:n], ALU.mult, r=[pk, rck], w=[Ak])
                for (o, s_) in ((0, 32), (32, 0), (64, 96), (96, 64)):
                    tt('dve', B[o:o + 32, 0:n], ps[s_:s_ + 32, pc0:pc0 + n], ropeS[s_:s_ + 32, 0:n], ALU.mult,
                       r=[pk, rsk], w=[Bk])
                tt(ROPENG, dst, A[:, 0:n], B[:, 0:n], ALU.add, r=[Ak, Bk], w=[dkey])

            def group(kind, gi):
                ug = (0 if kind == 'ctx' else 1 + gi + (4 if kind == 'oth' else 0)) % 2
                ugk = 'uT%d' % ug
                n = 256 if kind == 'ctx' else 512
                for t in range(n // 128):
                    if kind == 'ctx':
                        ln_tile(D['ctxb'][t * 128:(t + 1) * 128, :], 1, ug, ugk, t * 128)
                    elif kind == 'own':
                        row = gi * 512 + t * 128
                        ln_tile(D['x_own'][row:row + 128, :], 0, ug, ugk, t * 128, spill_row=row)
                    else:
                        row = gi * 512 + t * 128
                        ln_tile(D['x_oth'][row:row + 128, :], 0, ug, ugk, t * 128)
                for ct in range(4):
                    ps, pk = proj(ug, ugk, w_s, ['w_s'], ct * 128, n)
                    if kind == 'own':
                        cp('act', sT_own[:, ct, gi * 512:(gi + 1) * 512], ps[:, 0:n], r=[pk], w=[('sT_own', gi)])
                    elif kind == 'ctx':
                        cp('act', sT_ctx[:, ct, :], ps[:, 0:n], r=[pk], w=['sT_ctx'])
                    else:
                        si = stgctr[0] % 4
                        stgctr[0] += 1
                        cp('act', stg[si][:], ps[:, 0:n], r=[pk], w=['stg%d' % si])
                        dma('sp', D['soth_scr'][ct, :, gi * 512:(gi + 1) * 512], stg[si][:], 'stg%d' % si,
                            r=['stg%d' % si], w=[('soth', ct, gi)])
                halo = (kind == 'oth' and gi == 3)
                if kind == 'own':
                    rope_load(gi % 2, gi * 512, 512)
                elif halo:
                    rope_load(0, 2048, 128)
                if kind != 'oth' or halo:
                    for kvh in range(2):
                        ps, pk = proj(ug, ugk, w_k, wk_keys, kvh * 128, n)
                        if kind == 'ctx':
                            cp('act', kT[:, kvh, 2176:2432], ps[:, 0:256], r=[pk], w=[('kT', kvh, 'ctx')])
                        elif kind == 'own':
                            rope(ps, pk, 0, 512, kT[:, kvh, gi * 512:(gi + 1) * 512], ('kT', kvh, gi))
                        else:
                            rope(ps, pk, 384, 128, kT[:, kvh, 2048:2176], ('kT', kvh, 'halo'))
                    tl = range(n // 128) if not halo else [3]
                    for t in tl:
                        vt = {'ctx': 17 + t, 'own': gi * 4 + t, 'oth': 16}[kind]
                        ps, pk = nps()
                        for k in range(8):
                            mm(ps[:, 0:128], uT[ug][:, k, t * 128:(t + 1) * 128], w_v[:, k, :], k == 0, k == 7,
                               r=[ugk, 'w_v'], w=[pk])
                        cp('act', vv[:, vt, :], ps[:, 0:128], r=[pk], w=[('vv', vt)])
                if kind == 'own':
                    for qt in range(4):
                        ps, pk = proj(ug, ugk, w_q, ['w_q'], qt * 128, n)
                        rope(ps, pk, 0, 512, qT[:, qt, gi * 512:(gi + 1) * 512], ('qT', gi))
                    for (wt, wkey, scr, nm) in ((w_gs, 'w_gs', 'sgs_scr', 'sgs'), (w_ga, 'w_ga', 'sga_scr', 'sga')):
                        for ot in range(8):
                            ps, pk = proj(ug, ugk, wt, [wkey], ot * 128, n)
                            si = stgctr[0] % 4
                            stgctr[0] += 1
                            act(stg[si][:], ps[:, 0:n], AF.Sigmoid, r=[pk], w=['stg%d' % si])
                            dma('sp', D[scr][ot, :, gi * 512:(gi + 1) * 512], stg[si][:], 'stg%d' % si,
                                r=['stg%d' % si], w=[(nm, ot, gi)])

            group('ctx', 0)
            for gi in range(4):
                group('own', gi)
            for gi in range(4):
                group('oth', gi)
            es1b.__exit__(None, None, None)
            OPEN.remove(es1b)
            S.barrier()
            debug_out('dbg_sT', sT_own[:, :, :], [('sT_own', g) for g in range(4)])
            debug_out('dbg_qT', qT[:, :, :], [('qT', g) for g in range(4)])
            debug_out('dbg_kT', kT[:, :, :], [('kT', a, b) for a in range(2) for b in (0, 1, 2, 3, 'halo', 'ctx')])
            debug_out('dbg_vv', vv[:, :, :], [('vv', t) for t in range(19)])
            checkpoint('p1')

            with contextlib.ExitStack() as es2:
                pT = [es2.enter_context(nc.sbuf_tensor('s_pT%d' % i, [128, 512], BF16)) for i in range(3)]
                rec = [es2.enter_context(nc.sbuf_tensor('s_rec%d' % i, [64, 512], F32)) for i in range(2)]
                ost = [es2.enter_context(nc.sbuf_tensor('s_ost%d' % i, [128, 2, 128], BF16)) for i in range(2)]
                GORD = [0, 2, 1, 3]
                items = []
                for kvh in range(2):
                    for qt in range(16):
                        gi = qt // 4
                        tiles = []
                        if qt > 0:
                            tiles.append(((qt - 1) * 128, qt - 1, 0, ('kT', kvh, (qt - 1) // 4), ('vv', qt - 1)))
                        tiles.append((qt * 128, qt, None, ('kT', kvh, gi), ('vv', qt)))
                        if qt < 15:
                            tiles.append(((qt + 1) * 128, qt + 1, 1, ('kT', kvh, (qt + 1) // 4), ('vv', qt + 1)))
                        else:
                            tiles.append((2048, 16, 2, ('kT', kvh, 'halo'), ('vv', 16)))
                        tiles.append((2176, 17, None, ('kT', kvh, 'ctx'), ('vv', 17)))
                        tiles.append((2304, 18, None, ('kT', kvh, 'ctx'), ('vv', 18)))
                        for ti, tl in enumerate(tiles):
                            items.append((kvh, qt, ti, len(tiles), tl))

                def emit_scores(idx):
                    kvh, qt, ti, nt, (kc0, vt, mk, kkey, vkey) = items[idx]
                    gi = qt // 4
                    sb_ = 4 + (idx % 2) * 2
                    pssA, pskA = psb[sb_], 'ps%d' % sb_
                    pssB, pskB = psb[sb_ + 1], 'ps%d' % (sb_ + 1)
                    for s_ in range(4):
                        g = GORD[s_]
                        h = kvh * 4 + g
                        hh = h % 2
                        pss, psk = (pssA, pskA) if hh == 0 else (pssB, pskB)
                        c0_ = (s_ % 2) * 128
                        mm(pss[:, c0_:c0_ + 128], kT[hh * 64:(hh + 1) * 64, kvh, kc0:kc0 + 128],
                           qT[hh * 64:(hh + 1) * 64, h // 2, qt * 128:(qt + 1) * 128], True, True,
                           r=[kkey, ('qT', gi)], w=[psk])
                    p = pT[idx % 3]; pkey = 'pT%d' % (idx % 3)
                    act(p[:, 0:256], pssA[:, 0:256], AF.Exp, r=[pskA], w=[pkey], scale=0.125)
                    act(p[:, 256:512], pssB[:, 0:256], AF.Exp, r=[pskB, pkey], w=[pkey], scale=0.125)
                    if mk is not None:
                        mv = masks[:, mk * 128:(mk + 1) * 128].unsqueeze(1).to_broadcast([128, 4, 128])
                        tt('dve', p[:].rearrange("p (g q) -> p g q", g=4), p[:].rearrange("p (g q) -> p g q", g=4), mv,
                           ALU.mult, r=[pkey, 'masks'], w=[pkey])

                def emit_pv(idx):
                    kvh, qt, ti, nt, (kc0, vt, mk, kkey, vkey) = items[idx]
                    it = kvh * 16 + qt
                    ab_ = (it % 2) * 2
                    pso, pok = psb[ab_], 'ps%d' % ab_
                    psd, pdk = psb[ab_ + 1], 'ps%d' % (ab_ + 1)
                    p = pT[idx % 3]; pkey = 'pT%d' % (idx % 3)
                    mm(pso[0:64, :], vv[:, vt, kvh * 64:(kvh + 1) * 64], p[:], ti == 0, ti == nt - 1, r=[vkey, pkey], w=[pok])
                    mm(psd[0:64, :], ones_b[:, 0:64], p[:], ti == 0, False, r=['ones_b', pkey], w=[pdk])
                    if ti < nt - 1:
                        return
                    mm(psd[0:64, :], ones_b[0:1, 0:64], esink[0:1, kvh * 512:(kvh + 1) * 512], False, True,
                       r=['ones_b', 'esink'], w=[pdk])
                    rc = rec[it % 2]; rk = 'rec%d' % (it % 2)
                    S.op('dve', lambda e: e.reciprocal(out=rc[:], in_=psd[0:64, :]), [pdk], [rk])
                    osl = it % 2
                    for s_ in range(4):
                        g = GORD[s_]
                        hh = g % 2
                        tt('dve', ost[osl][hh * 64:(hh + 1) * 64, g // 2, :],
                           pso[0:64, s_ * 128:(s_ + 1) * 128], rc[:, s_ * 128:(s_ + 1) * 128], ALU.mult,
                           r=[pok, rk], w=['ost%d' % osl])
                    dma('sp', D['o_scr'][kvh * 2:kvh * 2 + 2, :, qt * 128:(qt + 1) * 128].rearrange("k p t -> p k t"),
                        ost[osl][:], 'ost%d' % osl, r=['ost%d' % osl], w=[('oscr', kvh, qt)])

                for idx in range(len(items)):
                    emit_scores(idx)
                    if idx > 0:
                        emit_pv(idx - 1)
                emit_pv(len(items) - 1)
            es1.__exit__(None, None, None)
            OPEN.remove(es1)
            S.barrier()
            debug_out('dbg_o', D['o_scr'], [('oscr', a, b) for a in range(2) for b in range(16)], 'sp')
            checkpoint('p2')

            with contextlib.ExitStack() as es3:
                def t3(name, shape, dt=F32):
                    return es3.enter_context(nc.sbuf_tensor('s_' + name, list(shape), dt))
                Bm = t3('Bm', [128, 64, 128], BF16)
                Cm = t3('Cm', [128, 64, 128], BF16)
                magt = t3('magt', [128, 32]); thr = t3('thr', [128, 32])
                s5_setup(Bm, Cm, magt, thr)
                nmU = ['XR', 'XI', 'M1', 'M2', 'TR', 'TI']
                ub = [{nm: t3('%s%d' % (nm, i), [128, 512]) for nm in nmU} for i in range(4)]
                tabC = [t3('tabC%d' % i, [128, 512]) for i in range(4)]
                tabS = [t3('tabS%d' % i, [128, 512]) for i in range(4)]
                sre = [[t3('sre%d_%d' % (i, jj), [128, 512], BF16) for jj in range(4)] for i in range(2)]
                sim = [[t3('sim%d_%d' % (i, jj), [128, 512], BF16) for jj in range(4)] for i in range(2)]
                soth = [t3('soth%d' % i, [128, 512], BF16) for i in range(2)]
                carry = t3('carry', [128, 64])
                ctmp = t3('ctmp', [128, 8])
                sctr = 0
                octr = 0

                def rev(ap2):
                    n = ap2.ap[-1][1]
                    return bass.AP(ap2.tensor, ap2.offset + (n - 1) * ap2.ap[-1][0], [list(ap2.ap[0]), [-ap2.ap[-1][0], n]])

                ENGJ = ['dve', 'dve', 'dve', 'dve']
                for d in range(2):
                    if d == 0:
                        segs = [('ctx', 0, False)] + [('own', c, False) for c in range(4)]
                    else:
                        segs = [('ctx', 0, True)] + [('oth', c, False) for c in range(4)] + [('own', c, True) for c in (3, 2, 1, 0)]
                    for ct in range(4):
                        for jj in range(4):
                            dj = d * 16 + ct * 4 + jj
                            E = ENGJ[jj]
                            u = ub[jj]
                            kM1 = 'M1_%d' % jj; kM2 = 'M2_%d' % jj; kC = 'tabC%d' % jj; kS = 'tabS%d' % jj
                            ts(E, u['M1'][:], iota1[:], thr[:, dj:dj + 1], ALU.mult, r=['iota1', 'thr'], w=[kM1])
                            ts(E, u['M2'][:], u['M1'][:], MAGIC, ALU.add, r=[kM1], w=[kM2])
                            ts(E, u['M2'][:], u['M2'][:], -MAGIC, ALU.add, r=[kM2], w=[kM2])
                            tt(E, tabS[jj][:], u['M2'][:], u['M1'][:], ALU.subtract, r=[kM2, kM1], w=[kS])
                            act(tabC[jj][:], tabS[jj][:], AF.Abs, r=[kS], w=[kC])
                            act(tabS[jj][:], tabS[jj][:], AF.Sin, r=[kS, kC], w=[kS], scale=-TWO_PI)
                            act(tabC[jj][:], tabC[jj][:], AF.Sin, r=[kC, 'halfpi'], w=[kC], bias=halfpi[:, 0:1], scale=-TWO_PI)
                        for si_, (kind, c, rv) in enumerate(segs):
                            n = 256 if kind == 'ctx' else 512
                            if kind == 'ctx':
                                src = sT_ctx[:, ct, :]; skey = 'sT_ctx'
                            elif kind == 'own':
                                src = sT_own[:, ct, c * 512:(c + 1) * 512]; skey = ('sT_own', c)
                            else:
                                so = soth[octr % 2]; skey = 'soth%d' % (octr % 2)
                                octr += 1
                                dma('sp', so[:], D['soth_scr'][ct, :, c * 512:(c + 1) * 512], skey, r=[('soth', ct, c)], w=[skey])
                                src = so[:]
                            if rv:
                                src = rev(src)
                            sslot = sctr % 2
                            if kind == 'own':
                                sctr += 1
                            K = lambda nm, jj: '%s_%d' % (nm, jj)
                            for jj in range(4):
                                dj = d * 16 + ct * 4 + jj
                                u = ub[jj]
                                psr, prk = nps()
                                psi, pik = nps()
                                mm(psr[:, 0:n], Bm[:, dj * 2, :], src, True, True, r=[skey, 'Bm'], w=[prk])
                                mm(psi[:, 0:n], Bm[:, dj * 2 + 1, :], src, True, True, r=[skey, 'Bm'], w=[pik])
                                cp('act', u['XR'][:, 0:n], psr[:, 0:n], r=[prk], w=[K('XR', jj)])
                                cp('act', u['XI'][:, 0:n], psi[:, 0:n], r=[pik], w=[K('XI', jj)])
                            for jj in range(4):
                                E = ENGJ[jj]; u = ub[jj]
                                tt(E, u['M1'][:, 0:n], u['XR'][:, 0:n], tabC[jj][:, 0:n], ALU.mult, r=[K('XR', jj), 'tabC%d' % jj], w=[K('M1', jj)])
                                tt(E, u['M2'][:, 0:n], u['XI'][:, 0:n], tabS[jj][:, 0:n], ALU.mult, r=[K('XI', jj), 'tabS%d' % jj], w=[K('M2', jj)])
                            for jj in range(4):
                                E = ENGJ[jj]; u = ub[jj]
                                tt(E, u['TR'][:, 0:n], u['M1'][:, 0:n], u['M2'][:, 0:n], ALU.add, r=[K('M1', jj), K('M2', jj)], w=[K('TR', jj)])
                            for jj in range(4):
                                E = ENGJ[jj]; u = ub[jj]
                                tt(E, u['M1'][:, 0:n], u['XI'][:, 0:n], tabC[jj][:, 0:n], ALU.mult, r=[K('XI', jj), 'tabC%d' % jj], w=[K('M1', jj)])
                                tt(E, u['M2'][:, 0:n], u['XR'][:, 0:n], tabS[jj][:, 0:n], ALU.mult, r=[K('XR', jj), 'tabS%d' % jj], w=[K('M2', jj)])
                            for jj in range(4):
                                E = ENGJ[jj]; u = ub[jj]
                                tt(E, u['TI'][:, 0:n], u['M1'][:, 0:n], u['M2'][:, 0:n], ALU.subtract, r=[K('M1', jj), K('M2', jj)], w=[K('TI', jj)])
                            for jj in range(4):
                                dj = d * 16 + ct * 4 + jj
                                u = ub[jj]
                                ck = ('carry', dj)
                                mg = magt[:, dj:dj + 1].to_broadcast([128, n])
                                if si_ == 0:
                                    scan('dve', u['XR'][:, 0:n], mg, u['TR'][:, 0:n], 0.0, r=['magt', K('TR', jj)], w=[K('XR', jj)])
                                    scan('dve', u['XI'][:, 0:n], mg, u['TI'][:, 0:n], 0.0, r=['magt', K('TI', jj)], w=[K('XI', jj)])
                                else:
                                    scan('dve', u['XR'][:, 0:n], mg, u['TR'][:, 0:n], carry[:, 2 * dj:2 * dj + 1], r=['magt', K('TR', jj), ck], w=[K('XR', jj)])
                                    scan('dve', u['XI'][:, 0:n], mg, u['TI'][:, 0:n], carry[:, 2 * dj + 1:2 * dj + 2], r=['magt', K('TI', jj), ck], w=[K('XI', jj)])
                            if si_ < len(segs) - 1:
                                for jj in range(4):
                                    dj = d * 16 + ct * 4 + jj
                                    u = ub[jj]
                                    ck = ('carry', dj)
                                    cn = tabC[jj][:, n - 1:n]; sn_ = tabS[jj][:, n - 1:n]
                                    rl = u['XR'][:, n - 1:n]; il = u['XI'][:, n - 1:n]
                                    tk_ = 'ctmp%d' % jj
                                    tt('dve', ctmp[:, 2 * jj:2 * jj + 1], il, sn_, ALU.mult, r=[K('XI', jj), 'tabS%d' % jj], w=[tk_])
                                    tt('dve', ctmp[:, 2 * jj + 1:2 * jj + 2], il, cn, ALU.mult, r=[K('XI', jj), 'tabC%d' % jj, tk_], w=[tk_])
                                    stt('dve', carry[:, 2 * dj:2 * dj + 1], rl, cn, ctmp[:, 2 * jj:2 * jj + 1], ALU.mult, ALU.subtract,
                                        r=[K('XR', jj), 'tabC%d' % jj, tk_, ck], w=[ck])
                                    stt('dve', carry[:, 2 * dj + 1:2 * dj + 2], rl, sn_, ctmp[:, 2 * jj + 1:2 * jj + 2], ALU.mult, ALU.add,
                                        r=[K('XR', jj), 'tabS%d' % jj, tk_, ck], w=[ck])
                            if kind == 'own':
                                for jj in range(4):
                                    E = ENGJ[jj]; u = ub[jj]
                                    tt(E, u['M1'][:, 0:n], u['XR'][:, 0:n], tabC[jj][:, 0:n], ALU.mult, r=[K('XR', jj), 'tabC%d' % jj], w=[K('M1', jj)])
                                    tt(E, u['M2'][:, 0:n], u['XI'][:, 0:n], tabS[jj][:, 0:n], ALU.mult, r=[K('XI', jj), 'tabS%d' % jj], w=[K('M2', jj)])
                                for jj in range(4):
                                    E = ENGJ[jj]; u = ub[jj]
                                    tt(E, sre[sslot][jj][:], u['M1'][:, 0:n], u['M2'][:, 0:n], ALU.subtract, r=[K('M1', jj), K('M2', jj)], w=[('sre', sslot, jj)])
                                for jj in range(4):
                                    E = ENGJ[jj]; u = ub[jj]
                                    tt(E, u['M1'][:, 0:n], u['XR'][:, 0:n], tabS[jj][:, 0:n], ALU.mult, r=[K('XR', jj), 'tabS%d' % jj], w=[K('M1', jj)])
                                    tt(E, u['M2'][:, 0:n], u['XI'][:, 0:n], tabC[jj][:, 0:n], ALU.mult, r=[K('XI', jj), 'tabC%d' % jj], w=[K('M2', jj)])
                                for jj in range(4):
                                    E = ENGJ[jj]; u = ub[jj]
                                    tt(E, sim[sslot][jj][:], u['M1'][:, 0:n], u['M2'][:, 0:n], ALU.add, r=[K('M1', jj), K('M2', jj)], w=[('sim', sslot, jj)])
                                psy, pyk = nps()
                                for jj in range(4):
                                    dj = d * 16 + ct * 4 + jj
                                    a_re = sre[sslot][jj][:]; a_im = sim[sslot][jj][:]
                                    if rv:
                                        a_re = rev(a_re); a_im = rev(a_im)
                                    mm(psy[:], Cm[:, dj * 2, :], a_re, jj == 0, False, r=['Cm', ('sre', sslot, jj)], w=[pyk])
                                    mm(psy[:], Cm[:, dj * 2 + 1, :], a_im, False, jj == 3, r=['Cm', ('sim', sslot, jj)], w=[pyk])
                                ysl = yacc[:, ct, c * 512:(c + 1) * 512]
                                if d == 0:
                                    stt('dve', ysl, sT_own[:, ct, c * 512:(c + 1) * 512], dcol[:, ct:ct + 1], psy[:], ALU.mult, ALU.add,
                                        r=[pyk, ('sT_own', c), 'dcol'], w=[('yacc', ct, c)])
                                else:
                                    tt('dve', ysl, ysl, psy[:], ALU.add, r=[pyk, ('yacc', ct, c)], w=[('yacc', ct, c)])
            es13.__exit__(None, None, None)
            OPEN.remove(es13)
            S.barrier()
            debug_out('dbg_y', yacc[:, :, :], [('yacc', a, b) for a in range(4) for b in range(4)], 'sp')
            checkpoint('p3')

            GELUENG = _cfg('GELUENG', 'dve'); MTENG = _cfg('MTENG', 'dve'); LN1ENG = _cfg('LN1ENG', 'dve')
            HMENG = _cfg('HMENG', 'dve'); HBENG = _cfg('HBENG', 'dve')
            with contextlib.ExitStack() as es4:
                def t4(name, shape, dt=F32):
                    return es4.enter_context(nc.sbuf_tensor('s_' + name, list(shape), dt))
                wglu = t4('wglu', [128, 4, 512], BF16); wso = t4('wso', [128, 4, DM], BF16); wao = t4('wao', [128, 4, DM], BF16)
                wo = t4('wo', [128, 8, DM], BF16); wr = t4('wr', [128, 8, NE]); brt = t4('brt', [1, NE])
                dma('pool', wglu[:], D['w_glu'].rearrange("(k p) c -> p k c", p=128), 'c_wglu', w=['wglu'])
                dma('pool', wso[:], D['w_ssm_out'].rearrange("(k p) c -> p k c", p=128), 'c_wso', w=['wso'])
                dma('pool', wao[:], D['w_att_out'].rearrange("(k p) c -> p k c", p=128), 'c_wao', w=['wao'])
                dma('pool', wo[:], D['w_o'].rearrange("(k p) c -> p k c", p=128), 'c_wo', w=['wo'])
                dma('sp', wr[:], D['w_router'].rearrange("(k p) c -> p k c", p=128), 'c_wr', w=['wr'])
                dma('sp', brt[:], D['b_router'], 'c_br', w=['brt'])
                l1g = t4('l1g', [128, DM]); l1b = t4('l1b', [128, DM])
                dma('sp', l1g[:], pbc(D['ln1_g']), 'c_l1g', w=['l1g'])
                dma('sp', l1b[:], pbc(D['ln1_b']), 'c_l1b', w=['l1b'])
                sgs = [t4('sgs%d' % i, [128, 8, 512], BF16) for i in range(1)]
                sga = [t4('sga%d' % i, [128, 8, 512], BF16) for i in range(1)]
                oTc = [t4('oTc%d' % i, [128, 4, 512], BF16) for i in range(2)]
                g_t = [t4('g_t%d' % i, [128, 512]) for i in range(2)]
                g_w = [t4('g_w%d' % i, [128, 512]) for i in range(2)]
                zT = t4('zT', [128, 4, 512], BF16); z2T = t4('z2T', [128, 4, 512], BF16)
                sgl = [t4('sgl%d' % i, [128, 512], BF16) for i in range(2)]
                m1 = [t4('m1_%d' % i, [128, 512]) for i in range(2)]
                m2 = [t4('m2_%d' % i, [128, 512]) for i in range(2)]
                mT = t4('mT', [128, 8, 512], BF16)
                hr = [t4('hr%d' % i, [128, DM]) for i in range(2)]
                tk = [t4('tk%d' % i, [128, DM]) for i in range(2)]
                hmf = [t4('hmf%d' % i, [128, 8, 128]) for i in range(2)]
                hmb = [t4('hmb%d' % i, [128, 8, 128], BF16) for i in range(2)]
                st4 = [t4('st4_%d' % i, [128, 16]) for i in range(2)]
                lg = [t4('lg%d' % i, [128, NE]) for i in range(2)]
                mx = [t4('mx%d' % i, [128, 16]) for i in range(2)]
                msk = [t4('msk%d' % i, [128, NE]) for i in range(2)]
                P4STOP = _cfg('P4STOP', '')
                for c in range(4):
                    if P4STOP and c > 0:
                        break
                    cs_ = slice(c * 512, (c + 1) * 512)
                    b2 = 0
                    oc = oTc[c % 2]; ock = 'oTc%d' % (c % 2)
                    for kvh in range(2):
                        dma('sp', oc[:, kvh * 2:kvh * 2 + 2, :], D['o_scr'][kvh * 2:kvh * 2 + 2, :, cs_].rearrange("k p t -> p k t"),
                            ock, r=[('oscr', kvh, qt) for qt in range(c * 4, c * 4 + 4)], w=[ock])
                    for ot in range(8):
                        dma('sp', sgs[b2][:, ot, :], D['sgs_scr'][ot, :, cs_], 'sgsl%d' % b2, r=[('sgs', ot, c)], w=['sgs%d' % b2])
                        dma('sp', sga[b2][:, ot, :], D['sga_scr'][ot, :, cs_], 'sgal%d' % b2, r=[('sga', ot, c)], w=['sga%d' % b2])
                    if P4STOP == 'loads':
                        break
                    for ct in range(4):
                        i = ct % 2
                        y = yacc[:, ct, cs_]; yk = ('yacc', ct, c)
                        tt(GELUENG, g_t[i][:], y, y, ALU.mult, r=[yk], w=['g_t%d' % i])
                        ts(GELUENG, g_t[i][:], g_t[i][:], 0.044715, ALU.mult, r=['g_t%d' % i], w=['g_t%d' % i], s2=1.0, op1=ALU.add)
                        tt(GELUENG, g_w[i][:], g_t[i][:], y, ALU.mult, r=['g_t%d' % i, yk], w=['g_w%d' % i])
                        act(g_w[i][:], g_w[i][:], AF.Sigmoid, r=['g_w%d' % i], w=['g_w%d' % i], scale=1.5957691216057308)
                        tt(GELUENG, zT[:, ct, :], g_w[i][:], y, ALU.mult, r=['g_w%d' % i, yk], w=[('zT', ct)])
                    if P4STOP == 'gelu':
                        break
                    for ct in range(4):
                        ps, pk = nps()
                        for k in range(4):
                            mm(ps[:], wglu[:, k, ct * 128:(ct + 1) * 128], zT[:, k, :], k == 0, k == 3, r=['wglu', ('zT', k)], w=[pk])
                        i = ct % 2
                        act(sgl[i][:], ps[:], AF.Sigmoid, r=[pk, 'bgluT'], w=['sgl%d' % i], bias=bgluT[:, ct:ct + 1])
                        tt('dve', z2T[:, ct, :], zT[:, ct, :], sgl[i][:], ALU.mult, r=[('zT', ct), 'sgl%d' % i], w=[('z2T', ct)])
                    if P4STOP == 'glu':
                        break
                    for ot in range(8):
                        i = ot % 2
                        psa, pak = nps()
                        for k in range(4):
                            mm(psa[:], wso[:, k, ot * 128:(ot + 1) * 128], z2T[:, k, :], k == 0, k == 3, r=['wso', ('z2T', k)], w=[pak])
                        psb_, pbk = nps()
                        for k in range(4):
                            mm(psb_[:], wao[:, k, ot * 128:(ot + 1) * 128], oc[:, k, :], k == 0, k == 3, r=['wao', ock], w=[pbk])
                        tt('dve', m1[i][:], psa[:], sgs[b2][:, ot, :], ALU.mult, r=[pak, 'sgs%d' % b2], w=['m1_%d' % i])
                        tt('dve', m2[i][:], psb_[:], sga[b2][:, ot, :], ALU.mult, r=[pbk, 'sga%d' % b2], w=['m2_%d' % i])
                        tt(MTENG, mT[:, ot, :], m1[i][:], m2[i][:], ALU.add, r=['m1_%d' % i, 'm2_%d' % i], w=[('mT', ot)])
                    if P4STOP == 'branch':
                        break
                    for t in range(4):
                        tg = c * 4 + t
                        i = tg % 2
                        row = tg * 128
                        h = hr[i]; hk = 'hr%d' % i
                        x = tk[i]; xk = 'tk%d' % i
                        st = st4[i]; sk = 'st4_%d' % i
                        dma('sp', h[:], D['h_scr'][row:row + 128, :], hk, r=[('hscr', tg)], w=[hk])
                        for half in range(2):
                            ps, pk = nps()
                            for k in range(8):
                                mm(ps[:], mT[:, k, t * 128:(t + 1) * 128], wo[:, k, half * 512:(half + 1) * 512], k == 0, k == 7,
                                   r=[('mT', k), 'wo'], w=[pk])
                            tt('dve', x[:, half * 512:(half + 1) * 512], ps[:], G1[:, half * 512:(half + 1) * 512], ALU.mult,
                               r=[pk, 'modbc'], w=[xk])
                        stt('dve', x[:], h[:], ALPHA, x[:], ALU.mult, ALU.add, r=[hk, xk], w=[xk])
                        if P4STOP == 'mix':
                            continue
                        S.op('dve', lambda e, st=st, x=x: e.bn_stats(out=st[:, 0:6], in_=x[:, 0:512]), [xk], [sk])
                        S.op('dve', lambda e, st=st, x=x: e.bn_stats(out=st[:, 6:12], in_=x[:, 512:1024]), [xk, sk], [sk])
                        S.op('dve', lambda e, st=st: e.bn_aggr(out=st[:, 12:14], in_=st[:, 0:12]), [sk], [sk])
                        rstd(st, sk)
                        ts('dve', x[:], x[:], st[:, 12:13], ALU.subtract, r=[xk, sk], w=[xk], s2=st[:, 14:15], op1=ALU.mult)
                        tt(LN1ENG, x[:], x[:], l1g[:], ALU.mult, r=[xk, 'l1g'], w=[xk])
                        tt(LN1ENG, h[:], x[:], l1b[:], ALU.add, r=[xk, 'l1b', hk], w=[hk])
                        if P4STOP == 'ln':
                            continue
                        dma('sp', D['h1_scr'][row:row + 128, :], h[:], 'h1s%d' % (tg % 4), r=[hk], w=[('h1scr', tg)])
                        tt(HMENG, x[:], h[:], ONESC2, ALU.mult, r=[hk, 'modbc'], w=[xk])
                        tt(HMENG, x[:], x[:], SH2, ALU.add, r=[xk, 'modbc'], w=[xk])
                        if P4STOP == 'spill':
                            continue
                        hf_ = hmf[i]; hfk = 'hmf%d' % i
                        hb_ = hmb[i]; hbk = 'hmb%d' % i
                        for half in range(2):
                            ps, pk = nps()
                            for kk in range(4):
                                k = half * 4 + kk
                                tr(ps[:, kk * 128:(kk + 1) * 128], x[:, k * 128:(k + 1) * 128], identf[:], r=[xk, 'identf'], w=[pk])
                            cp('act', hf_[:, half * 4:(half + 1) * 4, :], ps[:].rearrange("p (a c) -> p a c", c=128), r=[pk], w=[hfk])
                        cp(HBENG, hb_[:], hf_[:], r=[hfk], w=[hbk])
                        for k in range(8):
                            dma('sp', D['hmT_scr'][k, :, row:row + 128], hb_[:, k, :], 'hmts%d' % i, r=[hbk], w=[('hmT', tg, k)])
                        if 'router' in _cfg('P4SKIP', ''):
                            mset('dve', gates[:, tg, :], 0.25, w=[('gates', tg)])
                            continue
                        ps, pk = nps()
                        for k in range(8):
                            mm(ps[:, 0:NE], hf_[:, k, :], wr[:, k, :], k == 0, False, r=[hfk, 'wr'], w=[pk])
                        mm(ps[:, 0:NE], ones_f[0:1, :], brt[0:1, :], False, True, r=['ones_f', 'brt'], w=[pk])
                        L = lg[i]; lk = 'lg%d' % i
                        M = mx[i]; mk_ = 'mx%d' % i
                        K_ = msk[i]; kk_ = 'msk%d' % i
                        cp('act', L[:], ps[:, 0:NE], r=[pk], w=[lk])
                        S.op('dve', lambda e, M=M, L=L: e.max(out=M[:, 0:8], in_=L[:]), [lk], [mk_])
                        ts('dve', K_[:], L[:], M[:, 3:4], ALU.is_ge, r=[lk, mk_], w=[kk_])
                        ts('dve', M[:, 8:9], M[:, 0:1], -1.0, ALU.mult, r=[mk_], w=[mk_])
                        act(L[:], L[:], AF.Exp, r=[lk, mk_], w=[lk], bias=M[:, 8:9])
                        tt('dve', L[:], L[:], K_[:], ALU.mult, r=[lk, kk_], w=[lk])
                        S.op('dve', lambda e, M=M, L=L: e.reduce_sum(out=M[:, 9:10], in_=L[:], axis=mybir.AxisListType.X), [lk, mk_], [mk_])
                        S.op('dve', lambda e, M=M: e.reciprocal(out=M[:, 10:11], in_=M[:, 9:10]), [mk_], [mk_])
                        ts('dve', gates[:, tg, :], L[:], M[:, 10:11], ALU.mult, r=[lk, mk_], w=[('gates', tg)])

            S.barrier()
            debug_out('dbg_gates', gates[:, :, :], [('gates', t) for t in range(16)], 'sp')
            debug_out('dbg_h1', D['h1_scr'], [('h1scr', t) for t in range(16)], 'sp')
            checkpoint('p4')
            finals = []
            with contextlib.ExitStack() as es5:
                def t5(name, shape, dt=F32):
                    return es5.enter_context(nc.sbuf_tensor('s_' + name, list(shape), dt))
                bguT = t5('bguT', [128, NE * 16]); bgu1 = t5('bgu1', [128, NE * 16])
                dma('sp', bguT[:], D['b_guT'], 'c_bgu', w=['bguT'])
                ts('pool', bgu1[:], bguT[:], 1.0, ALU.add, r=['bguT'], w=['bgu1'])
                l2g = t5('l2g', [128, DM]); l2b = t5('l2b', [128, DM])
                dma('sp', l2g[:], pbc(D['ln2_g']), 'c_l2g', w=['l2g'])
                dma('sp', l2b[:], pbc(D['ln2_b']), 'c_l2b', w=['l2b'])
                acc = yacc[:, :, :].rearrange('p a (b c) -> p (a b) c', c=1024)
                hmT = t5('hmT', [128, 8, 1024], BF16)
                NR = 8
                ring = [t5('ring%d' % i, [128, 8, 512], BF16) for i in range(NR)]
                bdall = t5('bdall', [NE, DM])
                dma('sp', bdall[:], D['b_down'], 'c_bdall', w=['bdall'])
                gTt = [t5('gTt%d' % i, [NE, 128]) for i in range(2)]
                actT = t5('actT', [128, 8, 1024], BF16)
                NGS = 4
                Gt = [t5('Gt%d' % i, [128, 512]) for i in range(NGS)]
                Lt = [t5('Lt%d' % i, [128, 512]) for i in range(NGS)]
                Sg = [t5('Sg%d' % i, [128, 512]) for i in range(NGS)]
                pend = []
                h1t = [t5('h1t%d' % i, [128, DM]) for i in range(2)]
                st6 = [t5('st6_%d' % i, [128, 16]) for i in range(2)]
                rctr = 0
                ectr = 0
                bctr = 0
                for half in range(2):
                    for k in range(8):
                        dma('sp', hmT[:, k, :], D['hmT_scr'][k, :, half * 1024:(half + 1) * 1024], 'hmTl',
                            r=[('hmT', tg, k) for tg in range(half * 8, half * 8 + 8)], w=['hmT'])
                    for t in range(8):
                        tg = half * 8 + t
                        gT = gTt[t % 2]; gTk = 'gTt%d' % (t % 2)
                        ps, pk = nps()
                        tr(ps[0:NE, 0:128], gates[:, tg, :], identf[:], r=[('gates', tg), 'identf'], w=[pk])
                        cp('act', gT[:], ps[0:NE, 0:128], r=[pk], w=[gTk])
                        for dh in range(2):
                            ps2, pk2 = nps()
                            mm(ps2[:], gT[:], bdall[:, dh * 512:(dh + 1) * 512], True, True, r=[gTk, 'bdall'], w=[pk2])
                            cp('act', acc[:, t, dh * 512:(dh + 1) * 512], ps2[:], r=[pk2], w=[('acc', t, dh)])
                    NODMA = bool(_cfg('MOE_NODMA'))
                    for e in range(NE):
                        if NODMA and (e > 0 or half > 0):
                            units = list(range(6))
                        wgu_v = D['w_gate_up'][e].rearrange("(k p) c -> p k c", p=128)
                        wd_v = D['w_down'][e].rearrange("(k p) c -> p k c", p=128)
                        units = [] if not (NODMA and (e > 0 or half > 0)) else units
                        for c in range(4):
                            if NODMA and (e > 0 or half > 0):
                                break
                            ri_ = rctr % NR
                            rctr += 1
                            dma('pool', ring[ri_][:, :, 0:256], wgu_v[:, :, c * 256:(c + 1) * 256], 'ringa%d' % ri_, w=[('ring', ri_, 0)])
                            dma('pool', ring[ri_][:, :, 256:512], wgu_v[:, :, 1024 + c * 256:1024 + (c + 1) * 256], 'ringb%d' % ri_, w=[('ring', ri_, 1)])
                            units.append(ri_)
                        for dh in range(2):
                            if NODMA and (e > 0 or half > 0):
                                break
                            ri_ = rctr % NR
                            rctr += 1
                            dma('pool', ring[ri_][:, :, 0:256], wd_v[:, :, dh * 512:dh * 512 + 256], 'ringa%d' % ri_, w=[('ring', ri_, 0)])
                            dma('pool', ring[ri_][:, :, 256:512], wd_v[:, :, dh * 512 + 256:(dh + 1) * 512], 'ringb%d' % ri_, w=[('ring', ri_, 1)])
                            units.append(ri_)
                        for c in range(4):
                            ru = units[c]
                            for sub in range(2):
                                fi = 2 * c + sub
                                for tch in range(2):
                                    tok = slice(tch * 512, (tch + 1) * 512)
                                    psg, pgk = nps()
                                    psl, plk = nps()
                                    for k in range(8):
                                        mm(psg[:], ring[ru][:, k, sub * 128:(sub + 1) * 128], hmT[:, k, tok], k == 0, k == 7,
                                           r=[('ring', ru, 0), 'hmT'], w=[pgk])
                                    for k in range(8):
                                        mm(psl[:], ring[ru][:, k, 256 + sub * 128:256 + (sub + 1) * 128], hmT[:, k, tok], k == 0, k == 7,
                                           r=[('ring', ru, 1), 'hmT'], w=[plk])
                                    i = ectr % NGS
                                    ectr += 1
                                    G = Gt[i]; gk = 'Gt%d' % i
                                    L = Lt[i]; lk = 'Lt%d' % i
                                    Sg_ = Sg[i]; sgk = 'Sg%d' % i
                                    ts('dve', G[:], psg[:], bguT[:, e * 16 + fi:e * 16 + fi + 1], ALU.add, r=[pgk, 'bguT'], w=[gk], s2=7.0, op1=ALU.min)
                                    if not _cfg('MOE_SKIPL'):
                                        ts('dve', L[:], psl[:], bgu1[:, e * 16 + 8 + fi:e * 16 + 8 + fi + 1], ALU.add, r=[plk, 'bgu1'], w=[lk], s2=8.0, op1=ALU.min)
                                    act(Sg_[:], G[:], AF.Sigmoid, r=[gk], w=[sgk], scale=1.702)
                                    tt(_cfg('MOEGENG', 'pool'), G[:], G[:], Sg_[:], ALU.mult, r=[gk, sgk], w=[gk])
                                    pend.append((actT[:, fi, tok], L[:], G[:], lk, gk, ('actT', fi, tch)))
                                    if len(pend) > 2:
                                        o_, l_, g_, lk_, gk_, ak_ = pend.pop(0)
                                        stt('dve', o_, l_, -6.0, g_, ALU.max, ALU.mult, r=[lk_, gk_], w=[ak_])
                        while pend:
                            o_, l_, g_, lk_, gk_, ak_ = pend.pop(0)
                            stt('dve', o_, l_, -6.0, g_, ALU.max, ALU.mult, r=[lk_, gk_], w=[ak_])
                        for t in range(8):
                            tg = half * 8 + t
                            tch = t // 4
                            for dh in range(2):
                                ru = units[4 + dh]
                                ps, pk = nps()
                                for k in range(8):
                                    mm(ps[:], actT[:, k, t * 128:(t + 1) * 128], ring[ru][:, k, :], k == 0, k == 7,
                                       r=[('actT', k, tch), ('ring', ru, 0), ('ring', ru, 1)], w=[pk])
                                asl = acc[:, t, dh * 512:(dh + 1) * 512]
                                ak = ('acc', t, dh)
                                stt('dve', asl, ps[:], gates[:, tg, e:e + 1], asl, ALU.mult, ALU.add, r=[pk, ('gates', tg), ak], w=[ak])
                    for t in range(8):
                        tg = half * 8 + t
                        i = tg % 2
                        row = tg * 128
                        h = h1t[i]; hk = 'h1t%d' % i
                        st = st6[i]; sk = 'st6_%d' % i
                        x = acc[:, t, :]
                        xk0 = ('acc', t, 0); xk1 = ('acc', t, 1)
                        dma('sp', h[:], D['h1_scr'][row:row + 128, :], hk, r=[('h1scr', tg)], w=[hk])
                        tt(_cfg('LN2ENG', 'dve'), x, x, G2, ALU.mult, r=[xk0, xk1, 'modbc'], w=[xk0, xk1])
                        stt('dve', x, h[:], ALPHA, x, ALU.mult, ALU.add, r=[hk, xk0, xk1], w=[xk0, xk1])
                        S.op('dve', lambda e, st=st, x=x: e.bn_stats(out=st[:, 0:6], in_=x[:, 0:512]), [xk0, xk1], [sk])
                        S.op('dve', lambda e, st=st, x=x: e.bn_stats(out=st[:, 6:12], in_=x[:, 512:1024]), [xk0, xk1, sk], [sk])
                        S.op('dve', lambda e, st=st: e.bn_aggr(out=st[:, 12:14], in_=st[:, 0:12]), [sk], [sk])
                        rstd(st, sk)
                        ts('dve', x, x, st[:, 12:13], ALU.subtract, r=[xk0, xk1, sk], w=[xk0, xk1], s2=st[:, 14:15], op1=ALU.mult)
                        tt('dve', x, x, l2g[:], ALU.mult, r=[xk0, xk1, 'l2g'], w=[xk0, xk1])
                        tt(_cfg('LN2ENG', 'dve'), x, x, l2b[:], ALU.add, r=[xk0, xk1, 'l2b'], w=[xk0, xk1])
                        ev = dma('sp', D['out'][row:row + 128, :], x, 'outs%d' % i, r=[xk0, xk1])
                        finals.append(ev)
            return finals

        try:
            finals_ = body()
        except _Stop:
            finals_ = []
            for st_ in reversed(OPEN):
                st_.__exit__(None, None, None)
        finish(finals_)
    return nc


def _rope_tables(local_real):
    pos = np.asarray(local_real, dtype=np.int64)
    row = (pos // 64).astype(np.float64)
    col = (pos % 64).astype(np.float64)
    inv = 10000.0 ** (-np.arange(16, dtype=np.float64) / 16.0)
    ang = np.concatenate([row[None, :] * inv[:, None], col[None, :] * inv[:, None]], axis=0)
    c = np.cos(ang); s = np.sin(ang)
    C = np.concatenate([c, c, c, c], axis=0)
    Sg = np.concatenate([s, -s, s, -s], axis=0)
    return C.astype(np.float32), Sg.astype(np.float32)


def _make_inputs(inp, r):
    b, hf = r // 2, r % 2
    f = np.float32
    x = inp['x'][b]
    L = np.arange(4096) if hf == 0 else np.arange(4095, -1, -1)
    own = L[:2048]
    oth = L[2048:][::-1]
    m = {}
    m['x_own'] = np.ascontiguousarray(x[own])
    m['x_oth'] = np.ascontiguousarray(x[oth])
    cx = inp['ctx'][b]
    m['ctxb'] = np.ascontiguousarray(cx if hf == 0 else cx[::-1])
    cv = np.zeros((128, 8, 2), f)
    cv[:, :, 0] = inp['c'][b].reshape(8, 128).T
    cv[:, :, 1] = inp['c_ctx'].reshape(8, 128).T
    m['cvec'] = cv.reshape(128, 16)
    m['ln_in_g'] = inp['ln_in_g'].reshape(1, -1); m['ln_in_b'] = inp['ln_in_b'].reshape(1, -1)
    m['w_mod'] = inp['w_mod'][0]
    m['b_modT'] = np.ascontiguousarray(inp['b_mod'][0].reshape(48, 128).T)
    m['b_mod_row'] = inp['b_mod'][0].reshape(1, -1)
    m['w_in'] = inp['w_in'][0]
    dsel = [0, 1] if hf == 0 else [1, 0]

    def sp_layout(a):
        a = a[dsel].reshape(2, 16, 2, 64)
        return np.ascontiguousarray(a.transpose(2, 3, 0, 1).reshape(128, 32))
    m['lamre'] = sp_layout(inp['ssm_lam_re'][0]); m['lamim'] = sp_layout(inp['ssm_lam_im'][0])
    m['lstep'] = sp_layout(np.broadcast_to(inp['ssm_log_step'][0][:, :, None], (2, 32, 64)))

    def b_layout(a):
        a = a[dsel].reshape(2, 16, 2, 64, 16)
        return np.ascontiguousarray(a.transpose(2, 3, 0, 1, 4).reshape(128, 512))
    m['bre'] = b_layout(inp['ssm_b_re'][0]); m['bim'] = b_layout(inp['ssm_b_im'][0])

    def c_layout(a):
        a = a[dsel].reshape(2, 4, 8, 16, 64)
        return np.ascontiguousarray(a.transpose(2, 3, 0, 1, 4).reshape(128, 512))
    m['cre'] = c_layout(inp['ssm_c_re'][0]); m['cim'] = c_layout(inp['ssm_c_im'][0])
    m['dcol'] = np.ascontiguousarray(inp['ssm_d'][0].reshape(4, 128).T)
    gl = np.arange(128) // 16
    par = np.zeros((128, 4), f)
    par[:, 0] = (gl % 2 == 0); par[:, 1] = (gl % 2 == 1); par[:, 2] = -par[:, 0]; par[:, 3] = -par[:, 1]
    m['par'] = par
    m['w_glu'] = inp['w_glu'][0]
    m['b_gluT'] = np.ascontiguousarray(inp['b_glu'][0].reshape(4, 128).T)
    m['sinkrow'] = np.ascontiguousarray(np.repeat(inp['attn_sink'][0][[0, 2, 1, 3, 4, 6, 5, 7]], 128).reshape(1, 1024))
    m['w_ssm_out'] = inp['w_ssm_out'][0]; m['w_att_out'] = inp['w_att_out'][0]; m['w_o'] = inp['w_o'][0]
    m['ln1_g'] = inp['ln1_g'][0].reshape(1, -1); m['ln1_b'] = inp['ln1_b'][0].reshape(1, -1)
    m['w_router'] = inp['w_router'][0]; m['b_router'] = inp['b_router'][0].reshape(1, -1)
    m['w_gate_up'] = inp['w_gate_up'][0]
    m['b_guT'] = np.ascontiguousarray(inp['b_gate_up'][0].reshape(32, 16, 128).transpose(2, 0, 1).reshape(128, 512))
    m['w_down'] = inp['w_down'][0]; m['b_down'] = inp['b_down'][0]
    m['ln2_g'] = inp['ln2_g'][0].reshape(1, -1); m['ln2_b'] = inp['ln2_b'][0].reshape(1, -1)
    m['ident'] = np.eye(128, dtype=f)
    halo = oth[1920:2048]
    C, Sg = _rope_tables(np.concatenate([own, halo]))
    m['ropeC'] = C; m['ropeS'] = Sg
    ki = np.arange(128)[:, None]; qi = np.arange(128)[None, :]
    mk = np.zeros((128, 3, 128), f)
    mk[:, 0] = (qi <= ki); mk[:, 1] = (ki <= qi); mk[:, 2] = (ki + qi >= 127)
    m['masks'] = mk.reshape(128, 384)
    m['iota1'] = np.ascontiguousarray(np.broadcast_to(np.arange(1, 513, dtype=f)[None, :], (128, 512)))
    return {k: np.ascontiguousarray(v, dtype=f) for k, v in m.items()}


_NC_CACHE = {}


def kernel(**inputs):
    inp = {k: np.asarray(v) for k, v in inputs.items()}
    if 'nc' not in _NC_CACHE:
        _NC_CACHE['nc'] = build_nc()
    nc = _NC_CACHE['nc']
    in_maps = [_make_inputs(inp, r) for r in range(8)]
    res = run_bass_kernel_spmd(nc, in_maps, core_ids=list(range(8)))
    out = np.zeros((4, 4096, 1024), np.float32)
    for r in range(8):
        b, hf = r // 2, r % 2
        o = np.asarray(res.results[r]['out'])
        if hf == 0:
            out[b, :2048] = o
        else:
            out[b, 2048:] = o[::-1]
    return out
```
